# Optimizing a Trainium2 kernel written in Bass

```python
import jax, jax.numpy as jnp
from jax import lax
import numpy as np

D_MODEL = 1024
BATCH = 4
SEQ = 8192
DEPTH = 4

N_META = 16
BLOCK = 128
PAD = BLOCK - N_META

HEAD_DIM = 64
N_Q_HEADS = 16
N_KV_HEADS = 4
Q_PER_KV = N_Q_HEADS // N_KV_HEADS
WINDOW = 128
ROPE_THETA = 10000.0
ATTN_WIDTH = N_Q_HEADS * HEAD_DIM
KV_WIDTH = N_KV_HEADS * HEAD_DIM

D_INNER = 2 * D_MODEL
SSD_HEAD_DIM = 64
SSD_HEADS = D_INNER // SSD_HEAD_DIM
SSD_GROUPS = 4
SSD_HEADS_PER_GROUP = SSD_HEADS // SSD_GROUPS
SSD_STATE = 128
CONV_WIDTH = 4
CONV_DIM = D_INNER + 2 * SSD_GROUPS * SSD_STATE

IN_SIZES = (D_MODEL, D_MODEL, ATTN_WIDTH, KV_WIDTH, KV_WIDTH, D_INNER, CONV_DIM, SSD_HEADS)
IN_WIDTH = sum(IN_SIZES)

D_FF = 2816
N_EXPERTS = 8
TOP_K = 2
D_FF_EXPERT = 3584
N_DENSE = (DEPTH + 1) // 2
N_MOE = DEPTH // 2

ALPHA = (2 * DEPTH) ** 0.25
BETA = (8 * DEPTH) ** -0.25
LN_EPS = 1e-5
RMS_EPS = 1e-5

kernel_name = "hybrid_swa_ssd_moe_deepnorm"


def layer_norm(x, g, b):
    xf = x.astype(jnp.float32)
    mu = jnp.mean(xf, axis=-1, keepdims=True)
    var = jnp.mean(jnp.square(xf - mu), axis=-1, keepdims=True)
    y = (xf - mu) * lax.rsqrt(var + LN_EPS) * g.astype(jnp.float32) + b.astype(jnp.float32)
    return y.astype(x.dtype)


def rope(x, cos, sin):
    c = cos.astype(x.dtype)[None, :, None, :]
    s = sin.astype(x.dtype)[None, :, None, :]
    x1, x2 = jnp.split(x, 2, axis=-1)
    return jnp.concatenate([x1 * c - x2 * s, x2 * c + x1 * s], axis=-1)


def sliding_window_attention(q, k, v, sinks):
    b, T = q.shape[0], q.shape[1]
    nb = T // BLOCK
    qb = q.reshape(b, nb, BLOCK, N_KV_HEADS, Q_PER_KV, HEAD_DIM)
    kb = k.reshape(b, nb, BLOCK, N_KV_HEADS, HEAD_DIM)
    vb = v.reshape(b, nb, BLOCK, N_KV_HEADS, HEAD_DIM)
    shift = ((0, 0), (1, 0), (0, 0), (0, 0), (0, 0))
    kk = jnp.concatenate([jnp.pad(kb[:, :-1], shift), kb], axis=2)
    vv = jnp.concatenate([jnp.pad(vb[:, :-1], shift), vb], axis=2)
    s = jnp.einsum("bnqgrd,bnkgd->bngrqk", qb, kk).astype(jnp.float32) * (HEAD_DIM ** -0.5)
    i = jnp.arange(BLOCK)[:, None]
    j = jnp.arange(2 * BLOCK)[None, :]
    rel = BLOCK + i - j
    band = (rel >= 0) & (rel < WINDOW)
    kpos = (jnp.arange(nb)[:, None] - 1) * BLOCK + jnp.arange(2 * BLOCK)[None, :]
    kvalid = kpos >= PAD
    mask = band[None] & kvalid[:, None, :]
    s = jnp.where(mask[None, :, None, None], s, -jnp.inf)
    sink = sinks.astype(jnp.float32).reshape(N_KV_HEADS, Q_PER_KV)[None, None, :, :, None, None]
    m = jnp.maximum(jnp.max(s, axis=-1, keepdims=True), sink)
    p = jnp.exp(s - m)
    probs = p / (jnp.sum(p, axis=-1, keepdims=True) + jnp.exp(sink - m))
    o = jnp.einsum("bngrqk,bnkgd->bnqgrd", probs.astype(v.dtype), vv)
    return o.reshape(b, T, ATTN_WIDTH)


def causal_depthwise_conv(x, w, bias):
    y = lax.conv_general_dilated(
        x, w[:, None, :], window_strides=(1,), padding=[(CONV_WIDTH - 1, 0)],
        dimension_numbers=("NWC", "WIO", "NWC"), feature_group_count=x.shape[-1])
    return y + bias


def ssd_chunked(x, dt, a_log, bm, cm, d_skip):
    b, T = x.shape[0], x.shape[1]
    nc = T // BLOCK
    dtype = x.dtype
    a = -jnp.exp(a_log.astype(jnp.float32))
    da = dt * a
    xc = (x * dt[..., None].astype(dtype)).reshape(b, nc, BLOCK, SSD_GROUPS, SSD_HEADS_PER_GROUP, SSD_HEAD_DIM)
    bc = bm.reshape(b, nc, BLOCK, SSD_GROUPS, SSD_STATE)
    cc = cm.reshape(b, nc, BLOCK, SSD_GROUPS, SSD_STATE)
    a_cum = jnp.cumsum(jnp.moveaxis(da.reshape(b, nc, BLOCK, SSD_GROUPS, SSD_HEADS_PER_GROUP), 2, -1), axis=-1)
    causal = jnp.tril(jnp.ones((BLOCK, BLOCK), dtype=bool))
    seg = a_cum[..., :, None] - a_cum[..., None, :]
    lmat = jnp.exp(jnp.where(causal, seg, -jnp.inf)).astype(dtype)
    cb = jnp.einsum("bclgn,bcsgn->bcgls", cc, bc)
    y_diag = jnp.einsum("bcgrls,bcsgrp->bclgrp", cb[:, :, :, None] * lmat, xc)
    decay_states = jnp.exp(a_cum[..., -1:] - a_cum).astype(dtype)
    states = jnp.einsum("bclgn,bcgrl,bclgrp->bcgrpn", bc, decay_states, xc)
    chunk_decay = jnp.exp(a_cum[..., -1]).astype(dtype)

    def step(h, inp):
        st, dec = inp
        return h * dec[..., None, None] + st, h

    h0 = jnp.zeros_like(states[:, 0])
    _, state_in = lax.scan(step, h0, (jnp.moveaxis(states, 1, 0), jnp.moveaxis(chunk_decay, 1, 0)))
    state_in = jnp.moveaxis(state_in, 0, 1)
    y_off = jnp.einsum("bclgn,bcgrpn,bcgrl->bclgrp", cc, state_in, jnp.exp(a_cum).astype(dtype))
    y = (y_diag + y_off).reshape(b, T, SSD_HEADS, SSD_HEAD_DIM) + x * d_skip.astype(dtype)[:, None]
    return y.astype(dtype)


def gated_rmsnorm(y, z, g):
    u = (y * jax.nn.silu(z)).astype(jnp.float32)
    ug = u.reshape(u.shape[:-1] + (SSD_GROUPS, D_INNER // SSD_GROUPS))
    ug = ug * lax.rsqrt(jnp.mean(jnp.square(ug), axis=-1, keepdims=True) + RMS_EPS)
    return (ug.reshape(u.shape) * g.astype(jnp.float32)).astype(y.dtype)


def hybrid_mixer(h, w_in, conv_w, conv_b, dt_bias, a_log, d_skip, ssd_norm_g, sinks,
                 w_attn_out, w_ssd_out, w_o, cos, sin, valid):
    b, T, _ = h.shape
    proj = h @ w_in
    g_a, g_s, q, k, v, z, xbc, dt_raw = jnp.split(proj, list(np.cumsum(IN_SIZES)[:-1]), axis=-1)
    q = rope(q.reshape(b, T, N_Q_HEADS, HEAD_DIM), cos, sin)
    k = rope(k.reshape(b, T, N_KV_HEADS, HEAD_DIM), cos, sin)
    attn = sliding_window_attention(q, k, v.reshape(b, T, N_KV_HEADS, HEAD_DIM), sinks)
    xbc = jax.nn.silu(causal_depthwise_conv(xbc * valid, conv_w, conv_b))
    xs, bm, cm = jnp.split(xbc, [D_INNER, D_INNER + SSD_GROUPS * SSD_STATE], axis=-1)
    xs = xs * valid
    dt = jax.nn.softplus(dt_raw.astype(jnp.float32) + dt_bias.astype(jnp.float32))
    y = ssd_chunked(xs.reshape(b, T, SSD_HEADS, SSD_HEAD_DIM), dt, a_log,
                    bm.reshape(b, T, SSD_GROUPS, SSD_STATE), cm.reshape(b, T, SSD_GROUPS, SSD_STATE), d_skip)
    y = gated_rmsnorm(y.reshape(b, T, D_INNER), z, ssd_norm_g)
    merged = jax.nn.sigmoid(g_a) * (attn @ w_attn_out) + jax.nn.sigmoid(g_s) * (y @ w_ssd_out)
    return merged @ w_o


def swiglu(h, wg, wu, wd):
    return (jax.nn.silu(h @ wg) * (h @ wu)) @ wd


def moe_swiglu(h, w_router, wg, wu, wd):
    b, T, d = h.shape
    hf = h.reshape(b * T, d)
    logits = (hf @ w_router).astype(jnp.float32)
    top_v, top_i = lax.top_k(logits, TOP_K)
    top_w = jax.nn.softmax(top_v, axis=-1)
    gate = jnp.sum(jax.nn.one_hot(top_i, N_EXPERTS, dtype=jnp.float32) * top_w[..., None], axis=1).astype(h.dtype)
    out = jnp.zeros_like(hf)
    for e in range(N_EXPERTS):
        out = out + gate[:, e:e + 1] * swiglu(hf, wg[e], wu[e], wd[e])
    return out.reshape(b, T, d)


def setup_inputs(seed: int = 0) -> dict:
    key = jax.random.key(seed)
    ks = jax.random.split(key, 32)
    nrm = lambda k, shape, scale: jax.random.normal(k, shape, jnp.float32) * scale
    dt0 = jnp.exp(jax.random.uniform(ks[5], (DEPTH, SSD_HEADS), jnp.float32, np.log(1e-3), np.log(1e-1)))
    return {
        "x": nrm(ks[0], (BATCH, SEQ, D_MODEL), 1.0),
        "meta_tokens": nrm(ks[1], (N_META, D_MODEL), 1.0),
        "ln_in_g": 1.0 + nrm(ks[2], (D_MODEL,), 0.02),
        "ln_in_b": nrm(ks[3], (D_MODEL,), 0.02),
        "w_in": nrm(ks[4], (DEPTH, D_MODEL, IN_WIDTH), D_MODEL ** -0.5),
        "conv_w": nrm(ks[6], (DEPTH, CONV_WIDTH, CONV_DIM), CONV_WIDTH ** -0.5),
        "conv_b": nrm(ks[7], (DEPTH, CONV_DIM), 0.02),
        "dt_bias": dt0 + jnp.log(-jnp.expm1(-dt0)),
        "a_log": jnp.log(jax.random.uniform(ks[8], (DEPTH, SSD_HEADS), jnp.float32, 1.0, 16.0)),
        "d_skip": 1.0 + nrm(ks[9], (DEPTH, SSD_HEADS), 0.1),
        "ssd_norm_g": 1.0 + nrm(ks[10], (DEPTH, D_INNER), 0.02),
        "sinks": nrm(ks[11], (DEPTH, N_Q_HEADS), 0.5),
        "w_attn_out": nrm(ks[12], (DEPTH, ATTN_WIDTH, D_MODEL), ATTN_WIDTH ** -0.5),
        "w_ssd_out": nrm(ks[13], (DEPTH, D_INNER, D_MODEL), D_INNER ** -0.5),
        "w_o": nrm(ks[14], (DEPTH, D_MODEL, D_MODEL), BETA * D_MODEL ** -0.5),
        "ln1_g": 1.0 + nrm(ks[15], (DEPTH, D_MODEL), 0.02),
        "ln1_b": nrm(ks[16], (DEPTH, D_MODEL), 0.02),
        "ffn_wg": nrm(ks[17], (N_DENSE, D_MODEL, D_FF), D_MODEL ** -0.5),
        "ffn_wu": nrm(ks[18], (N_DENSE, D_MODEL, D_FF), D_MODEL ** -0.5),
        "ffn_wd": nrm(ks[19], (N_DENSE, D_FF, D_MODEL), BETA * D_FF ** -0.5),
        "moe_router": nrm(ks[20], (N_MOE, D_MODEL, N_EXPERTS), D_MODEL ** -0.5),
        "moe_wg": nrm(ks[21], (N_MOE, N_EXPERTS, D_MODEL, D_FF_EXPERT), D_MODEL ** -0.5),
        "moe_wu": nrm(ks[22], (N_MOE, N_EXPERTS, D_MODEL, D_FF_EXPERT), D_MODEL ** -0.5),
        "moe_wd": nrm(ks[23], (N_MOE, N_EXPERTS, D_FF_EXPERT, D_MODEL), BETA * D_FF_EXPERT ** -0.5),
        "ln2_g": 1.0 + nrm(ks[24], (DEPTH, D_MODEL), 0.02),
        "ln2_b": nrm(ks[25], (DEPTH, D_MODEL), 0.02),
    }


def reference(x, meta_tokens, ln_in_g, ln_in_b, w_in, conv_w, conv_b, dt_bias, a_log, d_skip,
              ssd_norm_g, sinks, w_attn_out, w_ssd_out, w_o, ln1_g, ln1_b, ffn_wg, ffn_wu, ffn_wd,
              moe_router, moe_wg, moe_wu, moe_wd, ln2_g, ln2_b):
    b, s, d = x.shape
    T = s + BLOCK
    h = jnp.concatenate([jnp.zeros((b, PAD, d), x.dtype),
                         jnp.broadcast_to(meta_tokens.astype(x.dtype)[None], (b, N_META, d)), x], axis=1)
    h = layer_norm(h, ln_in_g, ln_in_b)
    pos = (jnp.arange(T) - PAD).astype(jnp.float32)
    inv_freq = ROPE_THETA ** (-jnp.arange(0, HEAD_DIM, 2, dtype=jnp.float32) / HEAD_DIM)
    ang = pos[:, None] * inv_freq[None, :]
    cos, sin = jnp.cos(ang), jnp.sin(ang)
    valid = (jnp.arange(T) >= PAD).astype(x.dtype)[None, :, None]
    for l in range(DEPTH):
        mix = hybrid_mixer(h, w_in[l], conv_w[l], conv_b[l], dt_bias[l], a_log[l], d_skip[l],
                           ssd_norm_g[l], sinks[l], w_attn_out[l], w_ssd_out[l], w_o[l], cos, sin, valid)
        h = layer_norm(ALPHA * h + mix, ln1_g[l], ln1_b[l])
        if l % 2 == 0:
            f = swiglu(h, ffn_wg[l // 2], ffn_wu[l // 2], ffn_wd[l // 2])
        else:
            f = moe_swiglu(h, moe_router[l // 2], moe_wg[l // 2], moe_wu[l // 2], moe_wd[l // 2])
        h = layer_norm(ALPHA * h + f, ln2_g[l], ln2_b[l])
    return h[:, BLOCK:]
```

```python
import numpy as np
from contextlib import ExitStack
import concourse.bass as bass
import concourse.mybir as mybir
from concourse.bass_utils import run_bass_kernel_spmd

F32 = mybir.dt.float32
BF16 = mybir.dt.bfloat16
AF = mybir.ActivationFunctionType
ALU = mybir.AluOpType
AX = mybir.AxisListType

D = 1024; KC = 8; TT = 512; NB = 4; BLK = 128
DEPTH = 4; PAD = 112; NMETA = 16
DFF = 2816; DFFE = 3584; NE = 8
ALPHA = (2 * DEPTH) ** 0.25
LN_EPS = 1e-5; RMS_EPS = 1e-5
NEG = -30000.0
PAGE = 512
NPP = 288


class View:
    def __init__(self, ap, key, lo, hi):
        self.ap = ap; self.key = key; self.lo = lo; self.hi = hi

    def pages(self):
        if self.key is None:
            return []
        return [(self.key, p) for p in range(self.lo // PAGE, (self.hi - 1) // PAGE + 1)]


class Op:
    __slots__ = ("eng", "fn", "deps", "idx", "inc", "is_dma", "sem", "semval", "has_dep", "epoch", "noval")

    def __init__(self, eng, fn):
        self.eng = eng; self.fn = fn; self.deps = []; self.inc = None
        self.is_dma = False; self.sem = None; self.semval = 0; self.has_dep = False; self.noval = False


class Prog:
    ENGS = ("pe", "act", "dve", "pool", "sp")

    def __init__(self, nc, es):
        self.nc = nc; self.es = es
        self.q = {e: [] for e in self.ENGS}
        self.pw = {}; self.pr = {}
        self.dsem = {}; self.dcnt = {}
        self.toggle = 0
        self.const_keys = set()
        self.epoch = 0

    def op(self, eng, fn, reads=(), writes=(), dma_sem=None):
        o = Op(eng, fn); o.epoch = self.epoch
        deps = {}
        def add(p):
            if p is None or p is o:
                return
            deps[id(p)] = p
        for v in reads:
            for pg in v.pages():
                add(self.pw.get(pg))
        for v in writes:
            for pg in v.pages():
                add(self.pw.get(pg))
                for r in self.pr.get(pg, ()):
                    add(r)
        for v in writes:
            for pg in v.pages():
                self.pw[pg] = o; self.pr[pg] = []
        for v in reads:
            if v.key in self.const_keys:
                continue
            for pg in v.pages():
                self.pr.setdefault(pg, []).append(o)
        best = {}
        for p in deps.values():
            if p.is_dma:
                best[("d", id(p))] = p
            else:
                if p.eng == o.eng and (o.eng == "pe"):
                    continue
                b = best.get(p.eng)
                if b is None or p.idx > b.idx:
                    best[p.eng] = p
        o.deps = list(best.values())
        for p in o.deps:
            p.has_dep = True
        if dma_sem is not None:
            o.is_dma = True
            if dma_sem[0] == "w":
                dma_sem = dma_sem + "_%d" % self.epoch
            if dma_sem not in self.dsem:
                self.dsem[dma_sem] = self.es.enter_context(self.nc.semaphore("d_" + dma_sem))
                self.dcnt[dma_sem] = 0
            self.dcnt[dma_sem] += 16
            o.sem = self.dsem[dma_sem]; o.semval = self.dcnt[dma_sem]
        o.idx = len(self.q[eng])
        self.q[eng].append(o)
        return o

    def emit(self, final_waits):
        nc = self.nc
        esem = {}
        for e in self.ENGS:
            c = {}
            for o in self.q[e]:
                if o.is_dma:
                    continue
                if o.has_dep:
                    c[o.epoch] = c.get(o.epoch, 0) + 1; o.inc = c[o.epoch]
                    if (e, o.epoch) not in esem:
                        esem[(e, o.epoch)] = self.es.enter_context(nc.semaphore("e_%s_%d" % (e, o.epoch)))
        def run(e, eng):
            waited = {}
            for o in self.q[e]:
                for p in o.deps:
                    if p.is_dma:
                        s, v = p.sem, p.semval
                    else:
                        s, v = esem[(p.eng, p.epoch)], p.inc
                    k = id(s)
                    if waited.get(k, 0) >= v:
                        continue
                    waited[k] = v
                    eng.wait_ge(s, v)
                ins = o.fn(eng)
                if o.is_dma:
                    ins.then_inc(o.sem, 16)
                elif o.inc is not None:
                    if o.noval:
                        ins.then_inc(esem[(e, o.epoch)])
                    else:
                        ins.then_inc(esem[(e, o.epoch)], 1)
            if e == "sp":
                for (s, v) in final_waits():
                    eng.wait_ge(s, v)
        with nc.Block() as block:
            @block.tensor
            def _(eng): run("pe", eng)
            @block.scalar
            def _(eng): run("act", eng)
            @block.vector
            def _(eng): run("dve", eng)
            @block.gpsimd
            def _(eng): run("pool", eng)
            @block.sync
            def _(eng): run("sp", eng)


class Arena:
    def __init__(self, nc, es, name, nbytes, dt=F32):
        self.t = es.enter_context(nc.sbuf_tensor(name, [128, nbytes // 4], F32))
        self.key = name; self.nbytes = nbytes; self.top = 0

    def alloc(self, nbytes):
        o = self.top; self.top += (nbytes + 63) // 64 * 64
        assert self.top <= self.nbytes, (self.key, self.top, self.nbytes)
        return o

    def view(self, off, dt, shape, part=(0, 128)):
        esz = 4 if dt == F32 else 2
        n = int(np.prod(shape))
        ap = self.t[part[0]:part[1], off // 4:(off + n * esz) // 4]
        if dt != F32:
            ap = ap.bitcast(dt)
        if len(shape) == 2:
            ap = ap.rearrange("p (a b) -> p a b", a=shape[0])
        elif len(shape) == 3:
            ap = ap.rearrange("p (a b c) -> p a b c", a=shape[0], b=shape[1])
        return View(ap, self.key, off, off + n * esz)


def build(cfg):
    layers = cfg["layers"]; NL = len(layers); NSTEP = cfg["nstep"]
    nc = bass.Bass("TRN2", target_bir_lowering=False)
    es = ExitStack()
    P = Prog(nc, es)

    def din(name, shape, dt=F32):
        return nc.dram_tensor(name, list(shape), dt, kind="ExternalInput").ap()

    x_d = din("x", [NSTEP, NB, 128, D])
    cs_d = din("cossin", [NSTEP, 2, 128, TT])
    cst_d = din("cst", [128, 6 * 128 + 2 * 512 + 2])
    fb_d = din("fb", [128, 1]); vfm_d = din("vfm", [2, 128, TT]); vkb_d = din("vkb", [128, 8])
    send_d = nc.dram_tensor("send", [TT, D], F32, kind="Internal").ap()
    gath_d = nc.dram_tensor("gath", [2 * TT, D], F32, kind="Internal").ap()
    lnin_d = din("lnin", [128, 16])
    pp_d = din("pp", [128, NL * NPP])
    wfm_d = [din(f"wfm{l}", [D, 3072 + 3072 + 2048]) for l in range(NL)]
    wtm_d = [din(f"wtm{l}", [D, 512 + 2048 + 32]) for l in range(NL)]
    wao_d = [din(f"wao{l}", [D, D]) for l in range(NL)]
    wso_d = [din(f"wso{l}", [2 * D, D]) for l in range(NL)]
    wo_d = [din(f"wo{l}", [D, D]) for l in range(NL)]
    wg_d = []; wu_d = []; wd_d = []; wr_d = []
    for l, kind in enumerate(layers):
        if kind == "dense":
            wg_d.append(din(f"wg{l}", [1, D, DFF])); wu_d.append(din(f"wu{l}", [1, D, DFF]))
            wd_d.append(din(f"wd{l}", [1, DFF, D])); wr_d.append(None)
        else:
            wg_d.append(din(f"wg{l}", [NE, D, DFFE])); wu_d.append(din(f"wu{l}", [NE, D, DFFE]))
            wd_d.append(din(f"wd{l}", [NE, DFFE, D])); wr_d.append(din(f"wr{l}", [D, NE]))
    y_d = nc.dram_tensor("y", [NSTEP, NB, 128, D], F32, kind="ExternalOutput").ap()
    st_d = [nc.dram_tensor(f"state{l}", [128, 2048], F32, kind="Internal").ap() for l in range(NL)]

    PER = Arena(nc, es, "per", 84 * 1024)
    WR = Arena(nc, es, "wring", 32 * 1024)
    TR = Arena(nc, es, "trans", 88 * 1024)
    NSLOT = 4
    wslots = [WR.alloc(8192) for _ in range(NSLOT)]
    hres = PER.view(PER.alloc(KC * TT * 4), F32, [KC, TT])
    hbf = PER.view(PER.alloc(KC * TT * 2), BF16, [KC, TT])
    cst = PER.view(PER.alloc((6 * 128 + 1024 + 2) * 4), F32, [6 * 128 + 1024 + 2])
    fbv = PER.view(PER.alloc(64), F32, [1]); vkb = PER.view(PER.alloc(64), F32, [8])
    vfm = [PER.view(PER.alloc(TT * 4), F32, [TT]) for _ in range(2)]
    cstb = PER.view(PER.alloc((2 * 128 + 1024) * 2), BF16, [2 * 128 + 1024])
    lnin = PER.view(PER.alloc(64), F32, [16])
    pp = PER.view(PER.alloc(NL * NPP * 4), F32, [NL * NPP])
    cosv = PER.view(PER.alloc(TT * 4), F32, [TT]); sinv = PER.view(PER.alloc(TT * 4), F32, [TT])
    tails = [PER.view(PER.alloc(24 * 3 * 4), F32, [24, 3]) for _ in range(NL)]
    kprev = [PER.view(PER.alloc(2 * 4 * 128 * 2), BF16, [2, 4, 128]) for _ in range(NL)]
    vprev = [PER.view(PER.alloc(512 * 2), BF16, [512]) for _ in range(NL)]
    attnT = PER.view(PER.alloc(KC * TT * 2), BF16, [KC, TT])
    unT = PER.view(PER.alloc(16 * TT * 2), BF16, [16, TT])
    P.const_keys.add("cstk")
    for v in (cst, cstb, lnin, pp, fbv, vkb, vfm[0], vfm[1]):
        v.key = "cstk"
    def csub(v, a, b):
        return View(v.ap[:, a:b], v.key, v.lo, v.hi)
    identf = csub(cst, 0, 128); tri = csub(cst, 128, 256); ustr = csub(cst, 256, 384)
    onesD = csub(cst, 384, 512); onesf = csub(cst, 512, 640); sel = csub(cst, 640, 768)
    kb0 = csub(cst, 768 + 1024, 768 + 1025); kball = csub(cst, 768 + 1025, 768 + 1026)
    identb = csub(cstb, 0, 128); onesb = csub(cstb, 128, 256)
    mprev = csub(cstb, 256, 768); mcur = csub(cstb, 768, 1280)

    psum = [es.enter_context(nc.psum_tensor(f"ps{i}", [128, 512], F32)) for i in range(8)]
    pstate = {"i": 0}
    def bank(dt=F32, shape=None):
        i = pstate["i"] % 8; pstate["i"] += 1
        ap = psum[i][:, :]
        n = 512
        if dt == BF16:
            ap = ap.bitcast(BF16); n = 1024
        v = View(ap, "psum", i * 2048, (i + 1) * 2048)
        return v
    def sub(v, *idx):
        return View(v.ap[idx], v.key, v.lo, v.hi)

    class TAlloc:
        def __init__(self): self.base = 0
        def reset(self, to=0): TR.top = to
        def get(self, dt, shape):
            esz = 4 if dt == F32 else 2
            return TR.view(TR.alloc(int(np.prod(shape)) * esz), dt, shape)
    T = TAlloc()

    def mm(out, lhsT, rhs, start=True, stop=True):
        return P.op("pe", lambda e: e.matmul(out.ap, lhsT.ap, rhs.ap, start=start, stop=stop),
                    reads=[lhsT, rhs] + ([] if start else []), writes=[out])
    def tp(out, in_, ident):
        return P.op("pe", lambda e: e.transpose(out.ap, in_.ap, ident.ap), reads=[in_, ident], writes=[out])
    def act(out, in_, func, bias=None, scale=1.0, accum=None):
        rd = [in_]; kw = {}
        if isinstance(bias, View): rd.append(bias); kw["bias"] = bias.ap
        elif bias is not None: kw["bias"] = float(bias)
        if isinstance(scale, View): rd.append(scale); kw["scale"] = scale.ap
        else: kw["scale"] = float(scale)
        wr = [out]
        if accum is not None: wr.append(accum); kw["accum_out"] = accum.ap
        return P.op("act", lambda e: e.activation(out.ap, in_.ap, func, **kw), reads=rd, writes=wr)
    def tt(out, a, b, op, eng="dve"):
        return P.op(eng, lambda e: e.tensor_tensor(out.ap, a.ap, b.ap, op), reads=[a, b], writes=[out])
    def ts(out, a, s1, s2, op0, op1=None, eng="dve"):
        rd = [a]
        s1a = s1.ap if isinstance(s1, View) else float(s1)
        if isinstance(s1, View): rd.append(s1)
        s2a = None
        if s2 is not None:
            s2a = s2.ap if isinstance(s2, View) else float(s2)
            if isinstance(s2, View): rd.append(s2)
        if op1 is None:
            return P.op(eng, lambda e: e.tensor_scalar(out.ap, a.ap, s1a, None, op0), reads=rd, writes=[out])
        return P.op(eng, lambda e: e.tensor_scalar(out.ap, a.ap, s1a, s2a, op0, op1), reads=rd, writes=[out])
    def stt(out, a, s, b, op0, op1, eng="dve"):
        rd = [a, b]
        sa = s.ap if isinstance(s, View) else float(s)
        if isinstance(s, View): rd.append(s)
        return P.op(eng, lambda e: e.scalar_tensor_tensor(out.ap, a.ap, sa, b.ap, op0, op1), reads=rd, writes=[out])
    def cp(out, in_, eng=None):
        if eng is None:
            P.toggle ^= 1; eng = "act" if P.toggle else "dve"
        if eng == "act":
            return act(out, in_, AF.Copy)
        return P.op("dve", lambda e: e.tensor_copy(out.ap, in_.ap), reads=[in_], writes=[out])
    def recip(out, in_):
        return P.op("dve", lambda e: e.reciprocal(out.ap, in_.ap), reads=[in_], writes=[out])
    def red(out, in_, op=ALU.add):
        return P.op("dve", lambda e: e.tensor_reduce(out.ap, in_.ap, AX.X, op), reads=[in_], writes=[out])
    def memset(v, val, eng="dve"):
        return P.op(eng, lambda e: e.memset(v.ap, val), writes=[v])
    def dma(q, out, in_, sem, rd=(), wr=()):
        return P.op(q, lambda e: e.dma_start(out=out, in_=in_), reads=list(rd), writes=list(wr), dma_sem=sem)

    wcnt = {"i": 0}
    def wload(dram_ap, shape):
        i = wcnt["i"] % NSLOT; wcnt["i"] += 1
        v = WR.view(wslots[i], BF16, shape)
        dma("pool", v.ap, dram_ap, f"w{i}", wr=[v])
        return v

    def wtile_fm(w_d, k0, nk, c0, ncols):
        return wload(w_d[k0 * 128:(k0 + nk) * 128, c0:c0 + ncols].rearrange("(c p) n -> p c n", p=128), [nk, ncols])

    dma("sp", cst.ap, cst_d, "c0", wr=[cst])
    dma("sp", lnin.ap, lnin_d, "c1", wr=[lnin])
    dma("sp", pp.ap, pp_d, "c2", wr=[pp])
    dma("sp", fbv.ap, fb_d, "c3", wr=[fbv]); dma("sp", vkb.ap, vkb_d, "c4", wr=[vkb])
    dma("sp", vfm[0].ap, vfm_d[0], "c5", wr=[vfm[0]]); dma("sp", vfm[1].ap, vfm_d[1], "c6", wr=[vfm[1]])
    gathv = View(None, "gathd", 0, 1); sendv = View(None, "sendd", 0, 1)
    P.op("dve", lambda e: e.tensor_copy(cstb.ap[:, 0:128], cst.ap[:, 0:128]), reads=[cst], writes=[cstb])
    P.op("dve", lambda e: e.tensor_copy(cstb.ap[:, 128:256], cst.ap[:, 512:640]), reads=[cst], writes=[cstb])
    P.op("dve", lambda e: e.tensor_copy(cstb.ap[:, 256:1280], cst.ap[:, 768:1792]), reads=[cst], writes=[cstb])
    for l in range(NL):
        memset(tails[l], 0.0); memset(kprev[l], 0.0); memset(vprev[l], 0.0)
        zt = T.get(F32, [2048]) if l == 0 else zt
        if l == 0:
            memset(zt, 0.0)
        dma("sp", st_d[l], zt.ap, f"st{l}", rd=[zt], wr=[View(None, f"std{l}", 0, 1)])
    stv = [View(None, f"std{l}", 0, 1) for l in range(NL)]
    for b in range(NB):
        dma("sp", gath_d[b * 128:(b + 1) * 128, :], zt.ap[:, 0:D], f"gz{b}", rd=[zt], wr=[gathv])

    def ppv(l, a, b):
        return View(pp.ap[:, l * NPP + a:l * NPP + b], pp.key, pp.lo, pp.hi)

    def ln_fm(src, g, b, t0):
        TR.top = t0
        s1 = T.get(F32, [TT]); s2 = T.get(F32, [TT]); sq = T.get(F32, [KC, TT])
        red(s1, View(src.ap.rearrange("p c t -> p t c"), src.key, src.lo, src.hi))
        act(sq, src, AF.Square)
        red(s2, View(sq.ap.rearrange("p c t -> p t c"), sq.key, sq.lo, sq.hi))
        pm = bank(); pq = bank()
        mm(pm, onesD, s1); mm(pq, onesD, s2)
        mean = T.get(F32, [TT]); var = T.get(F32, [TT]); rstd = T.get(F32, [TT])
        cp(mean, pm, eng="act")
        tt(var, mean, mean, ALU.mult)
        tt(var, pq, var, ALU.subtract)
        ts(var, var, LN_EPS, None, ALU.add)
        act(var, var, AF.Sqrt)
        recip(rstd, var)
        tmps = [T.get(F32, [TT]), T.get(F32, [TT])]
        for c in range(KC):
            tmp = tmps[c % 2]
            tt(tmp, sub(src, slice(None), c), mean, ALU.subtract)
            tt(tmp, tmp, rstd, ALU.mult)
            act(sub(hres, slice(None), c), tmp, AF.Identity, bias=sub(b, slice(None), slice(c, c + 1)),
                scale=sub(g, slice(None), slice(c, c + 1)))
            cp(sub(hbf, slice(None), c), sub(hres, slice(None), c), eng="dve")

    def proj_fm(w_d, c0, nchunks, src, nk, consume, group=4):
        for j0 in range(0, nchunks, group):
            ng = min(group, nchunks - j0)
            wt = wtile_fm(w_d, 0, nk, c0 + j0 * 128, ng * 128) if nk * ng * 128 <= 4096 else None
            for jj in range(ng):
                ps = bank()
                for k in range(nk):
                    mm(ps, sub(wt, slice(None), k, slice(jj * 128, (jj + 1) * 128)), sub(src, slice(None), k),
                       start=(k == 0), stop=(k == nk - 1))
                consume(j0 + jj, ps)

    out_sems = []
    for s in range(NSTEP):
        P.epoch = s // cfg.get("epoch_steps", 3)
        TR.top = 0
        dma("sp", cosv.ap, cs_d[s, 0], "cos", wr=[cosv])
        dma("sp", sinv.ap, cs_d[s, 1], "sin", wr=[sinv])
        for b in range(NB):
            xt = T.get(F32, [D])
            dma("sp", xt.ap, x_d[s, b], f"x{b}", wr=[xt])
            ssum = T.get(F32, [1]); ssq = T.get(F32, [1]); junk = T.get(F32, [D])
            act(junk, xt, AF.Identity, accum=ssum)
            act(junk, xt, AF.Square, accum=ssq)
            mean = T.get(F32, [1]); var = T.get(F32, [1]); nmr = T.get(F32, [1])
            ts(mean, ssum, 1.0 / D, None, ALU.mult)
            ts(var, ssq, 1.0 / D, None, ALU.mult)
            tt(nmr, mean, mean, ALU.mult)
            tt(var, var, nmr, ALU.subtract)
            ts(var, var, LN_EPS, None, ALU.add)
            act(var, var, AF.Sqrt)
            recip(var, var)
            ts(xt, xt, mean, var, ALU.subtract, ALU.mult)
            rt = T.get(F32, [D])
            dma("sp", rt.ap, gath_d[b * 128:(b + 1) * 128, :], f"r{b}", rd=[gathv], wr=[rt])
            stt(xt, rt, sub(fbv, slice(None), slice(0, 1)), xt, ALU.mult, ALU.add)
            for c0 in range(0, KC, 4):
                ps = bank()
                for c in range(4):
                    tp(sub(ps, slice(None), slice(c * 128, (c + 1) * 128)), sub(xt, slice(None), slice((c0 + c) * 128, (c0 + c + 1) * 128)), identf)
                for c in range(4):
                    act(sub(hres, slice(None), c0 + c, slice(b * 128, (b + 1) * 128)), sub(ps, slice(None), slice(c * 128, (c + 1) * 128)),
                        AF.Identity, bias=sub(lnin, slice(None), slice(8 + c0 + c, 9 + c0 + c)), scale=sub(lnin, slice(None), slice(c0 + c, c0 + c + 1)))
        cp(hbf, hres, eng="dve")

        for l, kind in enumerate(layers):
            cw = View(pp.ap[:, l * NPP:l * NPP + 96].rearrange("p (c k) -> p c k", k=4), pp.key, pp.lo, pp.hi)
            cb = ppv(l, 96, 120)
            ln1g = ppv(l, 120, 128); ln1b = ppv(l, 128, 136); ln2g = ppv(l, 136, 144); ln2b = ppv(l, 144, 152)
            dtb = ppv(l, 152, 184); alog = ppv(l, 184, 216); dsk = ppv(l, 216, 248); snk = ppv(l, 248, 264)
            gn = ppv(l, 264, 280)
            TR.top = 0
            aneg = T.get(F32, [32]); esink = T.get(F32, [16])
            act(aneg, alog, AF.Exp)
            ts(aneg, aneg, -1.0, None, ALU.mult)
            act(esink, snk, AF.Exp)
            base_top = TR.top

            qbf = T.get(BF16, [KC, TT]); kz = T.get(BF16, [2, 4, TT]); vcur = T.get(BF16, [NB, 512])
            memset(kz, 0.0)
            for (cbase, n, sbase) in ((0, 8, 8), (16, 4, 20)):
                for j0 in range(0, n, 4):
                    wa = wtile_fm(wfm_d[l], 0, KC, (cbase + j0) * 128, 512)
                    wb = wtile_fm(wfm_d[l], 0, KC, (sbase + j0) * 128, 512)
                    for jj in range(4):
                        pa = bank(); pb = bank()
                        for k in range(KC):
                            mm(pa, sub(wa, slice(None), k, slice(jj * 128, (jj + 1) * 128)), sub(hbf, slice(None), k), start=(k == 0), stop=(k == KC - 1))
                        for k in range(KC):
                            mm(pb, sub(wb, slice(None), k, slice(jj * 128, (jj + 1) * 128)), sub(hbf, slice(None), k), start=(k == 0), stop=(k == KC - 1))
                        t1 = T.get(F32, [TT]) if (cbase == 0 and j0 == 0 and jj == 0) else t1
                        t2 = T.get(F32, [TT]) if (cbase == 0 and j0 == 0 and jj == 0) else t2
                        tt(t1, pa, cosv, ALU.mult)
                        tt(t2, pb, sinv, ALU.mult)
                        j = j0 + jj
                        if cbase == 0:
                            tt(sub(qbf, slice(None), j), t1, t2, ALU.add)
                        else:
                            for half in range(2):
                                pr = slice(half * 64, (half + 1) * 64)
                                tt(View(kz.ap[pr, half, j, :], kz.key, kz.lo, kz.hi), View(t1.ap[pr, :], t1.key, t1.lo, t1.hi),
                                   View(t2.ap[pr, :], t2.key, t2.lo, t2.hi), ALU.add)
            wv = wtile_fm(wtm_d[l], 0, KC, 0, 512)
            for b in range(NB):
                ps = bank()
                for k in range(KC):
                    mm(ps, sub(hbf, slice(None), k, slice(b * 128, (b + 1) * 128)), sub(wv, slice(None), k), start=(k == 0), stop=(k == KC - 1))
                cp(sub(vcur, slice(None), b), ps)
            att_top = TR.top
            for b in range(NB):
                qs = slice(b * 128, (b + 1) * 128)
                for g in range(4):
                    TR.top = att_top + (g % 2) * 8192
                    pss = []
                    for kb in range(2):
                        ps = bank()
                        mm(ps, identb, mcur if kb == 1 else mprev, start=True, stop=False)
                        for slot in range(4):
                            cc, half = slot // 2, slot % 2
                            if kb == 1:
                                kv = sub(kz, slice(None), half, g, qs)
                            elif b > 0:
                                kv = sub(kz, slice(None), half, g, slice((b - 1) * 128, b * 128))
                            else:
                                kv = sub(kprev[l], slice(None), half, g)
                            mm(sub(ps, slice(None), slice(slot * 128, (slot + 1) * 128)), kv, sub(qbf, slice(None), 2 * g + cc, qs), start=False, stop=(slot == 3))
                        pss.append(ps)
                    pts = []
                    for kb in range(2):
                        pt = T.get(BF16, [512])
                        kbias = None
                        if s < 2:
                            if kb == 1:
                                col = s * 4 + b
                            elif b > 0:
                                col = s * 4 + b - 1
                            else:
                                col = 3 if s == 1 else None
                            kbias = kball if col is None else sub(vkb, slice(None), slice(col, col + 1))
                        act(pt, pss[kb], AF.Exp, bias=kbias, scale=0.125)
                        pts.append(pt)
                    pn = bank(); pd = bank()
                    for kb in range(2):
                        if kb == 1:
                            vv = sub(vcur, slice(None), b, slice(g * 128, (g + 1) * 128))
                        elif b > 0:
                            vv = sub(vcur, slice(None), b - 1, slice(g * 128, (g + 1) * 128))
                        else:
                            vv = sub(vprev[l], slice(None), slice(g * 128, (g + 1) * 128))
                        mm(pn, vv, pts[kb], start=(kb == 0), stop=(kb == 1))
                    for kb in range(2):
                        mm(pd, onesb, pts[kb], start=(kb == 0), stop=(kb == 1))
                    rden = T.get(F32, [512])
                    for slot in range(4):
                        cc, half = slot // 2, slot % 2
                        h = 4 * g + 2 * cc + half
                        sl = slice(slot * 128, (slot + 1) * 128)
                        ts(sub(rden, slice(None), sl), sub(pd, slice(None), sl), sub(esink, slice(None), slice(h, h + 1)), None, ALU.add)
                    recip(rden, rden)
                    for slot in range(4):
                        cc, half = slot // 2, slot % 2
                        pr = slice(half * 64, (half + 1) * 64)
                        sl = slice(slot * 128, (slot + 1) * 128)
                        tt(View(attnT.ap[pr, 2 * g + cc, qs], attnT.key, attnT.lo, attnT.hi),
                           View(pn.ap[pr, sl], pn.key, pn.lo, pn.hi), View(rden.ap[pr, sl], rden.key, rden.lo, rden.hi), ALU.mult)
            cp(kprev[l], sub(kz, slice(None), slice(None), slice(None), slice(TT - 128, TT)), eng="dve")
            cp(vprev[l], sub(vcur, slice(None), NB - 1), eng="dve")

            TR.top = base_top
            state = T.get(F32, [4, 512])
            dma("sp", state.ap, st_d[l].rearrange("p (g n) -> p g n", g=4), f"sl{l}", rd=[stv[l]], wr=[state])
            dtt = T.get(F32, [NB, 32]); dat = T.get(F32, [NB, 32]); expA = T.get(F32, [NB, 32]); cdec = T.get(F32, [NB, 32])
            wdt = wtile_fm(wtm_d[l], 0, KC, 512 + 2048, 32)
            for b in range(NB):
                ps = bank()
                for k in range(KC):
                    mm(sub(ps, slice(None), slice(0, 32)), sub(hbf, slice(None), k, slice(b * 128, (b + 1) * 128)), sub(wdt, slice(None), k), start=(k == 0), stop=(k == KC - 1))
                tt(sub(dtt, slice(None), b), sub(ps, slice(None), slice(0, 32)), dtb, ALU.add)
            act(dtt, dtt, AF.Exp)
            act(dtt, dtt, AF.Ln, bias=1.0)
            for b in range(NB):
                tt(sub(dat, slice(None), b), sub(dtt, slice(None), b), aneg, ALU.mult)
                ps = bank()
                mm(sub(ps, slice(None), slice(0, 32)), tri, sub(dat, slice(None), b))
                mm(sub(ps, slice(None), slice(32, 64)), onesf, sub(dat, slice(None), b))
                act(sub(expA, slice(None), b), sub(ps, slice(None), slice(0, 32)), AF.Exp)
                act(sub(cdec, slice(None), b), sub(ps, slice(None), slice(32, 64)), AF.Exp)
            ssd_top = TR.top
            for g in range(4):
                TR.top = ssd_top
                xg = T.get(BF16, [6, TT])
                xpre = [T.get(F32, [TT + 3]) for _ in range(2)]
                cacc = [T.get(F32, [TT]) for _ in range(2)]
                chunks = [4 * g + i for i in range(4)] + [16 + g, 20 + g]
                wx1 = wtile_fm(wfm_d[l], 0, KC, 3072 + chunks[0] * 128, 512)
                wx2 = wtile_fm(wfm_d[l], 0, KC, 3072 + chunks[4] * 128, 128)
                wx3 = wtile_fm(wfm_d[l], 0, KC, 3072 + chunks[5] * 128, 128)
                for i, c in enumerate(chunks):
                    wt = wx1 if i < 4 else (wx2 if i == 4 else wx3)
                    co = (i * 128) if i < 4 else 0
                    ps = bank()
                    for k in range(KC):
                        mm(ps, sub(wt, slice(None), k, slice(co, co + 128)), sub(hbf, slice(None), k), start=(k == 0), stop=(k == KC - 1))
                    xp = xpre[i % 2]; ca = cacc[i % 2]
                    cp(sub(xp, slice(None), slice(0, 3)), sub(tails[l], slice(None), c), eng="dve")
                    cp(sub(xp, slice(None), slice(3, TT + 3)), ps, eng="act")
                    if s < 2:
                        tt(sub(xp, slice(None), slice(3, TT + 3)), sub(xp, slice(None), slice(3, TT + 3)), vfm[s], ALU.mult)
                    cp(sub(tails[l], slice(None), c), sub(xp, slice(None), slice(TT, TT + 3)), eng="dve")
                    ts(ca, sub(xp, slice(None), slice(0, TT)), sub(cw, slice(None), c, slice(0, 1)), sub(cb, slice(None), slice(c, c + 1)), ALU.mult, ALU.add)
                    for k in range(1, 4):
                        stt(ca, sub(xp, slice(None), slice(k, TT + k)), sub(cw, slice(None), c, slice(k, k + 1)), ca, ALU.mult, ALU.add)
                    act(sub(xg, slice(None), i), ca, AF.Silu)
                    if s < 2 and i < 4:
                        tt(sub(xg, slice(None), i), sub(xg, slice(None), i), vfm[s], ALU.mult)
                wz = wtile_fm(wtm_d[l], 0, KC, 512 + g * 512, 512)
                blk_top = TR.top
                for b in range(NB):
                    TR.top = blk_top + (b % 2) * 24 * 1024
                    qs = slice(b * 128, (b + 1) * 128)
                    pt = bank(BF16)
                    for i in range(4):
                        tp(sub(pt, slice(None), slice(i * 128, (i + 1) * 128)), sub(xg, slice(None), i, qs), identb)
                    tp(sub(pt, slice(None), slice(512, 640)), sub(xg, slice(None), 4, qs), identb)
                    xs_t = T.get(BF16, [8, 64]); xc = T.get(BF16, [8, 64]); xd = T.get(BF16, [8, 64]); btok = T.get(BF16, [128])
                    ptx = View(pt.ap[:, 0:512].rearrange("p (h d) -> p h d", d=64), pt.key, pt.lo, pt.hi)
                    cp(xs_t, ptx, eng="act")
                    cp(btok, sub(pt, slice(None), slice(512, 640)), eng="act")
                    rhsD = T.get(F32, [8, 128]); E = T.get(F32, [8, 128])
                    dag = View(dat.ap[:, b, g * 8:(g + 1) * 8], dat.key, dat.lo, dat.hi)
                    tt(rhsD, View(tri.ap.unsqueeze(1).to_broadcast([128, 8, 128]), tri.key, tri.lo, tri.hi),
                       View(dag.ap.unsqueeze(2).to_broadcast([128, 8, 128]), dag.key, dag.lo, dag.hi), ALU.mult)
                    pseg = [bank(), bank()]
                    for hf in range(2):
                        mm(pseg[hf], ustr, View(rhsD.ap[:, hf * 4:(hf + 1) * 4, :].rearrange("p h l -> p (h l)"), rhsD.key, rhsD.lo, rhsD.hi))
                    for hf in range(2):
                        act(View(E.ap[:, hf * 4:(hf + 1) * 4, :].rearrange("p h l -> p (h l)"), E.key, E.lo, E.hi), pseg[hf], AF.Exp)
                    dtg = View(dtt.ap[:, b, g * 8:(g + 1) * 8], dtt.key, dtt.lo, dtt.hi)
                    w1 = T.get(F32, [8])
                    tt(w1, dtg, View(E.ap[:, :, 127], E.key, E.lo, E.hi), ALU.mult)
                    tt(xc, xs_t, View(dtg.ap.unsqueeze(2).to_broadcast([128, 8, 64]), dtg.key, dtg.lo, dtg.hi), ALU.mult)
                    tt(xd, xs_t, View(w1.ap.unsqueeze(2).to_broadcast([128, 8, 64]), w1.key, w1.lo, w1.hi), ALU.mult)
                    pcb = bank()
                    mm(sub(pcb, slice(None), slice(0, 128)), sub(xg, slice(None), 4, qs), sub(xg, slice(None), 5, qs))
                    mcb = T.get(F32, [128])
                    tt(mcb, sub(pcb, slice(None), slice(0, 128)), tri, ALU.mult)
                    MT = T.get(BF16, [8, 128])
                    tt(MT, E, View(mcb.ap.unsqueeze(1).to_broadcast([128, 8, 128]), mcb.key, mcb.lo, mcb.hi), ALU.mult)
                    py = bank(); po = bank()
                    for hh in range(8):
                        mm(sub(py, slice(None), slice(hh * 64, (hh + 1) * 64)), sub(MT, slice(None), hh), sub(xc, slice(None), hh))
                    sbf = T.get(BF16, [512])
                    cp(sbf, sub(state, slice(None), g), eng="dve")
                    mm(po, sub(xg, slice(None), 5, qs), sbf)
                    pst = bank()
                    mm(pst, btok, View(xd.ap.rearrange("p h d -> p (h d)"), xd.key, xd.lo, xd.hi))
                    cdg = View(cdec.ap[:, b, g * 8:(g + 1) * 8], cdec.key, cdec.lo, cdec.hi)
                    sg3 = View(state.ap[:, g, :].rearrange("p (h d) -> p h d", d=64), state.key, state.lo, state.hi)
                    tt(sg3, sg3, View(cdg.ap.unsqueeze(2).to_broadcast([128, 8, 64]), cdg.key, cdg.lo, cdg.hi), ALU.mult)
                    tt(sub(state, slice(None), g), sub(state, slice(None), g), pst, ALU.add)
                    yv = T.get(F32, [8, 64])
                    eag = View(expA.ap[:, b, g * 8:(g + 1) * 8], expA.key, expA.lo, expA.hi)
                    po3 = View(po.ap.rearrange("p (h d) -> p h d", d=64), po.key, po.lo, po.hi)
                    py3 = View(py.ap.rearrange("p (h d) -> p h d", d=64), py.key, py.lo, py.hi)
                    tt(yv, po3, View(eag.ap.unsqueeze(2).to_broadcast([128, 8, 64]), eag.key, eag.lo, eag.hi), ALU.mult)
                    tt(yv, yv, py3, ALU.add)
                    dsg = View(dsk.ap[:, g * 8:(g + 1) * 8], dsk.key, dsk.lo, dsk.hi)
                    xsd = T.get(F32, [8, 64])
                    tt(xsd, xs_t, View(dsg.ap.unsqueeze(2).to_broadcast([128, 8, 64]), dsg.key, dsg.lo, dsg.hi), ALU.mult)
                    tt(yv, yv, xsd, ALU.add)
                    pz = bank()
                    for k in range(KC):
                        mm(pz, sub(hbf, slice(None), k, qs), sub(wz, slice(None), k), start=(k == 0), stop=(k == KC - 1))
                    sz = T.get(F32, [512])
                    act(sz, pz, AF.Silu)
                    y2 = View(yv.ap.rearrange("p h d -> p (h d)"), yv.key, yv.lo, yv.hi)
                    tt(y2, y2, sz, ALU.mult)
                    ss = T.get(F32, [1])
                    act(sz, y2, AF.Square, accum=ss)
                    ts(ss, ss, 1.0 / 512, RMS_EPS, ALU.mult, ALU.add)
                    act(ss, ss, AF.Sqrt)
                    recip(ss, ss)
                    ub = T.get(BF16, [512])
                    ts(ub, y2, ss, None, ALU.mult)
                    pu = bank(BF16)
                    for i in range(4):
                        tp(sub(pu, slice(None), slice(i * 128, (i + 1) * 128)), sub(ub, slice(None), slice(i * 128, (i + 1) * 128)), identb)
                    for i in range(4):
                        act(sub(unT, slice(None), 4 * g + i, qs), sub(pu, slice(None), slice(i * 128, (i + 1) * 128)), AF.Identity,
                            scale=sub(gn, slice(None), slice(4 * g + i, 4 * g + i + 1)))
            dma("sp", st_d[l].rearrange("p (g n) -> p g n", g=4), state.ap, f"ss{l}", rd=[state], wr=[stv[l]])

            TR.top = base_top
            mT = T.get(BF16, [KC, TT])
            for c in range(KC):
                wa = wtile_fm(wao_d[l], 0, KC, c * 128, 128)
                ws = wtile_fm(wso_d[l], 0, 16, c * 128, 128)
                wg2 = wtile_fm(wfm_d[l], 0, KC, 6144 + c * 128, 128)
                wg3 = wtile_fm(wfm_d[l], 0, KC, 6144 + 1024 + c * 128, 128)
                pa = bank(); psd = bank(); pga = bank(); pgs = bank()
                for k in range(KC):
                    mm(pa, sub(wa, slice(None), k), sub(attnT, slice(None), k), start=(k == 0), stop=(k == KC - 1))
                for k in range(16):
                    mm(psd, sub(ws, slice(None), k), sub(unT, slice(None), k), start=(k == 0), stop=(k == 15))
                for k in range(KC):
                    mm(pga, sub(wg2, slice(None), k), sub(hbf, slice(None), k), start=(k == 0), stop=(k == KC - 1))
                for k in range(KC):
                    mm(pgs, sub(wg3, slice(None), k), sub(hbf, slice(None), k), start=(k == 0), stop=(k == KC - 1))
                if c == 0:
                    sa = T.get(F32, [TT]); sb_ = T.get(F32, [TT])
                act(sa, pga, AF.Sigmoid); act(sb_, pgs, AF.Sigmoid)
                tt(sa, sa, pa, ALU.mult); tt(sb_, sb_, psd, ALU.mult)
                tt(sub(mT, slice(None), c), sa, sb_, ALU.add)
            for c in range(KC):
                wo = wtile_fm(wo_d[l], 0, KC, c * 128, 128)
                po = bank()
                for k in range(KC):
                    mm(po, sub(wo, slice(None), k), sub(mT, slice(None), k), start=(k == 0), stop=(k == KC - 1))
                stt(sub(hres, slice(None), c), sub(hres, slice(None), c), ALPHA, po, ALU.mult, ALU.add)
            ln_fm(hres, ln1g, ln1b, TR.top)

            TR.top = base_top
            if kind == "dense":
                NF = DFF // 128
                actb = T.get(BF16, [NF, TT])
                tu = T.get(F32, [TT])
                for j0 in range(0, NF, 4):
                    ng = min(4, NF - j0)
                    wgt = wtile_fm(wg_d[l][0], 0, KC, j0 * 128, ng * 128)
                    wut = wtile_fm(wu_d[l][0], 0, KC, j0 * 128, ng * 128)
                    for jj in range(ng):
                        pg = bank(); pu = bank()
                        for k in range(KC):
                            mm(pg, sub(wgt, slice(None), k, slice(jj * 128, (jj + 1) * 128)), sub(hbf, slice(None), k), start=(k == 0), stop=(k == KC - 1))
                        for k in range(KC):
                            mm(pu, sub(wut, slice(None), k, slice(jj * 128, (jj + 1) * 128)), sub(hbf, slice(None), k), start=(k == 0), stop=(k == KC - 1))
                        act(tu, pg, AF.Silu)
                        tt(sub(actb, slice(None), j0 + jj), tu, pu, ALU.mult)
                for c in range(KC):
                    wds = [wtile_fm(wd_d[l][0], k0, min(11, NF - k0), c * 128, 128) for k0 in range(0, NF, 11)]
                    po = bank()
                    for k in range(NF):
                        mm(po, sub(wds[k // 11], slice(None), k % 11), sub(actb, slice(None), k), start=(k == 0), stop=(k == NF - 1))
                    stt(sub(hres, slice(None), c), sub(hres, slice(None), c), ALPHA, po, ALU.mult, ALU.add)
            else:
                NF = DFFE // 128
                actb = T.get(BF16, [NF, TT]); tu = T.get(F32, [TT])
                gateb = T.get(F32, [NE, TT])
                wrt = T.get(F32, [KC, NE])
                dma("sp", wrt.ap, wr_d[l].rearrange("(c p) e -> p c e", p=128), "wr", wr=[wrt])
                for b in range(NB):
                    qs = slice(b * 128, (b + 1) * 128)
                    ps = bank()
                    for k in range(KC):
                        mm(sub(ps, slice(None), slice(0, NE)), sub(hres, slice(None), k, qs), sub(wrt, slice(None), k), start=(k == 0), stop=(k == KC - 1))
                    lg = T.get(F32, [NE]); m1 = T.get(F32, [1]); m2 = T.get(F32, [1]); msk = T.get(F32, [NE]); gt = T.get(F32, [NE]); dg = T.get(F32, [128])
                    cp(lg, sub(ps, slice(None), slice(0, NE)), eng="dve")
                    red(m1, lg, ALU.max)
                    ts(msk, lg, m1, NEG, ALU.is_ge, ALU.mult)
                    tt(msk, msk, lg, ALU.add)
                    red(m2, msk, ALU.max)
                    ts(gt, lg, m1, None, ALU.subtract)
                    act(gt, gt, AF.Exp)
                    ts(msk, lg, m2, None, ALU.is_ge)
                    tt(gt, gt, msk, ALU.mult)
                    red(m1, gt, ALU.add)
                    recip(m1, m1)
                    ts(gt, gt, m1, None, ALU.mult)
                    for e_ in range(NE):
                        ts(dg, identf, sub(gt, slice(None), slice(e_, e_ + 1)), None, ALU.mult)
                        pb_ = bank()
                        mm(sub(pb_, slice(None), slice(0, 128)), onesf, dg)
                        cp(sub(gateb, slice(None), e_, qs), sub(pb_, slice(None), slice(0, 128)))
                ts(hres, hres, ALPHA, None, ALU.mult)
                for e_ in range(NE):
                    for j0 in range(0, NF, 4):
                        ng = min(4, NF - j0)
                        wgt = wtile_fm(wg_d[l][e_], 0, KC, j0 * 128, ng * 128)
                        wut = wtile_fm(wu_d[l][e_], 0, KC, j0 * 128, ng * 128)
                        for jj in range(ng):
                            pg = bank(); pu = bank()
                            for k in range(KC):
                                mm(pg, sub(wgt, slice(None), k, slice(jj * 128, (jj + 1) * 128)), sub(hbf, slice(None), k), start=(k == 0), stop=(k == KC - 1))
                            for k in range(KC):
                                mm(pu, sub(wut, slice(None), k, slice(jj * 128, (jj + 1) * 128)), sub(hbf, slice(None), k), start=(k == 0), stop=(k == KC - 1))
                            act(tu, pg, AF.Silu)
                            tt(sub(actb, slice(None), j0 + jj), tu, pu, ALU.mult)
                    for c in range(KC):
                        wds = [wtile_fm(wd_d[l][e_], k0, min(14, NF - k0), c * 128, 128) for k0 in range(0, NF, 14)]
                        po = bank()
                        for k in range(NF):
                            mm(po, sub(wds[k // 14], slice(None), k % 14), sub(actb, slice(None), k), start=(k == 0), stop=(k == NF - 1))
                        tt(tu, po, sub(gateb, slice(None), e_), ALU.mult)
                        tt(sub(hres, slice(None), c), sub(hres, slice(None), c), tu, ALU.add)
            ln_fm(hres, ln2g, ln2b, TR.top)

        TR.top = 0
        for b in range(NB):
            ot = T.get(F32, [D])
            for c0 in range(0, KC, 4):
                ps = bank()
                for c in range(4):
                    tp(sub(ps, slice(None), slice(c * 128, (c + 1) * 128)), sub(hres, slice(None), c0 + c, slice(b * 128, (b + 1) * 128)), identf)
                cp(sub(ot, slice(None), slice(c0 * 128, (c0 + 4) * 128)), ps)
            o = dma("sp", y_d[s, b], ot.ap, f"o{b}", rd=[ot], wr=[])
            out_sems.append(o)
            if s < NSTEP - 1:
                dma("sp", send_d[b * 128:(b + 1) * 128, :], ot.ap, f"sd{b}", rd=[ot], wr=[sendv])
        if s < NSTEP - 1:
            cco = P.op("pool", lambda e: e.collective_compute("AllGather", ALU.bypass, replica_groups=[[0, 1], [2, 3], [4, 5], [6, 7]],
                                                           ins=[send_d.opt()], outs=[gath_d.opt()]), reads=[sendv], writes=[gathv])
            cco.noval = True

    def final_waits():
        last = {}
        for o in out_sems:
            last[id(o.sem)] = (o.sem, o.semval)
        return list(last.values())
    P.emit(final_waits)
    return nc, es


def _consts():
    c = np.zeros((128, 6 * 128 + 1024 + 2), np.float32)
    i = np.arange(128)
    c[:, 0:128] = np.eye(128)
    c[:, 128:256] = (i[:, None] <= i[None, :])
    c[:, 256:384] = (i[:, None] > i[None, :])
    c[:, 384:512] = 1.0 / D
    c[:, 512:640] = 1.0
    mprev = np.where(i[:, None] > i[None, :], 0.0, NEG)
    mcur = np.where(i[:, None] <= i[None, :], 0.0, NEG)
    c[:, 768:768 + 512] = np.tile(mprev, (1, 4))
    c[:, 768 + 512:768 + 1024] = np.tile(mcur, (1, 4))
    c[:, 768 + 1024] = np.where(i < PAD, NEG, 0.0)
    c[:, 768 + 1025] = NEG
    return c


def _cossin(nstep):
    T_ = nstep * TT
    pos = (np.arange(T_) - PAD).astype(np.float32)
    inv = (np.float32(10000.0) ** (-np.arange(0, 64, 2, dtype=np.float32) / np.float32(64))).astype(np.float32)
    ang = (pos[:, None] * inv[None, :]).astype(np.float32)
    cos = np.cos(ang).astype(np.float32); sin = np.sin(ang).astype(np.float32)
    p = np.arange(128)
    f = p % 32
    sign = np.where((p % 64) < 32, -1.0, 1.0).astype(np.float32)
    cosT = cos[:, f].T
    sinT = sin[:, f].T * sign[:, None]
    out = np.zeros((nstep, 2, 128, TT), np.float32)
    for s in range(nstep):
        out[s, 0] = cosT[:, s * TT:(s + 1) * TT]
        out[s, 1] = sinT[:, s * TT:(s + 1) * TT]
    return out


def _fm(v, n):
    return np.ascontiguousarray(v.reshape(n, 128).T)


def _layer_inputs(inp, l, li, kind, m):
    w_in = inp["w_in"][l]
    q = w_in[:, 2048:3072]; k = w_in[:, 3072:3328]; v = w_in[:, 3328:3584]
    z = w_in[:, 3584:5632]; xbc = w_in[:, 5632:8704]; dtw = w_in[:, 8704:8736]
    ga = w_in[:, 0:1024]; gs = w_in[:, 1024:2048]
    sw = (np.arange(64) + 32) % 64
    qs = q.reshape(D, 16, 64)[:, :, sw].reshape(D, 1024)
    ks = k.reshape(D, 4, 64)[:, :, sw].reshape(D, 256)
    dup = lambda a: np.repeat(a.reshape(D, 4, 1, 64), 2, axis=2).reshape(D, 512)
    m[f"wfm{li}"] = np.ascontiguousarray(np.concatenate([q, qs, dup(k), dup(ks), xbc, ga, gs], axis=1))
    m[f"wtm{li}"] = np.ascontiguousarray(np.concatenate([dup(v), z, dtw], axis=1))
    m[f"wao{li}"] = inp["w_attn_out"][l]; m[f"wso{li}"] = inp["w_ssd_out"][l]; m[f"wo{li}"] = inp["w_o"][l]
    if kind == "dense":
        m[f"wg{li}"] = inp["ffn_wg"][l // 2][None]; m[f"wu{li}"] = inp["ffn_wu"][l // 2][None]; m[f"wd{li}"] = inp["ffn_wd"][l // 2][None]
    else:
        m[f"wg{li}"] = inp["moe_wg"][l // 2]; m[f"wu{li}"] = inp["moe_wu"][l // 2]; m[f"wd{li}"] = inp["moe_wd"][l // 2]
        m[f"wr{li}"] = inp["moe_router"][l // 2]
    pp = np.zeros((128, NPP), np.float32)
    pp[:, 0:96] = np.transpose(inp["conv_w"][l].reshape(4, 24, 128), (2, 1, 0)).reshape(128, 96)
    pp[:, 96:120] = _fm(inp["conv_b"][l], 24)
    pp[:, 120:128] = _fm(inp["ln1_g"][l], 8); pp[:, 128:136] = _fm(inp["ln1_b"][l], 8)
    pp[:, 136:144] = _fm(inp["ln2_g"][l], 8); pp[:, 144:152] = _fm(inp["ln2_b"][l], 8)
    pp[:, 152:184] = inp["dt_bias"][l][None, :]; pp[:, 184:216] = inp["a_log"][l][None, :]
    pp[:, 216:248] = inp["d_skip"][l][None, :]; pp[:, 248:264] = inp["sinks"][l][None, :]
    pp[:, 264:280] = _fm(inp["ssd_norm_g"][l], 16)
    return pp


def _masks(valid2):
    vfm = np.ascontiguousarray(np.broadcast_to(valid2[:, None, :], (2, 128, TT))).astype(np.float32)
    vkb = np.ascontiguousarray(((valid2.reshape(2, NB, 128) - 1.0) * (-NEG)).transpose(2, 0, 1).reshape(128, 8)).astype(np.float32)
    return vfm, vkb


def make_inputs(inp, stages, nstep, seqs):
    nst = len(stages)
    stage_shared = []
    kinds = None
    for layer_ids in stages:
        kinds = ["dense" if l % 2 == 0 else "moe" for l in layer_ids]
        sh = {}
        pps = [_layer_inputs(inp, l, li, kinds[li], sh) for li, l in enumerate(layer_ids)]
        sh["pp"] = np.ascontiguousarray(np.concatenate(pps, axis=1))
        sh["cst"] = _consts()
        sh["cossin"] = None
        stage_shared.append(sh)
    cs = _cossin(nstep)
    lnin_real = np.ascontiguousarray(np.concatenate([_fm(inp["ln_in_g"], 8), _fm(inp["ln_in_b"], 8)], axis=1))
    lnin_id = np.ascontiguousarray(np.concatenate([np.ones((128, 8), np.float32), np.zeros((128, 8), np.float32)], axis=1))
    vpad = np.ones(TT, np.float32); vpad[:PAD] = 0.0
    maps = []
    Tn = nstep * TT
    for b in seqs:
        for si in range(nst):
            m = dict(stage_shared[si])
            xs = np.zeros((Tn, D), np.float32)
            valid2 = np.ones((2, TT), np.float32)
            if si == 0:
                xs[PAD:PAD + NMETA] = inp["meta_tokens"]
                n = min(Tn - 128 - (nst - 1) * TT, inp["x"].shape[1])
                xs[128:128 + n] = inp["x"][b, :n]
                valid2[0] = vpad
                m["lnin"] = lnin_real; m["fb"] = np.zeros((128, 1), np.float32)
                m["cossin"] = cs
            else:
                valid2[0] = 0.0; valid2[1] = vpad
                m["lnin"] = lnin_id; m["fb"] = np.ones((128, 1), np.float32)
                cs1 = np.zeros_like(cs); cs1[1:] = cs[:-1]; cs1[0] = cs[0]
                m["cossin"] = cs1
            m["vfm"], m["vkb"] = _masks(valid2)
            m["x"] = xs.reshape(nstep, NB, 128, D)
            maps.append(m)
    return maps, kinds


NTILE_FULL = 17
STAGES = [[0, 1], [2, 3]]


def kernel(**inputs):
    inp = {k: np.asarray(v) for k, v in inputs.items()}
    nstep = NTILE_FULL + len(STAGES) - 1
    maps, kinds = make_inputs(inp, STAGES, nstep, list(range(4)))
    nc, es = build(dict(layers=kinds, nstep=nstep))
    with es:
        res = run_bass_kernel_spmd(nc, maps, core_ids=list(range(8)))
    S = inp["x"].shape[1]
    outs = []
    for b in range(4):
        y = res.results[2 * b + 1]["y"].reshape(nstep, TT, D)[1:].reshape(NTILE_FULL * TT, D)
        outs.append(y[128:128 + S])
    return np.stack(outs, axis=0).astype(np.float32)
```

```python
import numpy as np
from contextlib import ExitStack
import concourse.bass as bass
import concourse.mybir as mybir
from concourse.bass_utils import run_bass_kernel_spmd

F32 = mybir.dt.float32
BF16 = mybir.dt.bfloat16
AF = mybir.ActivationFunctionType
ALU = mybir.AluOpType
AX = mybir.AxisListType

D = 1024; KC = 8; TT = 512; NB = 4; BLK = 128
DEPTH = 4; PAD = 112; NMETA = 16
DFF = 2816; DFFE = 3584; NE = 8
ALPHA = (2 * DEPTH) ** 0.25
LN_EPS = 1e-5; RMS_EPS = 1e-5
NEG = -30000.0
PAGE = 512
NPP = 288


class View:
    def __init__(self, ap, key, lo, hi, shape=None, esz=None):
        self.ap = ap; self.key = key; self.lo = lo; self.hi = hi; self.shape = shape; self.esz = esz

    def pages(self):
        if self.key is None:
            return []
        return [(self.key, p) for p in range(self.lo // PAGE, (self.hi - 1) // PAGE + 1)]


class Op:
    __slots__ = ("eng", "fn", "deps", "idx", "inc", "is_dma", "sem", "semval", "has_dep", "epoch", "noval")

    def __init__(self, eng, fn):
        self.eng = eng; self.fn = fn; self.deps = []; self.inc = None
        self.is_dma = False; self.sem = None; self.semval = 0; self.has_dep = False; self.noval = False


class Prog:
    ENGS = ("pe", "act", "dve", "pool", "sp")

    def __init__(self, nc, es):
        self.nc = nc; self.es = es
        self.q = {e: [] for e in self.ENGS}
        self.pw = {}; self.pr = {}
        self.dsem = {}; self.dcnt = {}
        self.toggle = 0
        self.const_keys = set()
        self.epoch = 0

    def op(self, eng, fn, reads=(), writes=(), dma_sem=None):
        o = Op(eng, fn); o.epoch = self.epoch
        deps = {}
        def add(p):
            if p is None or p is o:
                return
            deps[id(p)] = p
        for v in reads:
            for pg in v.pages():
                add(self.pw.get(pg))
        for v in writes:
            for pg in v.pages():
                add(self.pw.get(pg))
                for r in self.pr.get(pg, ()):
                    add(r)
        for v in writes:
            for pg in v.pages():
                self.pw[pg] = o; self.pr[pg] = []
        for v in reads:
            if v.key in self.const_keys:
                continue
            for pg in v.pages():
                self.pr.setdefault(pg, []).append(o)
        best = {}
        for p in deps.values():
            if p.is_dma:
                best[("d", id(p))] = p
            else:
                if p.eng == o.eng and (o.eng == "pe"):
                    continue
                b = best.get(p.eng)
                if b is None or p.idx > b.idx:
                    best[p.eng] = p
        o.deps = list(best.values())
        for p in o.deps:
            p.has_dep = True
        if dma_sem is not None:
            o.is_dma = True
            if dma_sem[0] == "w":
                dma_sem = dma_sem + "_%d" % self.epoch
            if dma_sem not in self.dsem:
                self.dsem[dma_sem] = self.es.enter_context(self.nc.semaphore("d_" + dma_sem))
                self.dcnt[dma_sem] = 0
            self.dcnt[dma_sem] += 16
            o.sem = self.dsem[dma_sem]; o.semval = self.dcnt[dma_sem]
        o.idx = len(self.q[eng])
        self.q[eng].append(o)
        return o

    def emit(self, final_waits):
        nc = self.nc
        esem = {}
        for e in self.ENGS:
            c = {}
            for o in self.q[e]:
                if o.is_dma:
                    continue
                if o.has_dep:
                    c[o.epoch] = c.get(o.epoch, 0) + 1; o.inc = c[o.epoch]
                    if (e, o.epoch) not in esem:
                        esem[(e, o.epoch)] = self.es.enter_context(nc.semaphore("e_%s_%d" % (e, o.epoch)))
        def run(e, eng):
            waited = {}
            for o in self.q[e]:
                for p in o.deps:
                    if p.is_dma:
                        s, v = p.sem, p.semval
                    else:
                        s, v = esem[(p.eng, p.epoch)], p.inc
                    k = id(s)
                    if waited.get(k, 0) >= v:
                        continue
                    waited[k] = v
                    eng.wait_ge(s, v)
                ins = o.fn(eng)
                if o.is_dma:
                    ins.then_inc(o.sem, 16)
                elif o.inc is not None:
                    if o.noval:
                        ins.then_inc(esem[(e, o.epoch)])
                    else:
                        ins.then_inc(esem[(e, o.epoch)], 1)
            if e == "sp":
                for (s, v) in final_waits():
                    eng.wait_ge(s, v)
        with nc.Block() as block:
            @block.tensor
            def _(eng): run("pe", eng)
            @block.scalar
            def _(eng): run("act", eng)
            @block.vector
            def _(eng): run("dve", eng)
            @block.gpsimd
            def _(eng): run("pool", eng)
            @block.sync
            def _(eng): run("sp", eng)


class Arena:
    def __init__(self, nc, es, name, nbytes, dt=F32):
        self.t = es.enter_context(nc.sbuf_tensor(name, [128, nbytes // 4], F32))
        self.key = name; self.nbytes = nbytes; self.top = 0

    def alloc(self, nbytes):
        o = self.top; self.top += (nbytes + 63) // 64 * 64
        assert self.top <= self.nbytes, (self.key, self.top, self.nbytes)
        return o

    def view(self, off, dt, shape, part=(0, 128)):
        esz = 4 if dt == F32 else 2
        n = int(np.prod(shape))
        ap = self.t[part[0]:part[1], off // 4:(off + n * esz) // 4]
        if dt != F32:
            ap = ap.bitcast(dt)
        if len(shape) == 2:
            ap = ap.rearrange("p (a b) -> p a b", a=shape[0])
        elif len(shape) == 3:
            ap = ap.rearrange("p (a b c) -> p a b c", a=shape[0], b=shape[1])
        return View(ap, self.key, off, off + n * esz, shape=list(shape), esz=esz)


def interleave(gens, width):
    gens = list(gens); active = []
    while gens or active:
        while len(active) < width and gens:
            active.append(gens.pop(0))
        for g_ in list(active):
            try:
                next(g_)
            except StopIteration:
                active.remove(g_)


def build(cfg):
    layers = cfg["layers"]; NL = len(layers); NSTEP = cfg["nstep"]
    nc = bass.Bass("TRN2", target_bir_lowering=False)
    es = ExitStack()
    P = Prog(nc, es)

    def din(name, shape, dt=F32):
        return nc.dram_tensor(name, list(shape), dt, kind="ExternalInput").ap()

    x_d = din("x", [NSTEP, NB, 128, D])
    cs_d = din("cossin", [NSTEP, 2, 128, TT])
    cst_d = din("cst", [128, 6 * 128 + 2 * 512 + 2])
    fb_d = din("fb", [128, 1]); vfm_d = din("vfm", [2, 128, TT]); vkb_d = din("vkb", [128, 8])
    send_d = nc.dram_tensor("send", [TT, D], F32, kind="Internal").ap()
    gath_d = nc.dram_tensor("gath", [2 * TT, D], F32, kind="Internal").ap()
    lnin_d = din("lnin", [128, 16])
    pp_d = din("pp", [128, NL * NPP])
    wfm_d = [din(f"wfm{l}", [D, 3072 + 3072 + 2048]) for l in range(NL)]
    wtm_d = [din(f"wtm{l}", [D, 512 + 2048 + 32]) for l in range(NL)]
    wao_d = [din(f"wao{l}", [D, D]) for l in range(NL)]
    wso_d = [din(f"wso{l}", [2 * D, D]) for l in range(NL)]
    wo_d = [din(f"wo{l}", [D, D]) for l in range(NL)]
    wg_d = []; wu_d = []; wd_d = []; wr_d = []
    for l, kind in enumerate(layers):
        if kind == "dense":
            wg_d.append(din(f"wg{l}", [1, D, DFF])); wu_d.append(din(f"wu{l}", [1, D, DFF]))
            wd_d.append(din(f"wd{l}", [1, DFF, D])); wr_d.append(None)
        else:
            wg_d.append(din(f"wg{l}", [NE, D, DFFE])); wu_d.append(din(f"wu{l}", [NE, D, DFFE]))
            wd_d.append(din(f"wd{l}", [NE, DFFE, D])); wr_d.append(din(f"wr{l}", [D, NE]))
    y_d = nc.dram_tensor("y", [NSTEP, NB, 128, D], F32, kind="ExternalOutput").ap()
    st_d = [nc.dram_tensor(f"state{l}", [128, 2048], F32, kind="Internal").ap() for l in range(NL)]

    PER = Arena(nc, es, "per", 84 * 1024)
    WR = Arena(nc, es, "wring", 32 * 1024)
    TR = Arena(nc, es, "trans", 88 * 1024)
    NSLOT = 4
    wslots = [WR.alloc(8192) for _ in range(NSLOT)]
    hres = PER.view(PER.alloc(KC * TT * 4), F32, [KC, TT])
    hbf = PER.view(PER.alloc(KC * TT * 2), BF16, [KC, TT])
    cst = PER.view(PER.alloc((6 * 128 + 1024 + 2) * 4), F32, [6 * 128 + 1024 + 2])
    fbv = PER.view(PER.alloc(64), F32, [1]); vkb = PER.view(PER.alloc(64), F32, [8])
    vfm = [PER.view(PER.alloc(TT * 4), F32, [TT]) for _ in range(2)]
    cstb = PER.view(PER.alloc((2 * 128 + 1024) * 2), BF16, [2 * 128 + 1024])
    lnin = PER.view(PER.alloc(64), F32, [16])
    pp = PER.view(PER.alloc(NL * NPP * 4), F32, [NL * NPP])
    cosv = PER.view(PER.alloc(TT * 4), F32, [TT]); sinv = PER.view(PER.alloc(TT * 4), F32, [TT])
    tails = [PER.view(PER.alloc(24 * 3 * 4), F32, [24, 3]) for _ in range(NL)]
    kprev = [PER.view(PER.alloc(2 * 4 * 128 * 2), BF16, [2, 4, 128]) for _ in range(NL)]
    vprev = [PER.view(PER.alloc(512 * 2), BF16, [512]) for _ in range(NL)]
    attnT = PER.view(PER.alloc(KC * TT * 2), BF16, [KC, TT])
    unT = PER.view(PER.alloc(16 * TT * 2), BF16, [16, TT])
    P.const_keys.add("cstk")
    for v in (cst, cstb, lnin, pp, fbv, vkb, vfm[0], vfm[1]):
        v.key = "cstk"
    def csub(v, a, b):
        return View(v.ap[:, a:b], v.key, v.lo, v.hi)
    identf = csub(cst, 0, 128); tri = csub(cst, 128, 256); ustr = csub(cst, 256, 384)
    onesD = csub(cst, 384, 512); onesf = csub(cst, 512, 640); sel = csub(cst, 640, 768)
    kb0 = csub(cst, 768 + 1024, 768 + 1025); kball = csub(cst, 768 + 1025, 768 + 1026)
    identb = csub(cstb, 0, 128); onesb = csub(cstb, 128, 256)
    mprev = csub(cstb, 256, 768); mcur = csub(cstb, 768, 1280)

    psum = [es.enter_context(nc.psum_tensor(f"ps{i}", [128, 512], F32)) for i in range(8)]
    pstate = {"i": 0}
    def bank(dt=F32, shape=None):
        i = pstate["i"] % 8; pstate["i"] += 1
        ap = psum[i][:, :]
        n = 512
        if dt == BF16:
            ap = ap.bitcast(BF16); n = 1024
        v = View(ap, "psum", i * 2048, (i + 1) * 2048)
        return v
    def sub(v, *idx):
        lo, hi = v.lo, v.hi
        if v.shape is not None and v.key != "psum":
            strides = []
            st_ = 1
            for d_ in reversed(v.shape):
                strides.insert(0, st_); st_ *= d_
            mn = 0; mx = 0
            for i_, d_ in enumerate(v.shape):
                ix = idx[i_ + 1] if i_ + 1 < len(idx) else slice(None)
                if isinstance(ix, int):
                    a_, b_ = ix, ix + 1
                else:
                    a_ = 0 if ix.start is None else ix.start
                    b_ = d_ if ix.stop is None else ix.stop
                mn += a_ * strides[i_]; mx += (b_ - 1) * strides[i_]
            lo = v.lo + mn * v.esz; hi = v.lo + (mx + 1) * v.esz
        return View(v.ap[idx], v.key, lo, hi)

    class TAlloc:
        def __init__(self): self.base = 0
        def reset(self, to=0): TR.top = to
        def get(self, dt, shape):
            esz = 4 if dt == F32 else 2
            return TR.view(TR.alloc(int(np.prod(shape)) * esz), dt, shape)
    T = TAlloc()

    def mm(out, lhsT, rhs, start=True, stop=True):
        return P.op("pe", lambda e: e.matmul(out.ap, lhsT.ap, rhs.ap, start=start, stop=stop),
                    reads=[lhsT, rhs] + ([] if start else []), writes=[out])
    def tp(out, in_, ident):
        return P.op("pe", lambda e: e.transpose(out.ap, in_.ap, ident.ap), reads=[in_, ident], writes=[out])
    def act(out, in_, func, bias=None, scale=1.0, accum=None):
        rd = [in_]; kw = {}
        if isinstance(bias, View): rd.append(bias); kw["bias"] = bias.ap
        elif bias is not None: kw["bias"] = float(bias)
        if isinstance(scale, View): rd.append(scale); kw["scale"] = scale.ap
        else: kw["scale"] = float(scale)
        wr = [out]
        if accum is not None: wr.append(accum); kw["accum_out"] = accum.ap
        return P.op("act", lambda e: e.activation(out.ap, in_.ap, func, **kw), reads=rd, writes=wr)
    def tt(out, a, b, op, eng="dve"):
        return P.op(eng, lambda e: e.tensor_tensor(out.ap, a.ap, b.ap, op), reads=[a, b], writes=[out])
    def ts(out, a, s1, s2, op0, op1=None, eng="dve"):
        rd = [a]
        s1a = s1.ap if isinstance(s1, View) else float(s1)
        if isinstance(s1, View): rd.append(s1)
        s2a = None
        if s2 is not None:
            s2a = s2.ap if isinstance(s2, View) else float(s2)
            if isinstance(s2, View): rd.append(s2)
        if op1 is None:
            return P.op(eng, lambda e: e.tensor_scalar(out.ap, a.ap, s1a, None, op0), reads=rd, writes=[out])
        return P.op(eng, lambda e: e.tensor_scalar(out.ap, a.ap, s1a, s2a, op0, op1), reads=rd, writes=[out])
    def stt(out, a, s, b, op0, op1, eng="dve"):
        rd = [a, b]
        sa = s.ap if isinstance(s, View) else float(s)
        if isinstance(s, View): rd.append(s)
        return P.op(eng, lambda e: e.scalar_tensor_tensor(out.ap, a.ap, sa, b.ap, op0, op1), reads=rd, writes=[out])
    def cp(out, in_, eng=None):
        if eng is None:
            P.toggle ^= 1; eng = "act" if P.toggle else "dve"
        if eng == "act":
            return act(out, in_, AF.Copy)
        return P.op("dve", lambda e: e.tensor_copy(out.ap, in_.ap), reads=[in_], writes=[out])
    def recip(out, in_):
        return P.op("dve", lambda e: e.reciprocal(out.ap, in_.ap), reads=[in_], writes=[out])
    def red(out, in_, op=ALU.add):
        return P.op("dve", lambda e: e.tensor_reduce(out.ap, in_.ap, AX.X, op), reads=[in_], writes=[out])
    def memset(v, val, eng="dve"):
        return P.op(eng, lambda e: e.memset(v.ap, val), writes=[v])
    def dma(q, out, in_, sem, rd=(), wr=()):
        return P.op(q, lambda e: e.dma_start(out=out, in_=in_), reads=list(rd), writes=list(wr), dma_sem=sem)

    wcnt = {"i": 0}
    def wload(dram_ap, shape):
        i = wcnt["i"] % NSLOT; wcnt["i"] += 1
        v = WR.view(wslots[i], BF16, shape)
        dma("pool", v.ap, dram_ap, f"w{i}", wr=[v])
        return v

    def wtile_fm(w_d, k0, nk, c0, ncols):
        return wload(w_d[k0 * 128:(k0 + nk) * 128, c0:c0 + ncols].rearrange("(c p) n -> p c n", p=128), [nk, ncols])

    dma("sp", cst.ap, cst_d, "c0", wr=[cst])
    dma("sp", lnin.ap, lnin_d, "c1", wr=[lnin])
    dma("sp", pp.ap, pp_d, "c2", wr=[pp])
    dma("sp", fbv.ap, fb_d, "c3", wr=[fbv]); dma("sp", vkb.ap, vkb_d, "c4", wr=[vkb])
    dma("sp", vfm[0].ap, vfm_d[0], "c5", wr=[vfm[0]]); dma("sp", vfm[1].ap, vfm_d[1], "c6", wr=[vfm[1]])
    gathv = View(None, "gathd", 0, 1); sendv = View(None, "sendd", 0, 1)
    P.op("dve", lambda e: e.tensor_copy(cstb.ap[:, 0:128], cst.ap[:, 0:128]), reads=[cst], writes=[cstb])
    P.op("dve", lambda e: e.tensor_copy(cstb.ap[:, 128:256], cst.ap[:, 512:640]), reads=[cst], writes=[cstb])
    P.op("dve", lambda e: e.tensor_copy(cstb.ap[:, 256:1280], cst.ap[:, 768:1792]), reads=[cst], writes=[cstb])
    for l in range(NL):
        memset(tails[l], 0.0); memset(kprev[l], 0.0); memset(vprev[l], 0.0)
        zt = T.get(F32, [2048]) if l == 0 else zt
        if l == 0:
            memset(zt, 0.0)
        dma("sp", st_d[l], zt.ap, f"st{l}", rd=[zt], wr=[View(None, f"std{l}", 0, 1)])
    stv = [View(None, f"std{l}", 0, 1) for l in range(NL)]
    for b in range(NB):
        dma("sp", gath_d[b * 128:(b + 1) * 128, :], zt.ap[:, 0:D], f"gz{b}", rd=[zt], wr=[gathv])

    def ppv(l, a, b):
        return View(pp.ap[:, l * NPP + a:l * NPP + b], pp.key, pp.lo, pp.hi)

    def ln_fm(src, g, b, t0):
        TR.top = t0
        s1 = T.get(F32, [TT]); s2 = T.get(F32, [TT]); sq = T.get(F32, [KC, TT])
        red(s1, View(src.ap.rearrange("p c t -> p t c"), src.key, src.lo, src.hi))
        act(sq, src, AF.Square)
        red(s2, View(sq.ap.rearrange("p c t -> p t c"), sq.key, sq.lo, sq.hi))
        pm = bank(); pq = bank()
        mm(pm, onesD, s1); mm(pq, onesD, s2)
        mean = T.get(F32, [TT]); var = T.get(F32, [TT]); rstd = T.get(F32, [TT])
        cp(mean, pm, eng="act")
        tt(var, mean, mean, ALU.mult)
        tt(var, pq, var, ALU.subtract)
        ts(var, var, LN_EPS, None, ALU.add)
        act(var, var, AF.Sqrt)
        recip(rstd, var)
        tmps = [T.get(F32, [TT]), T.get(F32, [TT])]
        for c in range(KC):
            tmp = tmps[c % 2]
            tt(tmp, sub(src, slice(None), c), mean, ALU.subtract)
            tt(tmp, tmp, rstd, ALU.mult)
            act(sub(hres, slice(None), c), tmp, AF.Identity, bias=sub(b, slice(None), slice(c, c + 1)),
                scale=sub(g, slice(None), slice(c, c + 1)))
            cp(sub(hbf, slice(None), c), sub(hres, slice(None), c), eng="dve")

    def proj_fm(w_d, c0, nchunks, src, nk, consume, group=4):
        for j0 in range(0, nchunks, group):
            ng = min(group, nchunks - j0)
            wt = wtile_fm(w_d, 0, nk, c0 + j0 * 128, ng * 128) if nk * ng * 128 <= 4096 else None
            for jj in range(ng):
                ps = bank()
                for k in range(nk):
                    mm(ps, sub(wt, slice(None), k, slice(jj * 128, (jj + 1) * 128)), sub(src, slice(None), k),
                       start=(k == 0), stop=(k == nk - 1))
                consume(j0 + jj, ps)

    out_sems = []
    for s in range(NSTEP):
        P.epoch = s // cfg.get("epoch_steps", 3)
        TR.top = 0
        dma("sp", cosv.ap, cs_d[s, 0], "cos", wr=[cosv])
        dma("sp", sinv.ap, cs_d[s, 1], "sin", wr=[sinv])
        for b in range(NB):
            xt = T.get(F32, [D])
            dma("sp", xt.ap, x_d[s, b], f"x{b}", wr=[xt])
            ssum = T.get(F32, [1]); ssq = T.get(F32, [1]); junk = T.get(F32, [D])
            act(junk, xt, AF.Identity, accum=ssum)
            act(junk, xt, AF.Square, accum=ssq)
            mean = T.get(F32, [1]); var = T.get(F32, [1]); nmr = T.get(F32, [1])
            ts(mean, ssum, 1.0 / D, None, ALU.mult)
            ts(var, ssq, 1.0 / D, None, ALU.mult)
            tt(nmr, mean, mean, ALU.mult)
            tt(var, var, nmr, ALU.subtract)
            ts(var, var, LN_EPS, None, ALU.add)
            act(var, var, AF.Sqrt)
            recip(var, var)
            ts(xt, xt, mean, var, ALU.subtract, ALU.mult)
            rt = T.get(F32, [D])
            dma("sp", rt.ap, gath_d[b * 128:(b + 1) * 128, :], f"r{b}", rd=[gathv], wr=[rt])
            stt(xt, rt, sub(fbv, slice(None), slice(0, 1)), xt, ALU.mult, ALU.add)
            for c0 in range(0, KC, 4):
                ps = bank()
                for c in range(4):
                    tp(sub(ps, slice(None), slice(c * 128, (c + 1) * 128)), sub(xt, slice(None), slice((c0 + c) * 128, (c0 + c + 1) * 128)), identf)
                for c in range(4):
                    act(sub(hres, slice(None), c0 + c, slice(b * 128, (b + 1) * 128)), sub(ps, slice(None), slice(c * 128, (c + 1) * 128)),
                        AF.Identity, bias=sub(lnin, slice(None), slice(8 + c0 + c, 9 + c0 + c)), scale=sub(lnin, slice(None), slice(c0 + c, c0 + c + 1)))
        cp(hbf, hres, eng="dve")

        for l, kind in enumerate(layers):
            cw = View(pp.ap[:, l * NPP:l * NPP + 96].rearrange("p (c k) -> p c k", k=4), pp.key, pp.lo, pp.hi)
            cb = ppv(l, 96, 120)
            ln1g = ppv(l, 120, 128); ln1b = ppv(l, 128, 136); ln2g = ppv(l, 136, 144); ln2b = ppv(l, 144, 152)
            dtb = ppv(l, 152, 184); alog = ppv(l, 184, 216); dsk = ppv(l, 216, 248); snk = ppv(l, 248, 264)
            gn = ppv(l, 264, 280)
            TR.top = 0
            aneg = T.get(F32, [32]); esink = T.get(F32, [16])
            act(aneg, alog, AF.Exp)
            ts(aneg, aneg, -1.0, None, ALU.mult)
            act(esink, snk, AF.Exp)
            base_top = TR.top

            qbf = T.get(BF16, [KC, TT]); kz = T.get(BF16, [2, 4, TT]); vcur = T.get(BF16, [NB, 512])
            memset(kz, 0.0)
            for (cbase, n, sbase) in ((0, 8, 8), (16, 4, 20)):
                for j0 in range(0, n, 4):
                    wa = wtile_fm(wfm_d[l], 0, KC, (cbase + j0) * 128, 512)
                    wb = wtile_fm(wfm_d[l], 0, KC, (sbase + j0) * 128, 512)
                    for jj in range(4):
                        pa = bank(); pb = bank()
                        for k in range(KC):
                            mm(pa, sub(wa, slice(None), k, slice(jj * 128, (jj + 1) * 128)), sub(hbf, slice(None), k), start=(k == 0), stop=(k == KC - 1))
                        for k in range(KC):
                            mm(pb, sub(wb, slice(None), k, slice(jj * 128, (jj + 1) * 128)), sub(hbf, slice(None), k), start=(k == 0), stop=(k == KC - 1))
                        t1 = T.get(F32, [TT]) if (cbase == 0 and j0 == 0 and jj == 0) else t1
                        t2 = T.get(F32, [TT]) if (cbase == 0 and j0 == 0 and jj == 0) else t2
                        tt(t1, pa, cosv, ALU.mult)
                        tt(t2, pb, sinv, ALU.mult)
                        j = j0 + jj
                        if cbase == 0:
                            tt(sub(qbf, slice(None), j), t1, t2, ALU.add)
                        else:
                            for half in range(2):
                                pr = slice(half * 64, (half + 1) * 64)
                                tt(View(kz.ap[pr, half, j, :], kz.key, kz.lo, kz.hi), View(t1.ap[pr, :], t1.key, t1.lo, t1.hi),
                                   View(t2.ap[pr, :], t2.key, t2.lo, t2.hi), ALU.add)
            wv = wtile_fm(wtm_d[l], 0, KC, 0, 512)
            for b in range(NB):
                ps = bank()
                for k in range(KC):
                    mm(ps, sub(hbf, slice(None), k, slice(b * 128, (b + 1) * 128)), sub(wv, slice(None), k), start=(k == 0), stop=(k == KC - 1))
                cp(sub(vcur, slice(None), b), ps)
            att_top = TR.top
            def att_iter(b, g, it):
                qs = slice(b * 128, (b + 1) * 128)
                TR.top = att_top + (it % 3) * 4096
                pts = [T.get(BF16, [512]), T.get(BF16, [512])]
                rden = T.get(F32, [512])
                pss = []
                for kb in range(2):
                    ps = bank()
                    mm(ps, identb, mcur if kb == 1 else mprev, start=True, stop=False)
                    for slot in range(4):
                        cc, half = slot // 2, slot % 2
                        if kb == 1:
                            kv = sub(kz, slice(None), half, g, qs)
                        elif b > 0:
                            kv = sub(kz, slice(None), half, g, slice((b - 1) * 128, b * 128))
                        else:
                            kv = sub(kprev[l], slice(None), half, g)
                        mm(sub(ps, slice(None), slice(slot * 128, (slot + 1) * 128)), kv, sub(qbf, slice(None), 2 * g + cc, qs), start=False, stop=(slot == 3))
                    pss.append(ps)
                yield
                for kb in range(2):
                    kbias = None
                    if s < 2:
                        if kb == 1:
                            col = s * 4 + b
                        elif b > 0:
                            col = s * 4 + b - 1
                        else:
                            col = 3 if s == 1 else None
                        kbias = kball if col is None else sub(vkb, slice(None), slice(col, col + 1))
                    act(pts[kb], pss[kb], AF.Exp, bias=kbias, scale=0.125)
                yield
                pn = bank(); pd = bank()
                for kb in range(2):
                    if kb == 1:
                        vv = sub(vcur, slice(None), b, slice(g * 128, (g + 1) * 128))
                    elif b > 0:
                        vv = sub(vcur, slice(None), b - 1, slice(g * 128, (g + 1) * 128))
                    else:
                        vv = sub(vprev[l], slice(None), slice(g * 128, (g + 1) * 128))
                    mm(pn, vv, pts[kb], start=(kb == 0), stop=(kb == 1))
                for kb in range(2):
                    mm(pd, onesb, pts[kb], start=(kb == 0), stop=(kb == 1))
                yield
                for slot in range(4):
                    cc, half = slot // 2, slot % 2
                    h = 4 * g + 2 * cc + half
                    sl = slice(slot * 128, (slot + 1) * 128)
                    ts(sub(rden, slice(None), sl), sub(pd, slice(None), sl), sub(esink, slice(None), slice(h, h + 1)), None, ALU.add)
                recip(rden, rden)
                yield
                for slot in range(4):
                    cc, half = slot // 2, slot % 2
                    pr = slice(half * 64, (half + 1) * 64)
                    sl = slice(slot * 128, (slot + 1) * 128)
                    alo = attnT.lo + ((2 * g + cc) * TT + b * 128) * 2
                    tt(View(attnT.ap[pr, 2 * g + cc, qs], attnT.key, alo, alo + 256),
                       View(pn.ap[pr, sl], pn.key, pn.lo, pn.hi), View(rden.ap[pr, sl], rden.key, rden.lo, rden.hi), ALU.mult)
            interleave([att_iter(b, g, b * 4 + g) for b in range(NB) for g in range(4)], 3)
            TR.top = att_top + 3 * 4096
            cp(kprev[l], sub(kz, slice(None), slice(None), slice(None), slice(TT - 128, TT)), eng="dve")
            cp(vprev[l], sub(vcur, slice(None), NB - 1), eng="dve")

            TR.top = base_top
            state = T.get(F32, [4, 512])
            dma("sp", state.ap, st_d[l].rearrange("p (g n) -> p g n", g=4), f"sl{l}", rd=[stv[l]], wr=[state])
            dtt = T.get(F32, [NB, 32]); dat = T.get(F32, [NB, 32]); expA = T.get(F32, [NB, 32]); cdec = T.get(F32, [NB, 32])
            wdt = wtile_fm(wtm_d[l], 0, KC, 512 + 2048, 32)
            for b in range(NB):
                ps = bank()
                for k in range(KC):
                    mm(sub(ps, slice(None), slice(0, 32)), sub(hbf, slice(None), k, slice(b * 128, (b + 1) * 128)), sub(wdt, slice(None), k), start=(k == 0), stop=(k == KC - 1))
                tt(sub(dtt, slice(None), b), sub(ps, slice(None), slice(0, 32)), dtb, ALU.add)
            act(dtt, dtt, AF.Exp)
            act(dtt, dtt, AF.Ln, bias=1.0)
            for b in range(NB):
                tt(sub(dat, slice(None), b), sub(dtt, slice(None), b), aneg, ALU.mult)
                ps = bank()
                mm(sub(ps, slice(None), slice(0, 32)), tri, sub(dat, slice(None), b))
                mm(sub(ps, slice(None), slice(32, 64)), onesf, sub(dat, slice(None), b))
                act(sub(expA, slice(None), b), sub(ps, slice(None), slice(0, 32)), AF.Exp)
                act(sub(cdec, slice(None), b), sub(ps, slice(None), slice(32, 64)), AF.Exp)
            ssd_top = TR.top
            for g in range(4):
                TR.top = ssd_top
                xg = T.get(BF16, [6, TT])
                xpre = [T.get(F32, [TT + 3]) for _ in range(2)]
                cacc = [T.get(F32, [TT]) for _ in range(2)]
                chunks = [4 * g + i for i in range(4)] + [16 + g, 20 + g]
                wx1 = wtile_fm(wfm_d[l], 0, KC, 3072 + chunks[0] * 128, 512)
                wx2 = wtile_fm(wfm_d[l], 0, KC, 3072 + chunks[4] * 128, 128)
                wx3 = wtile_fm(wfm_d[l], 0, KC, 3072 + chunks[5] * 128, 128)
                for i, c in enumerate(chunks):
                    wt = wx1 if i < 4 else (wx2 if i == 4 else wx3)
                    co = (i * 128) if i < 4 else 0
                    ps = bank()
                    for k in range(KC):
                        mm(ps, sub(wt, slice(None), k, slice(co, co + 128)), sub(hbf, slice(None), k), start=(k == 0), stop=(k == KC - 1))
                    xp = xpre[i % 2]; ca = cacc[i % 2]
                    cp(sub(xp, slice(None), slice(0, 3)), sub(tails[l], slice(None), c), eng="dve")
                    cp(sub(xp, slice(None), slice(3, TT + 3)), ps, eng="act")
                    if s < 2:
                        tt(sub(xp, slice(None), slice(3, TT + 3)), sub(xp, slice(None), slice(3, TT + 3)), vfm[s], ALU.mult)
                    cp(sub(tails[l], slice(None), c), sub(xp, slice(None), slice(TT, TT + 3)), eng="dve")
                    ts(ca, sub(xp, slice(None), slice(0, TT)), sub(cw, slice(None), c, slice(0, 1)), sub(cb, slice(None), slice(c, c + 1)), ALU.mult, ALU.add)
                    for k in range(1, 4):
                        stt(ca, sub(xp, slice(None), slice(k, TT + k)), sub(cw, slice(None), c, slice(k, k + 1)), ca, ALU.mult, ALU.add)
                    act(sub(xg, slice(None), i), ca, AF.Silu)
                    if s < 2 and i < 4:
                        tt(sub(xg, slice(None), i), sub(xg, slice(None), i), vfm[s], ALU.mult)
                wz = wtile_fm(wtm_d[l], 0, KC, 512 + g * 512, 512)
                blk_top = TR.top
                def ssd_blk(b, g=g, xg=xg, wz=wz):
                    TR.top = blk_top + (b % 2) * 24 * 1024
                    qs = slice(b * 128, (b + 1) * 128)
                    xs_t = T.get(BF16, [8, 64]); xc = T.get(BF16, [8, 64]); xd = T.get(BF16, [8, 64]); btok = T.get(BF16, [128])
                    rhsD = T.get(F32, [8, 128]); E = T.get(F32, [8, 128]); w1 = T.get(F32, [8]); mcb = T.get(F32, [128])
                    MT = T.get(BF16, [8, 128]); sbf = T.get(BF16, [512]); yv = T.get(F32, [8, 64]); xsd = T.get(F32, [8, 64])
                    sz = T.get(F32, [512]); ss = T.get(F32, [1]); ub = T.get(BF16, [512])
                    pt = bank(BF16)
                    for i in range(4):
                        tp(sub(pt, slice(None), slice(i * 128, (i + 1) * 128)), sub(xg, slice(None), i, qs), identb)
                    tp(sub(pt, slice(None), slice(512, 640)), sub(xg, slice(None), 4, qs), identb)
                    dag = View(dat.ap[:, b, g * 8:(g + 1) * 8], dat.key, dat.lo, dat.hi)
                    tt(rhsD, View(tri.ap.unsqueeze(1).to_broadcast([128, 8, 128]), tri.key, tri.lo, tri.hi),
                       View(dag.ap.unsqueeze(2).to_broadcast([128, 8, 128]), dag.key, dag.lo, dag.hi), ALU.mult)
                    pcb = bank()
                    mm(sub(pcb, slice(None), slice(0, 128)), sub(xg, slice(None), 4, qs), sub(xg, slice(None), 5, qs))
                    pz = bank()
                    for k in range(KC):
                        mm(pz, sub(hbf, slice(None), k, qs), sub(wz, slice(None), k), start=(k == 0), stop=(k == KC - 1))
                    yield
                    ptx = View(pt.ap[:, 0:512].rearrange("p (h d) -> p h d", d=64), pt.key, pt.lo, pt.hi)
                    cp(xs_t, ptx, eng="act")
                    cp(btok, sub(pt, slice(None), slice(512, 640)), eng="act")
                    act(sz, pz, AF.Silu)
                    tt(mcb, sub(pcb, slice(None), slice(0, 128)), tri, ALU.mult)
                    pseg = [bank(), bank()]
                    for hf in range(2):
                        mm(pseg[hf], ustr, View(rhsD.ap[:, hf * 4:(hf + 1) * 4, :].rearrange("p h l -> p (h l)"), rhsD.key, rhsD.lo, rhsD.hi))
                    yield
                    for hf in range(2):
                        act(View(E.ap[:, hf * 4:(hf + 1) * 4, :].rearrange("p h l -> p (h l)"), E.key, E.lo, E.hi), pseg[hf], AF.Exp)
                    yield
                    dtg = View(dtt.ap[:, b, g * 8:(g + 1) * 8], dtt.key, dtt.lo, dtt.hi)
                    tt(w1, dtg, View(E.ap[:, :, 127], E.key, E.lo, E.hi), ALU.mult)
                    tt(xc, xs_t, View(dtg.ap.unsqueeze(2).to_broadcast([128, 8, 64]), dtg.key, dtg.lo, dtg.hi), ALU.mult)
                    tt(xd, xs_t, View(w1.ap.unsqueeze(2).to_broadcast([128, 8, 64]), w1.key, w1.lo, w1.hi), ALU.mult)
                    tt(MT, E, View(mcb.ap.unsqueeze(1).to_broadcast([128, 8, 128]), mcb.key, mcb.lo, mcb.hi), ALU.mult)
                    dsg = View(dsk.ap[:, g * 8:(g + 1) * 8], dsk.key, dsk.lo, dsk.hi)
                    tt(xsd, xs_t, View(dsg.ap.unsqueeze(2).to_broadcast([128, 8, 64]), dsg.key, dsg.lo, dsg.hi), ALU.mult)
                    yield
                    py = bank()
                    for hh in range(8):
                        mm(sub(py, slice(None), slice(hh * 64, (hh + 1) * 64)), sub(MT, slice(None), hh), sub(xc, slice(None), hh))
                    pst = bank()
                    mm(pst, btok, View(xd.ap.rearrange("p h d -> p (h d)"), xd.key, xd.lo, xd.hi))
                    yield
                    po = bank()
                    cp(sbf, sub(state, slice(None), g), eng="dve")
                    mm(po, sub(xg, slice(None), 5, qs), sbf)
                    cdg = View(cdec.ap[:, b, g * 8:(g + 1) * 8], cdec.key, cdec.lo, cdec.hi)
                    sgv = sub(state, slice(None), g)
                    sg3 = View(state.ap[:, g, :].rearrange("p (h d) -> p h d", d=64), state.key, sgv.lo, sgv.hi)
                    tt(sg3, sg3, View(cdg.ap.unsqueeze(2).to_broadcast([128, 8, 64]), cdg.key, cdg.lo, cdg.hi), ALU.mult)
                    tt(sgv, sgv, pst, ALU.add)
                    yield
                    eag = View(expA.ap[:, b, g * 8:(g + 1) * 8], expA.key, expA.lo, expA.hi)
                    po3 = View(po.ap.rearrange("p (h d) -> p h d", d=64), po.key, po.lo, po.hi)
                    py3 = View(py.ap.rearrange("p (h d) -> p h d", d=64), py.key, py.lo, py.hi)
                    tt(yv, po3, View(eag.ap.unsqueeze(2).to_broadcast([128, 8, 64]), eag.key, eag.lo, eag.hi), ALU.mult)
                    tt(yv, yv, py3, ALU.add)
                    tt(yv, yv, xsd, ALU.add)
                    y2 = View(yv.ap.rearrange("p h d -> p (h d)"), yv.key, yv.lo, yv.hi)
                    tt(y2, y2, sz, ALU.mult)
                    yield
                    act(sz, y2, AF.Square, accum=ss)
                    ts(ss, ss, 1.0 / 512, RMS_EPS, ALU.mult, ALU.add)
                    act(ss, ss, AF.Sqrt)
                    recip(ss, ss)
                    ts(ub, y2, ss, None, ALU.mult)
                    yield
                    pu = bank(BF16)
                    for i in range(4):
                        tp(sub(pu, slice(None), slice(i * 128, (i + 1) * 128)), sub(ub, slice(None), slice(i * 128, (i + 1) * 128)), identb)
                    yield
                    for i in range(4):
                        act(sub(unT, slice(None), 4 * g + i, qs), sub(pu, slice(None), slice(i * 128, (i + 1) * 128)), AF.Identity,
                            scale=sub(gn, slice(None), slice(4 * g + i, 4 * g + i + 1)))
                interleave([ssd_blk(b) for b in range(NB)], 2)
            dma("sp", st_d[l].rearrange("p (g n) -> p g n", g=4), state.ap, f"ss{l}", rd=[state], wr=[stv[l]])

            TR.top = base_top
            mT = T.get(BF16, [KC, TT])
            for c in range(KC):
                wa = wtile_fm(wao_d[l], 0, KC, c * 128, 128)
                ws = wtile_fm(wso_d[l], 0, 16, c * 128, 128)
                wg2 = wtile_fm(wfm_d[l], 0, KC, 6144 + c * 128, 128)
                wg3 = wtile_fm(wfm_d[l], 0, KC, 6144 + 1024 + c * 128, 128)
                pa = bank(); psd = bank(); pga = bank(); pgs = bank()
                for k in range(KC):
                    mm(pa, sub(wa, slice(None), k), sub(attnT, slice(None), k), start=(k == 0), stop=(k == KC - 1))
                for k in range(16):
                    mm(psd, sub(ws, slice(None), k), sub(unT, slice(None), k), start=(k == 0), stop=(k == 15))
                for k in range(KC):
                    mm(pga, sub(wg2, slice(None), k), sub(hbf, slice(None), k), start=(k == 0), stop=(k == KC - 1))
                for k in range(KC):
                    mm(pgs, sub(wg3, slice(None), k), sub(hbf, slice(None), k), start=(k == 0), stop=(k == KC - 1))
                if c == 0:
                    sa = T.get(F32, [TT]); sb_ = T.get(F32, [TT])
                act(sa, pga, AF.Sigmoid); act(sb_, pgs, AF.Sigmoid)
                tt(sa, sa, pa, ALU.mult); tt(sb_, sb_, psd, ALU.mult)
                tt(sub(mT, slice(None), c), sa, sb_, ALU.add)
            for c in range(KC):
                wo = wtile_fm(wo_d[l], 0, KC, c * 128, 128)
                po = bank()
                for k in range(KC):
                    mm(po, sub(wo, slice(None), k), sub(mT, slice(None), k), start=(k == 0), stop=(k == KC - 1))
                stt(sub(hres, slice(None), c), sub(hres, slice(None), c), ALPHA, po, ALU.mult, ALU.add)
            ln_fm(hres, ln1g, ln1b, TR.top)

            TR.top = base_top
            if kind == "dense":
                NF = DFF // 128
                actb = T.get(BF16, [NF, TT])
                tu = T.get(F32, [TT])
                for j0 in range(0, NF, 4):
                    ng = min(4, NF - j0)
                    wgt = wtile_fm(wg_d[l][0], 0, KC, j0 * 128, ng * 128)
                    wut = wtile_fm(wu_d[l][0], 0, KC, j0 * 128, ng * 128)
                    for jj in range(ng):
                        pg = bank(); pu = bank()
                        for k in range(KC):
                            mm(pg, sub(wgt, slice(None), k, slice(jj * 128, (jj + 1) * 128)), sub(hbf, slice(None), k), start=(k == 0), stop=(k == KC - 1))
                        for k in range(KC):
                            mm(pu, sub(wut, slice(None), k, slice(jj * 128, (jj + 1) * 128)), sub(hbf, slice(None), k), start=(k == 0), stop=(k == KC - 1))
                        act(tu, pg, AF.Silu)
                        tt(sub(actb, slice(None), j0 + jj), tu, pu, ALU.mult)
                for c0 in range(0, KC, 4):
                    pos = [bank() for _ in range(4)]
                    for k0 in range(0, NF, 8):
                        nk = min(8, NF - k0)
                        wdt_ = wtile_fm(wd_d[l][0], k0, nk, c0 * 128, 512)
                        for c in range(4):
                            for k in range(nk):
                                mm(pos[c], sub(wdt_, slice(None), k, slice(c * 128, (c + 1) * 128)), sub(actb, slice(None), k0 + k),
                                   start=(k0 + k == 0), stop=(k0 + k == NF - 1))
                    for c in range(4):
                        stt(sub(hres, slice(None), c0 + c), sub(hres, slice(None), c0 + c), ALPHA, pos[c], ALU.mult, ALU.add)
            else:
                NF = DFFE // 128
                actb = T.get(BF16, [NF, TT]); tu = T.get(F32, [TT])
                gateb = T.get(F32, [NE, TT])
                wrt = T.get(F32, [KC, NE])
                dma("sp", wrt.ap, wr_d[l].rearrange("(c p) e -> p c e", p=128), "wr", wr=[wrt])
                for b in range(NB):
                    qs = slice(b * 128, (b + 1) * 128)
                    ps = bank()
                    for k in range(KC):
                        mm(sub(ps, slice(None), slice(0, NE)), sub(hres, slice(None), k, qs), sub(wrt, slice(None), k), start=(k == 0), stop=(k == KC - 1))
                    lg = T.get(F32, [NE]); m1 = T.get(F32, [1]); m2 = T.get(F32, [1]); msk = T.get(F32, [NE]); gt = T.get(F32, [NE]); dg = T.get(F32, [128])
                    cp(lg, sub(ps, slice(None), slice(0, NE)), eng="dve")
                    red(m1, lg, ALU.max)
                    ts(msk, lg, m1, NEG, ALU.is_ge, ALU.mult)
                    tt(msk, msk, lg, ALU.add)
                    red(m2, msk, ALU.max)
                    ts(gt, lg, m1, None, ALU.subtract)
                    act(gt, gt, AF.Exp)
                    ts(msk, lg, m2, None, ALU.is_ge)
                    tt(gt, gt, msk, ALU.mult)
                    red(m1, gt, ALU.add)
                    recip(m1, m1)
                    ts(gt, gt, m1, None, ALU.mult)
                    for e_ in range(NE):
                        ts(dg, identf, sub(gt, slice(None), slice(e_, e_ + 1)), None, ALU.mult)
                        pb_ = bank()
                        mm(sub(pb_, slice(None), slice(0, 128)), onesf, dg)
                        cp(sub(gateb, slice(None), e_, qs), sub(pb_, slice(None), slice(0, 128)))
                ts(hres, hres, ALPHA, None, ALU.mult)
                for e_ in range(NE):
                    for j0 in range(0, NF, 4):
                        ng = min(4, NF - j0)
                        wgt = wtile_fm(wg_d[l][e_], 0, KC, j0 * 128, ng * 128)
                        wut = wtile_fm(wu_d[l][e_], 0, KC, j0 * 128, ng * 128)
                        for jj in range(ng):
                            pg = bank(); pu = bank()
                            for k in range(KC):
                                mm(pg, sub(wgt, slice(None), k, slice(jj * 128, (jj + 1) * 128)), sub(hbf, slice(None), k), start=(k == 0), stop=(k == KC - 1))
                            for k in range(KC):
                                mm(pu, sub(wut, slice(None), k, slice(jj * 128, (jj + 1) * 128)), sub(hbf, slice(None), k), start=(k == 0), stop=(k == KC - 1))
                            act(tu, pg, AF.Silu)
                            tt(sub(actb, slice(None), j0 + jj), tu, pu, ALU.mult)
                    for c in range(KC):
                        wds = [wtile_fm(wd_d[l][e_], k0, min(14, NF - k0), c * 128, 128) for k0 in range(0, NF, 14)]
                        po = bank()
                        for k in range(NF):
                            mm(po, sub(wds[k // 14], slice(None), k % 14), sub(actb, slice(None), k), start=(k == 0), stop=(k == NF - 1))
                        tt(tu, po, sub(gateb, slice(None), e_), ALU.mult)
                        tt(sub(hres, slice(None), c), sub(hres, slice(None), c), tu, ALU.add)
            ln_fm(hres, ln2g, ln2b, TR.top)

        TR.top = 0
        for b in range(NB):
            ot = T.get(F32, [D])
            for c0 in range(0, KC, 4):
                ps = bank()
                for c in range(4):
                    tp(sub(ps, slice(None), slice(c * 128, (c + 1) * 128)), sub(hres, slice(None), c0 + c, slice(b * 128, (b + 1) * 128)), identf)
                cp(sub(ot, slice(None), slice(c0 * 128, (c0 + 4) * 128)), ps)
            o = dma("sp", y_d[s, b], ot.ap, f"o{b}", rd=[ot], wr=[])
            out_sems.append(o)
            if s < NSTEP - 1:
                dma("sp", send_d[b * 128:(b + 1) * 128, :], ot.ap, f"sd{b}", rd=[ot], wr=[sendv])
        if s < NSTEP - 1:
            cco = P.op("pool", lambda e: e.collective_compute("AllGather", ALU.bypass, replica_groups=[[0, 1], [2, 3], [4, 5], [6, 7]],
                                                           ins=[send_d.opt()], outs=[gath_d.opt()]), reads=[sendv], writes=[gathv])
            cco.noval = True

    def final_waits():
        last = {}
        for o in out_sems:
            last[id(o.sem)] = (o.sem, o.semval)
        return list(last.values())
    P.emit(final_waits)
    return nc, es


def _consts():
    c = np.zeros((128, 6 * 128 + 1024 + 2), np.float32)
    i = np.arange(128)
    c[:, 0:128] = np.eye(128)
    c[:, 128:256] = (i[:, None] <= i[None, :])
    c[:, 256:384] = (i[:, None] > i[None, :])
    c[:, 384:512] = 1.0 / D
    c[:, 512:640] = 1.0
    mprev = np.where(i[:, None] > i[None, :], 0.0, NEG)
    mcur = np.where(i[:, None] <= i[None, :], 0.0, NEG)
    c[:, 768:768 + 512] = np.tile(mprev, (1, 4))
    c[:, 768 + 512:768 + 1024] = np.tile(mcur, (1, 4))
    c[:, 768 + 1024] = np.where(i < PAD, NEG, 0.0)
    c[:, 768 + 1025] = NEG
    return c


def _cossin(nstep):
    T_ = nstep * TT
    pos = (np.arange(T_) - PAD).astype(np.float32)
    inv = (np.float32(10000.0) ** (-np.arange(0, 64, 2, dtype=np.float32) / np.float32(64))).astype(np.float32)
    ang = (pos[:, None] * inv[None, :]).astype(np.float32)
    cos = np.cos(ang).astype(np.float32); sin = np.sin(ang).astype(np.float32)
    p = np.arange(128)
    f = p % 32
    sign = np.where((p % 64) < 32, -1.0, 1.0).astype(np.float32)
    cosT = cos[:, f].T
    sinT = sin[:, f].T * sign[:, None]
    out = np.zeros((nstep, 2, 128, TT), np.float32)
    for s in range(nstep):
        out[s, 0] = cosT[:, s * TT:(s + 1) * TT]
        out[s, 1] = sinT[:, s * TT:(s + 1) * TT]
    return out


def _fm(v, n):
    return np.ascontiguousarray(v.reshape(n, 128).T)


def _layer_inputs(inp, l, li, kind, m):
    w_in = inp["w_in"][l]
    q = w_in[:, 2048:3072]; k = w_in[:, 3072:3328]; v = w_in[:, 3328:3584]
    z = w_in[:, 3584:5632]; xbc = w_in[:, 5632:8704]; dtw = w_in[:, 8704:8736]
    ga = w_in[:, 0:1024]; gs = w_in[:, 1024:2048]
    sw = (np.arange(64) + 32) % 64
    qs = q.reshape(D, 16, 64)[:, :, sw].reshape(D, 1024)
    ks = k.reshape(D, 4, 64)[:, :, sw].reshape(D, 256)
    dup = lambda a: np.repeat(a.reshape(D, 4, 1, 64), 2, axis=2).reshape(D, 512)
    m[f"wfm{li}"] = np.ascontiguousarray(np.concatenate([q, qs, dup(k), dup(ks), xbc, ga, gs], axis=1))
    m[f"wtm{li}"] = np.ascontiguousarray(np.concatenate([dup(v), z, dtw], axis=1))
    m[f"wao{li}"] = inp["w_attn_out"][l]; m[f"wso{li}"] = inp["w_ssd_out"][l]; m[f"wo{li}"] = inp["w_o"][l]
    if kind == "dense":
        m[f"wg{li}"] = inp["ffn_wg"][l // 2][None]; m[f"wu{li}"] = inp["ffn_wu"][l // 2][None]; m[f"wd{li}"] = inp["ffn_wd"][l // 2][None]
    else:
        m[f"wg{li}"] = inp["moe_wg"][l // 2]; m[f"wu{li}"] = inp["moe_wu"][l // 2]; m[f"wd{li}"] = inp["moe_wd"][l // 2]
        m[f"wr{li}"] = inp["moe_router"][l // 2]
    pp = np.zeros((128, NPP), np.float32)
    pp[:, 0:96] = np.transpose(inp["conv_w"][l].reshape(4, 24, 128), (2, 1, 0)).reshape(128, 96)
    pp[:, 96:120] = _fm(inp["conv_b"][l], 24)
    pp[:, 120:128] = _fm(inp["ln1_g"][l], 8); pp[:, 128:136] = _fm(inp["ln1_b"][l], 8)
    pp[:, 136:144] = _fm(inp["ln2_g"][l], 8); pp[:, 144:152] = _fm(inp["ln2_b"][l], 8)
    pp[:, 152:184] = inp["dt_bias"][l][None, :]; pp[:, 184:216] = inp["a_log"][l][None, :]
    pp[:, 216:248] = inp["d_skip"][l][None, :]; pp[:, 248:264] = inp["sinks"][l][None, :]
    pp[:, 264:280] = _fm(inp["ssd_norm_g"][l], 16)
    return pp


def _masks(valid2):
    vfm = np.ascontiguousarray(np.broadcast_to(valid2[:, None, :], (2, 128, TT))).astype(np.float32)
    vkb = np.ascontiguousarray(((valid2.reshape(2, NB, 128) - 1.0) * (-NEG)).transpose(2, 0, 1).reshape(128, 8)).astype(np.float32)
    return vfm, vkb


def make_inputs(inp, stages, nstep, seqs):
    nst = len(stages)
    stage_shared = []
    kinds = None
    for layer_ids in stages:
        kinds = ["dense" if l % 2 == 0 else "moe" for l in layer_ids]
        sh = {}
        pps = [_layer_inputs(inp, l, li, kinds[li], sh) for li, l in enumerate(layer_ids)]
        sh["pp"] = np.ascontiguousarray(np.concatenate(pps, axis=1))
        sh["cst"] = _consts()
        sh["cossin"] = None
        stage_shared.append(sh)
    cs = _cossin(nstep)
    lnin_real = np.ascontiguousarray(np.concatenate([_fm(inp["ln_in_g"], 8), _fm(inp["ln_in_b"], 8)], axis=1))
    lnin_id = np.ascontiguousarray(np.concatenate([np.ones((128, 8), np.float32), np.zeros((128, 8), np.float32)], axis=1))
    vpad = np.ones(TT, np.float32); vpad[:PAD] = 0.0
    maps = []
    Tn = nstep * TT
    for b in seqs:
        for si in range(nst):
            m = dict(stage_shared[si])
            xs = np.zeros((Tn, D), np.float32)
            valid2 = np.ones((2, TT), np.float32)
            if si == 0:
                xs[PAD:PAD + NMETA] = inp["meta_tokens"]
                n = min(Tn - 128 - (nst - 1) * TT, inp["x"].shape[1])
                xs[128:128 + n] = inp["x"][b, :n]
                valid2[0] = vpad
                m["lnin"] = lnin_real; m["fb"] = np.zeros((128, 1), np.float32)
                m["cossin"] = cs
            else:
                valid2[0] = 0.0; valid2[1] = vpad
                m["lnin"] = lnin_id; m["fb"] = np.ones((128, 1), np.float32)
                cs1 = np.zeros_like(cs); cs1[1:] = cs[:-1]; cs1[0] = cs[0]
                m["cossin"] = cs1
            m["vfm"], m["vkb"] = _masks(valid2)
            m["x"] = xs.reshape(nstep, NB, 128, D)
            maps.append(m)
    return maps, kinds


NTILE_FULL = 17
STAGES = [[0, 1], [2, 3]]


def kernel(**inputs):
    inp = {k: np.asarray(v) for k, v in inputs.items()}
    nstep = NTILE_FULL + len(STAGES) - 1
    maps, kinds = make_inputs(inp, STAGES, nstep, list(range(4)))
    nc, es = build(dict(layers=kinds, nstep=nstep))
    with es:
        res = run_bass_kernel_spmd(nc, maps, core_ids=list(range(8)))
    S = inp["x"].shape[1]
    outs = []
    for b in range(4):
        y = res.results[2 * b + 1]["y"].reshape(nstep, TT, D)[1:].reshape(NTILE_FULL * TT, D)
        outs.append(y[128:128 + S])
    return np.stack(outs, axis=0).astype(np.float32)
```

```python
import numpy as np
from contextlib import ExitStack
import concourse.bass as bass
import concourse.mybir as mybir
from concourse.bass_utils import run_bass_kernel_spmd

F32 = mybir.dt.float32
BF16 = mybir.dt.bfloat16
AF = mybir.ActivationFunctionType
ALU = mybir.AluOpType
AX = mybir.AxisListType

D = 1024; KC = 8; TT = 512; NB = 4; BLK = 128
DEPTH = 4; PAD = 112; NMETA = 16
DFF = 2816; DFFE = 3584; NE = 8
ALPHA = (2 * DEPTH) ** 0.25
LN_EPS = 1e-5; RMS_EPS = 1e-5
NEG = -30000.0
PAGE = 512
NPP = 288


class View:
    def __init__(self, ap, key, lo, hi, shape=None, esz=None):
        self.ap = ap; self.key = key; self.lo = lo; self.hi = hi; self.shape = shape; self.esz = esz

    def pages(self):
        if self.key is None:
            return []
        return [(self.key, p) for p in range(self.lo // PAGE, (self.hi - 1) // PAGE + 1)]


class Op:
    __slots__ = ("eng", "fn", "deps", "idx", "inc", "is_dma", "sem", "semval", "has_dep", "epoch", "noval")

    def __init__(self, eng, fn):
        self.eng = eng; self.fn = fn; self.deps = []; self.inc = None
        self.is_dma = False; self.sem = None; self.semval = 0; self.has_dep = False; self.noval = False


class Prog:
    ENGS = ("pe", "act", "dve", "pool", "sp")

    def __init__(self, nc, es):
        self.nc = nc; self.es = es
        self.q = {e: [] for e in self.ENGS}
        self.pw = {}; self.pr = {}
        self.dsem = {}; self.dcnt = {}
        self.toggle = 0
        self.const_keys = set()
        self.epoch = 0

    def op(self, eng, fn, reads=(), writes=(), dma_sem=None):
        o = Op(eng, fn); o.epoch = self.epoch
        deps = {}
        def add(p):
            if p is None or p is o:
                return
            deps[id(p)] = p
        for v in reads:
            for pg in v.pages():
                add(self.pw.get(pg))
        for v in writes:
            for pg in v.pages():
                add(self.pw.get(pg))
                for r in self.pr.get(pg, ()):
                    add(r)
        for v in writes:
            for pg in v.pages():
                self.pw[pg] = o; self.pr[pg] = []
        for v in reads:
            if v.key in self.const_keys:
                continue
            for pg in v.pages():
                self.pr.setdefault(pg, []).append(o)
        best = {}
        for p in deps.values():
            if p.is_dma:
                best[("d", id(p))] = p
            else:
                if p.eng == o.eng and (o.eng == "pe"):
                    continue
                b = best.get(p.eng)
                if b is None or p.idx > b.idx:
                    best[p.eng] = p
        o.deps = list(best.values())
        for p in o.deps:
            p.has_dep = True
        if dma_sem is not None:
            o.is_dma = True
            if dma_sem[0] == "w":
                dma_sem = dma_sem + "_%d" % self.epoch
            if dma_sem not in self.dsem:
                self.dsem[dma_sem] = self.es.enter_context(self.nc.semaphore("d_" + dma_sem))
                self.dcnt[dma_sem] = 0
            self.dcnt[dma_sem] += 16
            o.sem = self.dsem[dma_sem]; o.semval = self.dcnt[dma_sem]
        o.idx = len(self.q[eng])
        self.q[eng].append(o)
        return o

    def emit(self, final_waits):
        nc = self.nc
        esem = {}
        for e in self.ENGS:
            c = {}
            for o in self.q[e]:
                if o.is_dma:
                    continue
                if o.has_dep:
                    c[o.epoch] = c.get(o.epoch, 0) + 1; o.inc = c[o.epoch]
                    if (e, o.epoch) not in esem:
                        esem[(e, o.epoch)] = self.es.enter_context(nc.semaphore("e_%s_%d" % (e, o.epoch)))
        def run(e, eng):
            waited = {}
            for o in self.q[e]:
                for p in o.deps:
                    if p.is_dma:
                        s, v = p.sem, p.semval
                    else:
                        s, v = esem[(p.eng, p.epoch)], p.inc
                    k = id(s)
                    if waited.get(k, 0) >= v:
                        continue
                    waited[k] = v
                    eng.wait_ge(s, v)
                ins = o.fn(eng)
                if o.is_dma:
                    ins.then_inc(o.sem, 16)
                elif o.inc is not None:
                    if o.noval:
                        ins.then_inc(esem[(e, o.epoch)])
                    else:
                        ins.then_inc(esem[(e, o.epoch)], 1)
            if e == "sp":
                for (s, v) in final_waits():
                    eng.wait_ge(s, v)
        with nc.Block() as block:
            @block.tensor
            def _(eng): run("pe", eng)
            @block.scalar
            def _(eng): run("act", eng)
            @block.vector
            def _(eng): run("dve", eng)
            @block.gpsimd
            def _(eng): run("pool", eng)
            @block.sync
            def _(eng): run("sp", eng)


class Arena:
    def __init__(self, nc, es, name, nbytes, dt=F32):
        self.t = es.enter_context(nc.sbuf_tensor(name, [128, nbytes // 4], F32))
        self.key = name; self.nbytes = nbytes; self.top = 0

    def alloc(self, nbytes):
        o = self.top; self.top += (nbytes + 63) // 64 * 64
        assert self.top <= self.nbytes, (self.key, self.top, self.nbytes)
        return o

    def view(self, off, dt, shape, part=(0, 128)):
        esz = 4 if dt == F32 else 2
        n = int(np.prod(shape))
        ap = self.t[part[0]:part[1], off // 4:(off + n * esz) // 4]
        if dt != F32:
            ap = ap.bitcast(dt)
        if len(shape) == 2:
            ap = ap.rearrange("p (a b) -> p a b", a=shape[0])
        elif len(shape) == 3:
            ap = ap.rearrange("p (a b c) -> p a b c", a=shape[0], b=shape[1])
        return View(ap, self.key, off, off + n * esz, shape=list(shape), esz=esz)


def interleave(gens, width):
    gens = list(gens); active = []
    while gens or active:
        while len(active) < width and gens:
            active.append(gens.pop(0))
        for g_ in list(active):
            try:
                next(g_)
            except StopIteration:
                active.remove(g_)


def build(cfg):
    layers = cfg["layers"]; NL = len(layers); NSTEP = cfg["nstep"]
    nc = bass.Bass("TRN2", target_bir_lowering=False)
    es = ExitStack()
    P = Prog(nc, es)

    def din(name, shape, dt=F32):
        return nc.dram_tensor(name, list(shape), dt, kind="ExternalInput").ap()

    x_d = din("x", [NSTEP, NB, 128, D])
    cs_d = din("cossin", [NSTEP, 2, 128, TT])
    cst_d = din("cst", [128, 6 * 128 + 2 * 512 + 2])
    fb_d = din("fb", [128, 1]); vfm_d = din("vfm", [2, 128, TT]); vkb_d = din("vkb", [128, 8])
    send_d = nc.dram_tensor("send", [TT, D], F32, kind="Internal").ap()
    gath_d = nc.dram_tensor("gath", [2 * TT, D], F32, kind="Internal").ap()
    lnin_d = din("lnin", [128, 16])
    pp_d = din("pp", [128, NL * NPP])
    wfm_d = [din(f"wfm{l}", [D, 3072 + 3072]) for l in range(NL)]
    wmg_d = [din(f"wmg{l}", [KC, 128, 40, 128]) for l in range(NL)]
    wtm_d = [din(f"wtm{l}", [D, 512 + 2048 + 32]) for l in range(NL)]
    wo_d = [din(f"wo{l}", [KC, 128, KC, 128]) for l in range(NL)]
    wg_d = []; wu_d = []; wd_d = []; wr_d = []
    for l, kind in enumerate(layers):
        if kind == "dense":
            wg_d.append(din(f"wg{l}", [1, D, DFF])); wu_d.append(din(f"wu{l}", [1, D, DFF]))
            wd_d.append(din(f"wd{l}", [1, DFF, D])); wr_d.append(None)
        else:
            wg_d.append(din(f"wg{l}", [NE, D, DFFE])); wu_d.append(din(f"wu{l}", [NE, D, DFFE]))
            wd_d.append(din(f"wd{l}", [NE, KC, 128, DFFE // 128, 128])); wr_d.append(din(f"wr{l}", [D, NE]))
    y_d = nc.dram_tensor("y", [NSTEP, NB, 128, D], F32, kind="ExternalOutput").ap()
    st_d = [nc.dram_tensor(f"state{l}", [128, 2048], F32, kind="Internal").ap() for l in range(NL)]

    PER = Arena(nc, es, "per", 84 * 1024)
    WR = Arena(nc, es, "wring", 32 * 1024)
    TR = Arena(nc, es, "trans", 88 * 1024)
    NSLOT = 4
    wslots = [WR.alloc(8192) for _ in range(NSLOT)]
    hres = PER.view(PER.alloc(KC * TT * 4), F32, [KC, TT])
    hbf = PER.view(PER.alloc(KC * TT * 2), BF16, [KC, TT])
    cst = PER.view(PER.alloc((6 * 128 + 1024 + 2) * 4), F32, [6 * 128 + 1024 + 2])
    fbv = PER.view(PER.alloc(64), F32, [1]); vkb = PER.view(PER.alloc(64), F32, [8])
    vfm = [PER.view(PER.alloc(TT * 4), F32, [TT]) for _ in range(2)]
    cstb = PER.view(PER.alloc((2 * 128 + 1024) * 2), BF16, [2 * 128 + 1024])
    lnin = PER.view(PER.alloc(64), F32, [16])
    pp = PER.view(PER.alloc(NL * NPP * 4), F32, [NL * NPP])
    cosv = PER.view(PER.alloc(TT * 4), F32, [TT]); sinv = PER.view(PER.alloc(TT * 4), F32, [TT])
    tails = [PER.view(PER.alloc(24 * 3 * 4), F32, [24, 3]) for _ in range(NL)]
    kprev = [PER.view(PER.alloc(2 * 4 * 128 * 2), BF16, [2, 4, 128]) for _ in range(NL)]
    vprev = [PER.view(PER.alloc(512 * 2), BF16, [512]) for _ in range(NL)]
    attnT = PER.view(PER.alloc(KC * TT * 2), BF16, [KC, TT])
    unT = PER.view(PER.alloc(16 * TT * 2), BF16, [16, TT])
    P.const_keys.add("cstk")
    for v in (cst, cstb, lnin, pp, fbv, vkb, vfm[0], vfm[1]):
        v.key = "cstk"
    def csub(v, a, b):
        return View(v.ap[:, a:b], v.key, v.lo, v.hi)
    identf = csub(cst, 0, 128); tri = csub(cst, 128, 256); ustr = csub(cst, 256, 384)
    onesD = csub(cst, 384, 512); onesf = csub(cst, 512, 640); sel = csub(cst, 640, 768)
    kb0 = csub(cst, 768 + 1024, 768 + 1025); kball = csub(cst, 768 + 1025, 768 + 1026)
    identb = csub(cstb, 0, 128); onesb = csub(cstb, 128, 256)
    mprev = csub(cstb, 256, 768); mcur = csub(cstb, 768, 1280)

    psum = [es.enter_context(nc.psum_tensor(f"ps{i}", [128, 512], F32)) for i in range(8)]
    pstate = {"i": 0}
    def bank(dt=F32, shape=None):
        i = pstate["i"] % 8; pstate["i"] += 1
        ap = psum[i][:, :]
        n = 512
        if dt == BF16:
            ap = ap.bitcast(BF16); n = 1024
        v = View(ap, "psum", i * 2048, (i + 1) * 2048)
        return v
    def sub(v, *idx):
        lo, hi = v.lo, v.hi
        if v.shape is not None and v.key != "psum":
            strides = []
            st_ = 1
            for d_ in reversed(v.shape):
                strides.insert(0, st_); st_ *= d_
            mn = 0; mx = 0
            for i_, d_ in enumerate(v.shape):
                ix = idx[i_ + 1] if i_ + 1 < len(idx) else slice(None)
                if isinstance(ix, int):
                    a_, b_ = ix, ix + 1
                else:
                    a_ = 0 if ix.start is None else ix.start
                    b_ = d_ if ix.stop is None else ix.stop
                mn += a_ * strides[i_]; mx += (b_ - 1) * strides[i_]
            lo = v.lo + mn * v.esz; hi = v.lo + (mx + 1) * v.esz
        return View(v.ap[idx], v.key, lo, hi)

    class TAlloc:
        def __init__(self): self.base = 0
        def reset(self, to=0): TR.top = to
        def get(self, dt, shape):
            esz = 4 if dt == F32 else 2
            return TR.view(TR.alloc(int(np.prod(shape)) * esz), dt, shape)
    T = TAlloc()

    def mm(out, lhsT, rhs, start=True, stop=True):
        return P.op("pe", lambda e: e.matmul(out.ap, lhsT.ap, rhs.ap, start=start, stop=stop),
                    reads=[lhsT, rhs] + ([] if start else []), writes=[out])
    def tp(out, in_, ident):
        return P.op("pe", lambda e: e.transpose(out.ap, in_.ap, ident.ap), reads=[in_, ident], writes=[out])
    def act(out, in_, func, bias=None, scale=1.0, accum=None):
        rd = [in_]; kw = {}
        if isinstance(bias, View): rd.append(bias); kw["bias"] = bias.ap
        elif bias is not None: kw["bias"] = float(bias)
        if isinstance(scale, View): rd.append(scale); kw["scale"] = scale.ap
        else: kw["scale"] = float(scale)
        wr = [out]
        if accum is not None: wr.append(accum); kw["accum_out"] = accum.ap
        return P.op("act", lambda e: e.activation(out.ap, in_.ap, func, **kw), reads=rd, writes=wr)
    def tt(out, a, b, op, eng="dve"):
        return P.op(eng, lambda e: e.tensor_tensor(out.ap, a.ap, b.ap, op), reads=[a, b], writes=[out])
    def ts(out, a, s1, s2, op0, op1=None, eng="dve"):
        rd = [a]
        s1a = s1.ap if isinstance(s1, View) else float(s1)
        if isinstance(s1, View): rd.append(s1)
        s2a = None
        if s2 is not None:
            s2a = s2.ap if isinstance(s2, View) else float(s2)
            if isinstance(s2, View): rd.append(s2)
        if op1 is None:
            return P.op(eng, lambda e: e.tensor_scalar(out.ap, a.ap, s1a, None, op0), reads=rd, writes=[out])
        return P.op(eng, lambda e: e.tensor_scalar(out.ap, a.ap, s1a, s2a, op0, op1), reads=rd, writes=[out])
    def stt(out, a, s, b, op0, op1, eng="dve"):
        rd = [a, b]
        sa = s.ap if isinstance(s, View) else float(s)
        if isinstance(s, View): rd.append(s)
        return P.op(eng, lambda e: e.scalar_tensor_tensor(out.ap, a.ap, sa, b.ap, op0, op1), reads=rd, writes=[out])
    def cp(out, in_, eng=None):
        if eng is None:
            P.toggle ^= 1; eng = "act" if P.toggle else "dve"
        if eng == "act":
            return act(out, in_, AF.Copy)
        return P.op("dve", lambda e: e.tensor_copy(out.ap, in_.ap), reads=[in_], writes=[out])
    def recip(out, in_):
        return P.op("dve", lambda e: e.reciprocal(out.ap, in_.ap), reads=[in_], writes=[out])
    def red(out, in_, op=ALU.add):
        return P.op("dve", lambda e: e.tensor_reduce(out.ap, in_.ap, AX.X, op), reads=[in_], writes=[out])
    def memset(v, val, eng="dve"):
        return P.op(eng, lambda e: e.memset(v.ap, val), writes=[v])
    def dma(q, out, in_, sem, rd=(), wr=()):
        return P.op(q, lambda e: e.dma_start(out=out, in_=in_), reads=list(rd), writes=list(wr), dma_sem=sem)

    wcnt = {"i": 0}
    def wload(dram_ap, shape):
        i = wcnt["i"] % NSLOT; wcnt["i"] += 1
        v = WR.view(wslots[i], BF16, shape)
        dma("pool", v.ap, dram_ap, f"w{i}", wr=[v])
        return v

    def wtile_fm(w_d, k0, nk, c0, ncols):
        return wload(w_d[k0 * 128:(k0 + nk) * 128, c0:c0 + ncols].rearrange("(c p) n -> p c n", p=128), [nk, ncols])

    dma("sp", cst.ap, cst_d, "c0", wr=[cst])
    dma("sp", lnin.ap, lnin_d, "c1", wr=[lnin])
    dma("sp", pp.ap, pp_d, "c2", wr=[pp])
    dma("sp", fbv.ap, fb_d, "c3", wr=[fbv]); dma("sp", vkb.ap, vkb_d, "c4", wr=[vkb])
    dma("sp", vfm[0].ap, vfm_d[0], "c5", wr=[vfm[0]]); dma("sp", vfm[1].ap, vfm_d[1], "c6", wr=[vfm[1]])
    gathv = View(None, "gathd", 0, 1); sendv = View(None, "sendd", 0, 1)
    P.op("dve", lambda e: e.tensor_copy(cstb.ap[:, 0:128], cst.ap[:, 0:128]), reads=[cst], writes=[cstb])
    P.op("dve", lambda e: e.tensor_copy(cstb.ap[:, 128:256], cst.ap[:, 512:640]), reads=[cst], writes=[cstb])
    P.op("dve", lambda e: e.tensor_copy(cstb.ap[:, 256:1280], cst.ap[:, 768:1792]), reads=[cst], writes=[cstb])
    for l in range(NL):
        memset(tails[l], 0.0); memset(kprev[l], 0.0); memset(vprev[l], 0.0)
        zt = T.get(F32, [2048]) if l == 0 else zt
        if l == 0:
            memset(zt, 0.0)
        dma("sp", st_d[l], zt.ap, f"st{l}", rd=[zt], wr=[View(None, f"std{l}", 0, 1)])
    stv = [View(None, f"std{l}", 0, 1) for l in range(NL)]
    for b in range(NB):
        dma("sp", gath_d[b * 128:(b + 1) * 128, :], zt.ap[:, 0:D], f"gz{b}", rd=[zt], wr=[gathv])

    def ppv(l, a, b):
        return View(pp.ap[:, l * NPP + a:l * NPP + b], pp.key, pp.lo, pp.hi)

    def ln_fm(src, g, b, t0):
        TR.top = t0
        s1 = T.get(F32, [TT]); s2 = T.get(F32, [TT]); sq = T.get(F32, [KC, TT])
        red(s1, View(src.ap.rearrange("p c t -> p t c"), src.key, src.lo, src.hi))
        act(sq, src, AF.Square)
        red(s2, View(sq.ap.rearrange("p c t -> p t c"), sq.key, sq.lo, sq.hi))
        pm = bank(); pq = bank()
        mm(pm, onesD, s1); mm(pq, onesD, s2)
        mean = T.get(F32, [TT]); var = T.get(F32, [TT]); rstd = T.get(F32, [TT])
        cp(mean, pm, eng="act")
        tt(var, mean, mean, ALU.mult)
        tt(var, pq, var, ALU.subtract)
        ts(var, var, LN_EPS, None, ALU.add)
        act(var, var, AF.Sqrt)
        recip(rstd, var)
        tmps = [T.get(F32, [TT]), T.get(F32, [TT])]
        for c in range(KC):
            tmp = tmps[c % 2]
            tt(tmp, sub(src, slice(None), c), mean, ALU.subtract)
            tt(tmp, tmp, rstd, ALU.mult)
            act(sub(hres, slice(None), c), tmp, AF.Identity, bias=sub(b, slice(None), slice(c, c + 1)),
                scale=sub(g, slice(None), slice(c, c + 1)))
            cp(sub(hbf, slice(None), c), sub(hres, slice(None), c), eng="dve")

    def proj_fm(w_d, c0, nchunks, src, nk, consume, group=4):
        for j0 in range(0, nchunks, group):
            ng = min(group, nchunks - j0)
            wt = wtile_fm(w_d, 0, nk, c0 + j0 * 128, ng * 128) if nk * ng * 128 <= 4096 else None
            for jj in range(ng):
                ps = bank()
                for k in range(nk):
                    mm(ps, sub(wt, slice(None), k, slice(jj * 128, (jj + 1) * 128)), sub(src, slice(None), k),
                       start=(k == 0), stop=(k == nk - 1))
                consume(j0 + jj, ps)

    out_sems = []
    for s in range(NSTEP):
        P.epoch = s // cfg.get("epoch_steps", 3)
        TR.top = 0
        dma("sp", cosv.ap, cs_d[s, 0], "cos", wr=[cosv])
        dma("sp", sinv.ap, cs_d[s, 1], "sin", wr=[sinv])
        for b in range(NB):
            xt = T.get(F32, [D])
            dma("sp", xt.ap, x_d[s, b], f"x{b}", wr=[xt])
            ssum = T.get(F32, [1]); ssq = T.get(F32, [1]); junk = T.get(F32, [D])
            act(junk, xt, AF.Identity, accum=ssum)
            act(junk, xt, AF.Square, accum=ssq)
            mean = T.get(F32, [1]); var = T.get(F32, [1]); nmr = T.get(F32, [1])
            ts(mean, ssum, 1.0 / D, None, ALU.mult)
            ts(var, ssq, 1.0 / D, None, ALU.mult)
            tt(nmr, mean, mean, ALU.mult)
            tt(var, var, nmr, ALU.subtract)
            ts(var, var, LN_EPS, None, ALU.add)
            act(var, var, AF.Sqrt)
            recip(var, var)
            ts(xt, xt, mean, var, ALU.subtract, ALU.mult)
            rt = T.get(F32, [D])
            dma("sp", rt.ap, gath_d[b * 128:(b + 1) * 128, :], f"r{b}", rd=[gathv], wr=[rt])
            stt(xt, rt, sub(fbv, slice(None), slice(0, 1)), xt, ALU.mult, ALU.add)
            for c0 in range(0, KC, 4):
                ps = bank()
                for c in range(4):
                    tp(sub(ps, slice(None), slice(c * 128, (c + 1) * 128)), sub(xt, slice(None), slice((c0 + c) * 128, (c0 + c + 1) * 128)), identf)
                for c in range(4):
                    act(sub(hres, slice(None), c0 + c, slice(b * 128, (b + 1) * 128)), sub(ps, slice(None), slice(c * 128, (c + 1) * 128)),
                        AF.Identity, bias=sub(lnin, slice(None), slice(8 + c0 + c, 9 + c0 + c)), scale=sub(lnin, slice(None), slice(c0 + c, c0 + c + 1)))
        cp(hbf, hres, eng="dve")

        for l, kind in enumerate(layers):
            cw = View(pp.ap[:, l * NPP:l * NPP + 96].rearrange("p (c k) -> p c k", k=4), pp.key, pp.lo, pp.hi)
            cb = ppv(l, 96, 120)
            ln1g = ppv(l, 120, 128); ln1b = ppv(l, 128, 136); ln2g = ppv(l, 136, 144); ln2b = ppv(l, 144, 152)
            dtb = ppv(l, 152, 184); alog = ppv(l, 184, 216); dsk = ppv(l, 216, 248); snk = ppv(l, 248, 264)
            gn = ppv(l, 264, 280)
            TR.top = 0
            aneg = T.get(F32, [32]); esink = T.get(F32, [16])
            act(aneg, alog, AF.Exp)
            ts(aneg, aneg, -1.0, None, ALU.mult)
            act(esink, snk, AF.Exp)
            base_top = TR.top

            qbf = T.get(BF16, [KC, TT]); kz = T.get(BF16, [2, 4, TT]); vcur = T.get(BF16, [NB, 512])
            memset(kz, 0.0)
            for (cbase, n, sbase) in ((0, 8, 8), (16, 4, 20)):
                for j0 in range(0, n, 4):
                    wa = wtile_fm(wfm_d[l], 0, KC, (cbase + j0) * 128, 512)
                    wb = wtile_fm(wfm_d[l], 0, KC, (sbase + j0) * 128, 512)
                    for jj in range(4):
                        pa = bank(); pb = bank()
                        for k in range(KC):
                            mm(pa, sub(wa, slice(None), k, slice(jj * 128, (jj + 1) * 128)), sub(hbf, slice(None), k), start=(k == 0), stop=(k == KC - 1))
                        for k in range(KC):
                            mm(pb, sub(wb, slice(None), k, slice(jj * 128, (jj + 1) * 128)), sub(hbf, slice(None), k), start=(k == 0), stop=(k == KC - 1))
                        t1 = T.get(F32, [TT]) if (cbase == 0 and j0 == 0 and jj == 0) else t1
                        t2 = T.get(F32, [TT]) if (cbase == 0 and j0 == 0 and jj == 0) else t2
                        tt(t1, pa, cosv, ALU.mult)
                        tt(t2, pb, sinv, ALU.mult)
                        j = j0 + jj
                        if cbase == 0:
                            tt(sub(qbf, slice(None), j), t1, t2, ALU.add)
                        else:
                            for half in range(2):
                                pr = slice(half * 64, (half + 1) * 64)
                                tt(View(kz.ap[pr, half, j, :], kz.key, kz.lo, kz.hi), View(t1.ap[pr, :], t1.key, t1.lo, t1.hi),
                                   View(t2.ap[pr, :], t2.key, t2.lo, t2.hi), ALU.add)
            wv = wtile_fm(wtm_d[l], 0, KC, 0, 512)
            for b in range(NB):
                ps = bank()
                for k in range(KC):
                    mm(ps, sub(hbf, slice(None), k, slice(b * 128, (b + 1) * 128)), sub(wv, slice(None), k), start=(k == 0), stop=(k == KC - 1))
                cp(sub(vcur, slice(None), b), ps)
            att_top = TR.top
            def att_iter(b, g, it):
                qs = slice(b * 128, (b + 1) * 128)
                TR.top = att_top + (it % 3) * 4096
                pts = [T.get(BF16, [512]), T.get(BF16, [512])]
                rden = T.get(F32, [512])
                pss = []
                for kb in range(2):
                    ps = bank()
                    mm(ps, identb, mcur if kb == 1 else mprev, start=True, stop=False)
                    for slot in range(4):
                        cc, half = slot // 2, slot % 2
                        if kb == 1:
                            kv = sub(kz, slice(None), half, g, qs)
                        elif b > 0:
                            kv = sub(kz, slice(None), half, g, slice((b - 1) * 128, b * 128))
                        else:
                            kv = sub(kprev[l], slice(None), half, g)
                        mm(sub(ps, slice(None), slice(slot * 128, (slot + 1) * 128)), kv, sub(qbf, slice(None), 2 * g + cc, qs), start=False, stop=(slot == 3))
                    pss.append(ps)
                yield
                for kb in range(2):
                    kbias = None
                    if s < 2:
                        if kb == 1:
                            col = s * 4 + b
                        elif b > 0:
                            col = s * 4 + b - 1
                        else:
                            col = 3 if s == 1 else None
                        kbias = kball if col is None else sub(vkb, slice(None), slice(col, col + 1))
                    act(pts[kb], pss[kb], AF.Exp, bias=kbias, scale=0.125)
                yield
                pn = bank(); pd = bank()
                for kb in range(2):
                    if kb == 1:
                        vv = sub(vcur, slice(None), b, slice(g * 128, (g + 1) * 128))
                    elif b > 0:
                        vv = sub(vcur, slice(None), b - 1, slice(g * 128, (g + 1) * 128))
                    else:
                        vv = sub(vprev[l], slice(None), slice(g * 128, (g + 1) * 128))
                    mm(pn, vv, pts[kb], start=(kb == 0), stop=(kb == 1))
                for kb in range(2):
                    mm(pd, onesb, pts[kb], start=(kb == 0), stop=(kb == 1))
                yield
                for slot in range(4):
                    cc, half = slot // 2, slot % 2
                    h = 4 * g + 2 * cc + half
                    sl = slice(slot * 128, (slot + 1) * 128)
                    ts(sub(rden, slice(None), sl), sub(pd, slice(None), sl), sub(esink, slice(None), slice(h, h + 1)), None, ALU.add)
                recip(rden, rden)
                yield
                for slot in range(4):
                    cc, half = slot // 2, slot % 2
                    pr = slice(half * 64, (half + 1) * 64)
                    sl = slice(slot * 128, (slot + 1) * 128)
                    alo = attnT.lo + ((2 * g + cc) * TT + b * 128) * 2
                    tt(View(attnT.ap[pr, 2 * g + cc, qs], attnT.key, alo, alo + 256),
                       View(pn.ap[pr, sl], pn.key, pn.lo, pn.hi), View(rden.ap[pr, sl], rden.key, rden.lo, rden.hi), ALU.mult)
            interleave([att_iter(b, g, b * 4 + g) for b in range(NB) for g in range(4)], 3)
            TR.top = att_top + 3 * 4096
            cp(kprev[l], sub(kz, slice(None), slice(None), slice(None), slice(TT - 128, TT)), eng="dve")
            cp(vprev[l], sub(vcur, slice(None), NB - 1), eng="dve")

            TR.top = base_top
            state = T.get(F32, [4, 512])
            dma("sp", state.ap, st_d[l].rearrange("p (g n) -> p g n", g=4), f"sl{l}", rd=[stv[l]], wr=[state])
            dtt = T.get(F32, [NB, 32]); dat = T.get(F32, [NB, 32]); expA = T.get(F32, [NB, 32]); cdec = T.get(F32, [NB, 32])
            wdt = wtile_fm(wtm_d[l], 0, KC, 512 + 2048, 32)
            for b in range(NB):
                ps = bank()
                for k in range(KC):
                    mm(sub(ps, slice(None), slice(0, 32)), sub(hbf, slice(None), k, slice(b * 128, (b + 1) * 128)), sub(wdt, slice(None), k), start=(k == 0), stop=(k == KC - 1))
                tt(sub(dtt, slice(None), b), sub(ps, slice(None), slice(0, 32)), dtb, ALU.add)
            act(dtt, dtt, AF.Exp)
            act(dtt, dtt, AF.Ln, bias=1.0)
            for b in range(NB):
                tt(sub(dat, slice(None), b), sub(dtt, slice(None), b), aneg, ALU.mult)
                ps = bank()
                mm(sub(ps, slice(None), slice(0, 32)), tri, sub(dat, slice(None), b))
                mm(sub(ps, slice(None), slice(32, 64)), onesf, sub(dat, slice(None), b))
                act(sub(expA, slice(None), b), sub(ps, slice(None), slice(0, 32)), AF.Exp)
                act(sub(cdec, slice(None), b), sub(ps, slice(None), slice(32, 64)), AF.Exp)
            ssd_top = TR.top
            for g in range(4):
                TR.top = ssd_top
                xg = T.get(BF16, [6, TT])
                xpre = [T.get(F32, [TT + 3]) for _ in range(2)]
                cacc = [T.get(F32, [TT]) for _ in range(2)]
                chunks = [4 * g + i for i in range(4)] + [16 + g, 20 + g]
                wx1 = wtile_fm(wfm_d[l], 0, KC, 3072 + chunks[0] * 128, 512)
                wx2 = wtile_fm(wfm_d[l], 0, KC, 3072 + chunks[4] * 128, 128)
                wx3 = wtile_fm(wfm_d[l], 0, KC, 3072 + chunks[5] * 128, 128)
                for i, c in enumerate(chunks):
                    wt = wx1 if i < 4 else (wx2 if i == 4 else wx3)
                    co = (i * 128) if i < 4 else 0
                    ps = bank()
                    for k in range(KC):
                        mm(ps, sub(wt, slice(None), k, slice(co, co + 128)), sub(hbf, slice(None), k), start=(k == 0), stop=(k == KC - 1))
                    xp = xpre[i % 2]; ca = cacc[i % 2]
                    cp(sub(xp, slice(None), slice(0, 3)), sub(tails[l], slice(None), c), eng="dve")
                    cp(sub(xp, slice(None), slice(3, TT + 3)), ps, eng="act")
                    if s < 2:
                        tt(sub(xp, slice(None), slice(3, TT + 3)), sub(xp, slice(None), slice(3, TT + 3)), vfm[s], ALU.mult)
                    cp(sub(tails[l], slice(None), c), sub(xp, slice(None), slice(TT, TT + 3)), eng="dve")
                    ts(ca, sub(xp, slice(None), slice(0, TT)), sub(cw, slice(None), c, slice(0, 1)), sub(cb, slice(None), slice(c, c + 1)), ALU.mult, ALU.add)
                    for k in range(1, 4):
                        stt(ca, sub(xp, slice(None), slice(k, TT + k)), sub(cw, slice(None), c, slice(k, k + 1)), ca, ALU.mult, ALU.add)
                    act(sub(xg, slice(None), i), ca, AF.Silu)
                    if s < 2 and i < 4:
                        tt(sub(xg, slice(None), i), sub(xg, slice(None), i), vfm[s], ALU.mult)
                wz = wtile_fm(wtm_d[l], 0, KC, 512 + g * 512, 512)
                blk_top = TR.top
                def ssd_blk(b, g=g, xg=xg, wz=wz):
                    TR.top = blk_top + (b % 2) * 24 * 1024
                    qs = slice(b * 128, (b + 1) * 128)
                    xs_t = T.get(BF16, [8, 64]); xc = T.get(BF16, [8, 64]); xd = T.get(BF16, [8, 64]); btok = T.get(BF16, [128])
                    rhsD = T.get(F32, [8, 128]); E = T.get(F32, [8, 128]); w1 = T.get(F32, [8]); mcb = T.get(F32, [128])
                    MT = T.get(BF16, [8, 128]); sbf = T.get(BF16, [512]); yv = T.get(F32, [8, 64]); xsd = T.get(F32, [8, 64])
                    sz = T.get(F32, [512]); ss = T.get(F32, [1]); ub = T.get(BF16, [512])
                    pt = bank(BF16)
                    for i in range(4):
                        tp(sub(pt, slice(None), slice(i * 128, (i + 1) * 128)), sub(xg, slice(None), i, qs), identb)
                    tp(sub(pt, slice(None), slice(512, 640)), sub(xg, slice(None), 4, qs), identb)
                    dag = View(dat.ap[:, b, g * 8:(g + 1) * 8], dat.key, dat.lo, dat.hi)
                    tt(rhsD, View(tri.ap.unsqueeze(1).to_broadcast([128, 8, 128]), tri.key, tri.lo, tri.hi),
                       View(dag.ap.unsqueeze(2).to_broadcast([128, 8, 128]), dag.key, dag.lo, dag.hi), ALU.mult)
                    pcb = bank()
                    mm(sub(pcb, slice(None), slice(0, 128)), sub(xg, slice(None), 4, qs), sub(xg, slice(None), 5, qs))
                    pz = bank()
                    for k in range(KC):
                        mm(pz, sub(hbf, slice(None), k, qs), sub(wz, slice(None), k), start=(k == 0), stop=(k == KC - 1))
                    yield
                    ptx = View(pt.ap[:, 0:512].rearrange("p (h d) -> p h d", d=64), pt.key, pt.lo, pt.hi)
                    cp(xs_t, ptx, eng="act")
                    cp(btok, sub(pt, slice(None), slice(512, 640)), eng="act")
                    act(sz, pz, AF.Silu)
                    tt(mcb, sub(pcb, slice(None), slice(0, 128)), tri, ALU.mult)
                    pseg = [bank(), bank()]
                    for hf in range(2):
                        mm(pseg[hf], ustr, View(rhsD.ap[:, hf * 4:(hf + 1) * 4, :].rearrange("p h l -> p (h l)"), rhsD.key, rhsD.lo, rhsD.hi))
                    yield
                    for hf in range(2):
                        act(View(E.ap[:, hf * 4:(hf + 1) * 4, :].rearrange("p h l -> p (h l)"), E.key, E.lo, E.hi), pseg[hf], AF.Exp)
                    yield
                    dtg = View(dtt.ap[:, b, g * 8:(g + 1) * 8], dtt.key, dtt.lo, dtt.hi)
                    tt(w1, dtg, View(E.ap[:, :, 127], E.key, E.lo, E.hi), ALU.mult)
                    tt(xc, xs_t, View(dtg.ap.unsqueeze(2).to_broadcast([128, 8, 64]), dtg.key, dtg.lo, dtg.hi), ALU.mult)
                    tt(xd, xs_t, View(w1.ap.unsqueeze(2).to_broadcast([128, 8, 64]), w1.key, w1.lo, w1.hi), ALU.mult)
                    tt(MT, E, View(mcb.ap.unsqueeze(1).to_broadcast([128, 8, 128]), mcb.key, mcb.lo, mcb.hi), ALU.mult)
                    dsg = View(dsk.ap[:, g * 8:(g + 1) * 8], dsk.key, dsk.lo, dsk.hi)
                    tt(xsd, xs_t, View(dsg.ap.unsqueeze(2).to_broadcast([128, 8, 64]), dsg.key, dsg.lo, dsg.hi), ALU.mult)
                    yield
                    py = bank()
                    for hh in range(8):
                        mm(sub(py, slice(None), slice(hh * 64, (hh + 1) * 64)), sub(MT, slice(None), hh), sub(xc, slice(None), hh))
                    pst = bank()
                    mm(pst, btok, View(xd.ap.rearrange("p h d -> p (h d)"), xd.key, xd.lo, xd.hi))
                    yield
                    po = bank()
                    cp(sbf, sub(state, slice(None), g), eng="dve")
                    mm(po, sub(xg, slice(None), 5, qs), sbf)
                    cdg = View(cdec.ap[:, b, g * 8:(g + 1) * 8], cdec.key, cdec.lo, cdec.hi)
                    sgv = sub(state, slice(None), g)
                    sg3 = View(state.ap[:, g, :].rearrange("p (h d) -> p h d", d=64), state.key, sgv.lo, sgv.hi)
                    tt(sg3, sg3, View(cdg.ap.unsqueeze(2).to_broadcast([128, 8, 64]), cdg.key, cdg.lo, cdg.hi), ALU.mult)
                    tt(sgv, sgv, pst, ALU.add)
                    yield
                    eag = View(expA.ap[:, b, g * 8:(g + 1) * 8], expA.key, expA.lo, expA.hi)
                    po3 = View(po.ap.rearrange("p (h d) -> p h d", d=64), po.key, po.lo, po.hi)
                    py3 = View(py.ap.rearrange("p (h d) -> p h d", d=64), py.key, py.lo, py.hi)
                    tt(yv, po3, View(eag.ap.unsqueeze(2).to_broadcast([128, 8, 64]), eag.key, eag.lo, eag.hi), ALU.mult)
                    tt(yv, yv, py3, ALU.add)
                    tt(yv, yv, xsd, ALU.add)
                    y2 = View(yv.ap.rearrange("p h d -> p (h d)"), yv.key, yv.lo, yv.hi)
                    tt(y2, y2, sz, ALU.mult)
                    yield
                    act(sz, y2, AF.Square, accum=ss)
                    ts(ss, ss, 1.0 / 512, RMS_EPS, ALU.mult, ALU.add)
                    act(ss, ss, AF.Sqrt)
                    recip(ss, ss)
                    ts(ub, y2, ss, None, ALU.mult)
                    yield
                    pu = bank(BF16)
                    for i in range(4):
                        tp(sub(pu, slice(None), slice(i * 128, (i + 1) * 128)), sub(ub, slice(None), slice(i * 128, (i + 1) * 128)), identb)
                    yield
                    for i in range(4):
                        act(sub(unT, slice(None), 4 * g + i, qs), sub(pu, slice(None), slice(i * 128, (i + 1) * 128)), AF.Identity,
                            scale=sub(gn, slice(None), slice(4 * g + i, 4 * g + i + 1)))
                interleave([ssd_blk(b) for b in range(NB)], 2)
            dma("sp", st_d[l].rearrange("p (g n) -> p g n", g=4), state.ap, f"ss{l}", rd=[state], wr=[stv[l]])

            TR.top = base_top
            mT = T.get(BF16, [KC, TT])
            for c in range(KC):
                w1t = wload(wmg_d[l][c, :, 0:24, :], [24, 128])
                w2t = wload(wmg_d[l][c, :, 24:40, :], [16, 128])
                pa = bank(); psd = bank(); pga = bank(); pgs = bank()
                for k in range(KC):
                    mm(pa, sub(w1t, slice(None), k), sub(attnT, slice(None), k), start=(k == 0), stop=(k == KC - 1))
                for k in range(16):
                    mm(psd, sub(w1t, slice(None), 8 + k), sub(unT, slice(None), k), start=(k == 0), stop=(k == 15))
                for k in range(KC):
                    mm(pga, sub(w2t, slice(None), k), sub(hbf, slice(None), k), start=(k == 0), stop=(k == KC - 1))
                for k in range(KC):
                    mm(pgs, sub(w2t, slice(None), 8 + k), sub(hbf, slice(None), k), start=(k == 0), stop=(k == KC - 1))
                if c == 0:
                    sa = T.get(F32, [TT]); sb_ = T.get(F32, [TT])
                act(sa, pga, AF.Sigmoid); act(sb_, pgs, AF.Sigmoid)
                tt(sa, sa, pa, ALU.mult); tt(sb_, sb_, psd, ALU.mult)
                tt(sub(mT, slice(None), c), sa, sb_, ALU.add)
            for c in range(KC):
                wo = wload(wo_d[l][c], [KC, 128])
                po = bank()
                for k in range(KC):
                    mm(po, sub(wo, slice(None), k), sub(mT, slice(None), k), start=(k == 0), stop=(k == KC - 1))
                stt(sub(hres, slice(None), c), sub(hres, slice(None), c), ALPHA, po, ALU.mult, ALU.add)
            ln_fm(hres, ln1g, ln1b, TR.top)

            TR.top = base_top
            if kind == "dense":
                NF = DFF // 128
                actb = T.get(BF16, [NF, TT])
                tus = [T.get(F32, [TT]), T.get(F32, [TT])]
                for j0 in range(0, NF, 4):
                    ng = min(4, NF - j0)
                    wgt = wtile_fm(wg_d[l][0], 0, KC, j0 * 128, ng * 128)
                    wut = wtile_fm(wu_d[l][0], 0, KC, j0 * 128, ng * 128)
                    for jj in range(ng):
                        pg = bank(); pu = bank()
                        for k in range(KC):
                            mm(pg, sub(wgt, slice(None), k, slice(jj * 128, (jj + 1) * 128)), sub(hbf, slice(None), k), start=(k == 0), stop=(k == KC - 1))
                        for k in range(KC):
                            mm(pu, sub(wut, slice(None), k, slice(jj * 128, (jj + 1) * 128)), sub(hbf, slice(None), k), start=(k == 0), stop=(k == KC - 1))
                        tq = tus[(j0 + jj) % 2]
                        act(tq, pg, AF.Silu)
                        tt(sub(actb, slice(None), j0 + jj), tq, pu, ALU.mult)
                for c0 in range(0, KC, 4):
                    pos = [bank() for _ in range(4)]
                    for k0 in range(0, NF, 8):
                        nk = min(8, NF - k0)
                        wdt_ = wtile_fm(wd_d[l][0], k0, nk, c0 * 128, 512)
                        for c in range(4):
                            for k in range(nk):
                                mm(pos[c], sub(wdt_, slice(None), k, slice(c * 128, (c + 1) * 128)), sub(actb, slice(None), k0 + k),
                                   start=(k0 + k == 0), stop=(k0 + k == NF - 1))
                    for c in range(4):
                        stt(sub(hres, slice(None), c0 + c), sub(hres, slice(None), c0 + c), ALPHA, pos[c], ALU.mult, ALU.add)
            else:
                NF = DFFE // 128
                actb = T.get(BF16, [NF, TT]); tu = T.get(F32, [TT]); tus = [tu, T.get(F32, [TT])]
                gateb = T.get(F32, [NE, TT])
                wrt = T.get(F32, [KC, NE])
                dma("sp", wrt.ap, wr_d[l].rearrange("(c p) e -> p c e", p=128), "wr", wr=[wrt])
                for b in range(NB):
                    qs = slice(b * 128, (b + 1) * 128)
                    ps = bank()
                    for k in range(KC):
                        mm(sub(ps, slice(None), slice(0, NE)), sub(hres, slice(None), k, qs), sub(wrt, slice(None), k), start=(k == 0), stop=(k == KC - 1))
                    lg = T.get(F32, [NE]); m1 = T.get(F32, [1]); m2 = T.get(F32, [1]); msk = T.get(F32, [NE]); gt = T.get(F32, [NE]); dg = T.get(F32, [128])
                    cp(lg, sub(ps, slice(None), slice(0, NE)), eng="dve")
                    red(m1, lg, ALU.max)
                    ts(msk, lg, m1, NEG, ALU.is_ge, ALU.mult)
                    tt(msk, msk, lg, ALU.add)
                    red(m2, msk, ALU.max)
                    ts(gt, lg, m1, None, ALU.subtract)
                    act(gt, gt, AF.Exp)
                    ts(msk, lg, m2, None, ALU.is_ge)
                    tt(gt, gt, msk, ALU.mult)
                    red(m1, gt, ALU.add)
                    recip(m1, m1)
                    ts(gt, gt, m1, None, ALU.mult)
                    for e_ in range(NE):
                        ts(dg, identf, sub(gt, slice(None), slice(e_, e_ + 1)), None, ALU.mult)
                        pb_ = bank()
                        mm(sub(pb_, slice(None), slice(0, 128)), onesf, dg)
                        cp(sub(gateb, slice(None), e_, qs), sub(pb_, slice(None), slice(0, 128)))
                ts(hres, hres, ALPHA, None, ALU.mult)
                for e_ in range(NE):
                    for j0 in range(0, NF, 4):
                        ng = min(4, NF - j0)
                        wgt = wtile_fm(wg_d[l][e_], 0, KC, j0 * 128, ng * 128)
                        wut = wtile_fm(wu_d[l][e_], 0, KC, j0 * 128, ng * 128)
                        for jj in range(ng):
                            pg = bank(); pu = bank()
                            for k in range(KC):
                                mm(pg, sub(wgt, slice(None), k, slice(jj * 128, (jj + 1) * 128)), sub(hbf, slice(None), k), start=(k == 0), stop=(k == KC - 1))
                            for k in range(KC):
                                mm(pu, sub(wut, slice(None), k, slice(jj * 128, (jj + 1) * 128)), sub(hbf, slice(None), k), start=(k == 0), stop=(k == KC - 1))
                            tq = tus[(j0 + jj) % 2]
                            act(tq, pg, AF.Silu)
                            tt(sub(actb, slice(None), j0 + jj), tq, pu, ALU.mult)
                    for c in range(KC):
                        wds = [wload(wd_d[l][e_, c, :, k0:k0 + 14, :], [14, 128]) for k0 in range(0, NF, 14)]
                        po = bank()
                        for k in range(NF):
                            mm(po, sub(wds[k // 14], slice(None), k % 14), sub(actb, slice(None), k), start=(k == 0), stop=(k == NF - 1))
                        tt(tu, po, sub(gateb, slice(None), e_), ALU.mult)
                        tt(sub(hres, slice(None), c), sub(hres, slice(None), c), tu, ALU.add)
            ln_fm(hres, ln2g, ln2b, TR.top)

        TR.top = 0
        for b in range(NB):
            ot = T.get(F32, [D])
            for c0 in range(0, KC, 4):
                ps = bank()
                for c in range(4):
                    tp(sub(ps, slice(None), slice(c * 128, (c + 1) * 128)), sub(hres, slice(None), c0 + c, slice(b * 128, (b + 1) * 128)), identf)
                cp(sub(ot, slice(None), slice(c0 * 128, (c0 + 4) * 128)), ps)
            o = dma("sp", y_d[s, b], ot.ap, f"o{b}", rd=[ot], wr=[])
            out_sems.append(o)
            if s < NSTEP - 1:
                dma("sp", send_d[b * 128:(b + 1) * 128, :], ot.ap, f"sd{b}", rd=[ot], wr=[sendv])
        if s < NSTEP - 1:
            cco = P.op("pool", lambda e: e.collective_compute("AllGather", ALU.bypass, replica_groups=[[0, 1], [2, 3], [4, 5], [6, 7]],
                                                           ins=[send_d.opt()], outs=[gath_d.opt()]), reads=[sendv], writes=[gathv])
            cco.noval = True

    def final_waits():
        last = {}
        for o in out_sems:
            last[id(o.sem)] = (o.sem, o.semval)
        return list(last.values())
    P.emit(final_waits)
    return nc, es


def _consts():
    c = np.zeros((128, 6 * 128 + 1024 + 2), np.float32)
    i = np.arange(128)
    c[:, 0:128] = np.eye(128)
    c[:, 128:256] = (i[:, None] <= i[None, :])
    c[:, 256:384] = (i[:, None] > i[None, :])
    c[:, 384:512] = 1.0 / D
    c[:, 512:640] = 1.0
    mprev = np.where(i[:, None] > i[None, :], 0.0, NEG)
    mcur = np.where(i[:, None] <= i[None, :], 0.0, NEG)
    c[:, 768:768 + 512] = np.tile(mprev, (1, 4))
    c[:, 768 + 512:768 + 1024] = np.tile(mcur, (1, 4))
    c[:, 768 + 1024] = np.where(i < PAD, NEG, 0.0)
    c[:, 768 + 1025] = NEG
    return c


def _cossin(nstep):
    T_ = nstep * TT
    pos = (np.arange(T_) - PAD).astype(np.float32)
    inv = (np.float32(10000.0) ** (-np.arange(0, 64, 2, dtype=np.float32) / np.float32(64))).astype(np.float32)
    ang = (pos[:, None] * inv[None, :]).astype(np.float32)
    cos = np.cos(ang).astype(np.float32); sin = np.sin(ang).astype(np.float32)
    p = np.arange(128)
    f = p % 32
    sign = np.where((p % 64) < 32, -1.0, 1.0).astype(np.float32)
    cosT = cos[:, f].T
    sinT = sin[:, f].T * sign[:, None]
    out = np.zeros((nstep, 2, 128, TT), np.float32)
    for s in range(nstep):
        out[s, 0] = cosT[:, s * TT:(s + 1) * TT]
        out[s, 1] = sinT[:, s * TT:(s + 1) * TT]
    return out


def _fm(v, n):
    return np.ascontiguousarray(v.reshape(n, 128).T)


def _layer_inputs(inp, l, li, kind, m):
    w_in = inp["w_in"][l]
    q = w_in[:, 2048:3072]; k = w_in[:, 3072:3328]; v = w_in[:, 3328:3584]
    z = w_in[:, 3584:5632]; xbc = w_in[:, 5632:8704]; dtw = w_in[:, 8704:8736]
    ga = w_in[:, 0:1024]; gs = w_in[:, 1024:2048]
    sw = (np.arange(64) + 32) % 64
    qs = q.reshape(D, 16, 64)[:, :, sw].reshape(D, 1024)
    ks = k.reshape(D, 4, 64)[:, :, sw].reshape(D, 256)
    dup = lambda a: np.repeat(a.reshape(D, 4, 1, 64), 2, axis=2).reshape(D, 512)
    m[f"wfm{li}"] = np.ascontiguousarray(np.concatenate([q, qs, dup(k), dup(ks), xbc], axis=1))
    def cpk(W):
        return W.reshape(W.shape[0] // 128, 128, 8, 128).transpose(2, 1, 0, 3)
    m[f"wmg{li}"] = np.ascontiguousarray(np.concatenate([cpk(inp["w_attn_out"][l]), cpk(inp["w_ssd_out"][l]), cpk(ga), cpk(gs)], axis=2))
    m[f"wtm{li}"] = np.ascontiguousarray(np.concatenate([dup(v), z, dtw], axis=1))
    m[f"wo{li}"] = np.ascontiguousarray(cpk(inp["w_o"][l]))
    if kind == "dense":
        m[f"wg{li}"] = inp["ffn_wg"][l // 2][None]; m[f"wu{li}"] = inp["ffn_wu"][l // 2][None]; m[f"wd{li}"] = inp["ffn_wd"][l // 2][None]
    else:
        m[f"wg{li}"] = inp["moe_wg"][l // 2]; m[f"wu{li}"] = inp["moe_wu"][l // 2]; m[f"wd{li}"] = np.ascontiguousarray(inp["moe_wd"][l // 2].reshape(NE, DFFE // 128, 128, 8, 128).transpose(0, 3, 2, 1, 4))
        m[f"wr{li}"] = inp["moe_router"][l // 2]
    pp = np.zeros((128, NPP), np.float32)
    pp[:, 0:96] = np.transpose(inp["conv_w"][l].reshape(4, 24, 128), (2, 1, 0)).reshape(128, 96)
    pp[:, 96:120] = _fm(inp["conv_b"][l], 24)
    pp[:, 120:128] = _fm(inp["ln1_g"][l], 8); pp[:, 128:136] = _fm(inp["ln1_b"][l], 8)
    pp[:, 136:144] = _fm(inp["ln2_g"][l], 8); pp[:, 144:152] = _fm(inp["ln2_b"][l], 8)
    pp[:, 152:184] = inp["dt_bias"][l][None, :]; pp[:, 184:216] = inp["a_log"][l][None, :]
    pp[:, 216:248] = inp["d_skip"][l][None, :]; pp[:, 248:264] = inp["sinks"][l][None, :]
    pp[:, 264:280] = _fm(inp["ssd_norm_g"][l], 16)
    return pp


def _masks(valid2):
    vfm = np.ascontiguousarray(np.broadcast_to(valid2[:, None, :], (2, 128, TT))).astype(np.float32)
    vkb = np.ascontiguousarray(((valid2.reshape(2, NB, 128) - 1.0) * (-NEG)).transpose(2, 0, 1).reshape(128, 8)).astype(np.float32)
    return vfm, vkb


def make_inputs(inp, stages, nstep, seqs):
    nst = len(stages)
    stage_shared = []
    kinds = None
    for layer_ids in stages:
        kinds = ["dense" if l % 2 == 0 else "moe" for l in layer_ids]
        sh = {}
        pps = [_layer_inputs(inp, l, li, kinds[li], sh) for li, l in enumerate(layer_ids)]
        sh["pp"] = np.ascontiguousarray(np.concatenate(pps, axis=1))
        sh["cst"] = _consts()
        sh["cossin"] = None
        stage_shared.append(sh)
    cs = _cossin(nstep)
    lnin_real = np.ascontiguousarray(np.concatenate([_fm(inp["ln_in_g"], 8), _fm(inp["ln_in_b"], 8)], axis=1))
    lnin_id = np.ascontiguousarray(np.concatenate([np.ones((128, 8), np.float32), np.zeros((128, 8), np.float32)], axis=1))
    vpad = np.ones(TT, np.float32); vpad[:PAD] = 0.0
    maps = []
    Tn = nstep * TT
    for b in seqs:
        for si in range(nst):
            m = dict(stage_shared[si])
            xs = np.zeros((Tn, D), np.float32)
            valid2 = np.ones((2, TT), np.float32)
            if si == 0:
                xs[PAD:PAD + NMETA] = inp["meta_tokens"]
                n = min(Tn - 128 - (nst - 1) * TT, inp["x"].shape[1])
                xs[128:128 + n] = inp["x"][b, :n]
                valid2[0] = vpad
                m["lnin"] = lnin_real; m["fb"] = np.zeros((128, 1), np.float32)
                m["cossin"] = cs
            else:
                valid2[0] = 0.0; valid2[1] = vpad
                m["lnin"] = lnin_id; m["fb"] = np.ones((128, 1), np.float32)
                cs1 = np.zeros_like(cs); cs1[1:] = cs[:-1]; cs1[0] = cs[0]
                m["cossin"] = cs1
            m["vfm"], m["vkb"] = _masks(valid2)
            m["x"] = xs.reshape(nstep, NB, 128, D)
            maps.append(m)
    return maps, kinds


NTILE_FULL = 17
STAGES = [[0, 1], [2, 3]]


def kernel(**inputs):
    inp = {k: np.asarray(v) for k, v in inputs.items()}
    nstep = NTILE_FULL + len(STAGES) - 1
    maps, kinds = make_inputs(inp, STAGES, nstep, list(range(4)))
    nc, es = build(dict(layers=kinds, nstep=nstep))
    with es:
        res = run_bass_kernel_spmd(nc, maps, core_ids=list(range(8)))
    S = inp["x"].shape[1]
    outs = []
    for b in range(4):
        y = res.results[2 * b + 1]["y"].reshape(nstep, TT, D)[1:].reshape(NTILE_FULL * TT, D)
        outs.append(y[128:128 + S])
    return np.stack(outs, axis=0).astype(np.float32)
```

```python
import numpy as np
from contextlib import ExitStack
import concourse.bass as bass
import concourse.mybir as mybir
from concourse.bass_utils import run_bass_kernel_spmd

F32 = mybir.dt.float32
BF16 = mybir.dt.bfloat16
AF = mybir.ActivationFunctionType
ALU = mybir.AluOpType
AX = mybir.AxisListType

D = 1024; KC = 8; TT = 512; NB = 4; BLK = 128
DEPTH = 4; PAD = 112; NMETA = 16
DFF = 2816; DFFE = 3584; NE = 8
ALPHA = (2 * DEPTH) ** 0.25
LN_EPS = 1e-5; RMS_EPS = 1e-5
NEG = -30000.0
PAGE = 512
NPP = 288


class View:
    def __init__(self, ap, key, lo, hi, shape=None, esz=None):
        self.ap = ap; self.key = key; self.lo = lo; self.hi = hi; self.shape = shape; self.esz = esz

    def pages(self):
        if self.key is None:
            return []
        return [(self.key, p) for p in range(self.lo // PAGE, (self.hi - 1) // PAGE + 1)]


class Op:
    __slots__ = ("eng", "fn", "deps", "idx", "inc", "is_dma", "sem", "semval", "has_dep", "epoch", "noval")

    def __init__(self, eng, fn):
        self.eng = eng; self.fn = fn; self.deps = []; self.inc = None
        self.is_dma = False; self.sem = None; self.semval = 0; self.has_dep = False; self.noval = False


class Prog:
    ENGS = ("pe", "act", "dve", "pool", "sp")

    def __init__(self, nc, es):
        self.nc = nc; self.es = es
        self.q = {e: [] for e in self.ENGS}
        self.pw = {}; self.pr = {}
        self.dsem = {}; self.dcnt = {}
        self.toggle = 0
        self.const_keys = set()
        self.epoch = 0

    def op(self, eng, fn, reads=(), writes=(), dma_sem=None):
        o = Op(eng, fn); o.epoch = self.epoch
        deps = {}
        def add(p):
            if p is None or p is o:
                return
            deps[id(p)] = p
        for v in reads:
            for pg in v.pages():
                add(self.pw.get(pg))
        for v in writes:
            for pg in v.pages():
                add(self.pw.get(pg))
                for r in self.pr.get(pg, ()):
                    add(r)
        for v in writes:
            for pg in v.pages():
                self.pw[pg] = o; self.pr[pg] = []
        for v in reads:
            if v.key in self.const_keys:
                continue
            for pg in v.pages():
                self.pr.setdefault(pg, []).append(o)
        best = {}
        for p in deps.values():
            if p.is_dma:
                best[("d", id(p))] = p
            else:
                if p.eng == o.eng and (o.eng == "pe"):
                    continue
                b = best.get(p.eng)
                if b is None or p.idx > b.idx:
                    best[p.eng] = p
        o.deps = list(best.values())
        for p in o.deps:
            p.has_dep = True
        if dma_sem is not None:
            o.is_dma = True
            if dma_sem[0] == "w":
                dma_sem = dma_sem + "_%d" % self.epoch
            if dma_sem not in self.dsem:
                self.dsem[dma_sem] = self.es.enter_context(self.nc.semaphore("d_" + dma_sem))
                self.dcnt[dma_sem] = 0
            self.dcnt[dma_sem] += 16
            o.sem = self.dsem[dma_sem]; o.semval = self.dcnt[dma_sem]
        o.idx = len(self.q[eng])
        self.q[eng].append(o)
        return o

    def emit(self, final_waits):
        nc = self.nc
        esem = {}
        for e in self.ENGS:
            c = {}
            for o in self.q[e]:
                if o.is_dma:
                    continue
                if o.has_dep:
                    c[o.epoch] = c.get(o.epoch, 0) + 1; o.inc = c[o.epoch]
                    if (e, o.epoch) not in esem:
                        esem[(e, o.epoch)] = self.es.enter_context(nc.semaphore("e_%s_%d" % (e, o.epoch)))
        def run(e, eng):
            waited = {}
            for o in self.q[e]:
                for p in o.deps:
                    if p.is_dma:
                        s, v = p.sem, p.semval
                    else:
                        s, v = esem[(p.eng, p.epoch)], p.inc
                    k = id(s)
                    if waited.get(k, 0) >= v:
                        continue
                    waited[k] = v
                    eng.wait_ge(s, v)
                ins = o.fn(eng)
                if o.is_dma:
                    ins.then_inc(o.sem, 16)
                elif o.inc is not None:
                    if o.noval:
                        ins.then_inc(esem[(e, o.epoch)])
                    else:
                        ins.then_inc(esem[(e, o.epoch)], 1)
            if e == "sp":
                for (s, v) in final_waits():
                    eng.wait_ge(s, v)
        with nc.Block() as block:
            @block.tensor
            def _(eng): run("pe", eng)
            @block.scalar
            def _(eng): run("act", eng)
            @block.vector
            def _(eng): run("dve", eng)
            @block.gpsimd
            def _(eng): run("pool", eng)
            @block.sync
            def _(eng): run("sp", eng)


class Arena:
    def __init__(self, nc, es, name, nbytes, dt=F32):
        self.t = es.enter_context(nc.sbuf_tensor(name, [128, nbytes // 4], F32))
        self.key = name; self.nbytes = nbytes; self.top = 0

    def alloc(self, nbytes):
        o = self.top; self.top += (nbytes + 63) // 64 * 64
        assert self.top <= self.nbytes, (self.key, self.top, self.nbytes)
        return o

    def view(self, off, dt, shape, part=(0, 128)):
        esz = 4 if dt == F32 else 2
        n = int(np.prod(shape))
        ap = self.t[part[0]:part[1], off // 4:(off + n * esz) // 4]
        if dt != F32:
            ap = ap.bitcast(dt)
        if len(shape) == 2:
            ap = ap.rearrange("p (a b) -> p a b", a=shape[0])
        elif len(shape) == 3:
            ap = ap.rearrange("p (a b c) -> p a b c", a=shape[0], b=shape[1])
        return View(ap, self.key, off, off + n * esz, shape=list(shape), esz=esz)


def interleave(gens, width):
    gens = list(gens); active = []
    while gens or active:
        while len(active) < width and gens:
            active.append(gens.pop(0))
        for g_ in list(active):
            try:
                next(g_)
            except StopIteration:
                active.remove(g_)


def build(cfg):
    layers = cfg["layers"]; NL = len(layers); NSTEP = cfg["nstep"]
    nc = bass.Bass("TRN2", target_bir_lowering=False)
    es = ExitStack()
    P = Prog(nc, es)

    def din(name, shape, dt=F32):
        return nc.dram_tensor(name, list(shape), dt, kind="ExternalInput").ap()

    x_d = din("x", [NSTEP, NB, 128, D])
    cs_d = din("cossin", [NSTEP, 2, 128, TT])
    cst_d = din("cst", [128, 6 * 128 + 2 * 512 + 2])
    fb_d = din("fb", [128, 1]); vfm_d = din("vfm", [2, 128, TT]); vkb_d = din("vkb", [128, 8])
    send_d = nc.dram_tensor("send", [TT, D], F32, kind="Internal").ap()
    gath_d = nc.dram_tensor("gath", [2 * TT, D], F32, kind="Internal").ap()
    lnin_d = din("lnin", [128, 16])
    pp_d = din("pp", [128, NL * NPP])
    wup_d = [din(f"wup{l}", [15, 128, KC, 512]) for l in range(NL)]
    wbc_d = [din(f"wbc{l}", [4, 128, KC, 256]) for l in range(NL)]
    wdtw_d = [din(f"wdtw{l}", [128, KC, 32]) for l in range(NL)]
    wmg_d = [din(f"wmg{l}", [KC, 128, 40, 128]) for l in range(NL)]
    wo_d = [din(f"wo{l}", [KC, 128, KC, 128]) for l in range(NL)]
    wg_d = []; wu_d = []; wd_d = []; wr_d = []
    for l, kind in enumerate(layers):
        if kind == "dense":
            wg_d.append(din(f"wg{l}", [1, DFF // 256, 128, KC, 256])); wu_d.append(din(f"wu{l}", [1, DFF // 256, 128, KC, 256]))
            wd_d.append(din(f"wd{l}", [2, 128, DFF // 128, 512])); wr_d.append(None)
        else:
            wg_d.append(din(f"wg{l}", [NE, DFFE // 512, 128, KC, 512])); wu_d.append(din(f"wu{l}", [NE, DFFE // 512, 128, KC, 512]))
            wd_d.append(din(f"wd{l}", [NE, KC, 128, DFFE // 128, 128])); wr_d.append(din(f"wr{l}", [D, NE]))
    y_d = nc.dram_tensor("y", [NSTEP, NB, 128, D], F32, kind="ExternalOutput").ap()
    st_d = [nc.dram_tensor(f"state{l}", [128, 2048], F32, kind="Internal").ap() for l in range(NL)]

    PER = Arena(nc, es, "per", 84 * 1024)
    WR = Arena(nc, es, "wring", 32 * 1024)
    TR = Arena(nc, es, "trans", 88 * 1024)
    NSLOT = 4
    wslots = [WR.alloc(8192) for _ in range(NSLOT)]
    hres = PER.view(PER.alloc(KC * TT * 4), F32, [KC, TT])
    hbf = PER.view(PER.alloc(KC * TT * 2), BF16, [KC, TT])
    cst = PER.view(PER.alloc((6 * 128 + 1024 + 2) * 4), F32, [6 * 128 + 1024 + 2])
    fbv = PER.view(PER.alloc(64), F32, [1]); vkb = PER.view(PER.alloc(64), F32, [8])
    vfm = [PER.view(PER.alloc(TT * 4), F32, [TT]) for _ in range(2)]
    cstb = PER.view(PER.alloc((2 * 128 + 1024) * 2), BF16, [2 * 128 + 1024])
    lnin = PER.view(PER.alloc(64), F32, [16])
    pp = PER.view(PER.alloc(NL * NPP * 4), F32, [NL * NPP])
    cosv = PER.view(PER.alloc(TT * 4), F32, [TT]); sinv = PER.view(PER.alloc(TT * 4), F32, [TT])
    tails = [PER.view(PER.alloc(24 * 3 * 4), F32, [24, 3]) for _ in range(NL)]
    kprev = [PER.view(PER.alloc(2 * 4 * 128 * 2), BF16, [2, 4, 128]) for _ in range(NL)]
    vprev = [PER.view(PER.alloc(512 * 2), BF16, [512]) for _ in range(NL)]
    attnT = PER.view(PER.alloc(KC * TT * 2), BF16, [KC, TT])
    unT = PER.view(PER.alloc(16 * TT * 2), BF16, [16, TT])
    P.const_keys.add("cstk")
    for v in (cst, cstb, lnin, pp, fbv, vkb, vfm[0], vfm[1]):
        v.key = "cstk"
    def csub(v, a, b):
        return View(v.ap[:, a:b], v.key, v.lo, v.hi)
    identf = csub(cst, 0, 128); tri = csub(cst, 128, 256); ustr = csub(cst, 256, 384)
    onesD = csub(cst, 384, 512); onesf = csub(cst, 512, 640); sel = csub(cst, 640, 768)
    kb0 = csub(cst, 768 + 1024, 768 + 1025); kball = csub(cst, 768 + 1025, 768 + 1026)
    identb = csub(cstb, 0, 128); onesb = csub(cstb, 128, 256)
    mprev = csub(cstb, 256, 768); mcur = csub(cstb, 768, 1280)

    psum = [es.enter_context(nc.psum_tensor(f"ps{i}", [128, 512], F32)) for i in range(8)]
    pstate = {"i": 0}
    def bank(dt=F32, shape=None):
        i = pstate["i"] % 8; pstate["i"] += 1
        ap = psum[i][:, :]
        n = 512
        if dt == BF16:
            ap = ap.bitcast(BF16); n = 1024
        v = View(ap, "psum", i * 2048, (i + 1) * 2048)
        return v
    def sub(v, *idx):
        lo, hi = v.lo, v.hi
        if v.shape is not None and v.key != "psum":
            strides = []
            st_ = 1
            for d_ in reversed(v.shape):
                strides.insert(0, st_); st_ *= d_
            mn = 0; mx = 0
            for i_, d_ in enumerate(v.shape):
                ix = idx[i_ + 1] if i_ + 1 < len(idx) else slice(None)
                if isinstance(ix, int):
                    a_, b_ = ix, ix + 1
                else:
                    a_ = 0 if ix.start is None else ix.start
                    b_ = d_ if ix.stop is None else ix.stop
                mn += a_ * strides[i_]; mx += (b_ - 1) * strides[i_]
            lo = v.lo + mn * v.esz; hi = v.lo + (mx + 1) * v.esz
        return View(v.ap[idx], v.key, lo, hi)

    class TAlloc:
        def __init__(self): self.base = 0
        def reset(self, to=0): TR.top = to
        def get(self, dt, shape):
            esz = 4 if dt == F32 else 2
            return TR.view(TR.alloc(int(np.prod(shape)) * esz), dt, shape)
    T = TAlloc()

    def mm(out, lhsT, rhs, start=True, stop=True):
        return P.op("pe", lambda e: e.matmul(out.ap, lhsT.ap, rhs.ap, start=start, stop=stop),
                    reads=[lhsT, rhs] + ([] if start else []), writes=[out])
    def tp(out, in_, ident):
        return P.op("pe", lambda e: e.transpose(out.ap, in_.ap, ident.ap), reads=[in_, ident], writes=[out])
    def act(out, in_, func, bias=None, scale=1.0, accum=None):
        rd = [in_]; kw = {}
        if isinstance(bias, View): rd.append(bias); kw["bias"] = bias.ap
        elif bias is not None: kw["bias"] = float(bias)
        if isinstance(scale, View): rd.append(scale); kw["scale"] = scale.ap
        else: kw["scale"] = float(scale)
        wr = [out]
        if accum is not None: wr.append(accum); kw["accum_out"] = accum.ap
        return P.op("act", lambda e: e.activation(out.ap, in_.ap, func, **kw), reads=rd, writes=wr)
    def tt(out, a, b, op, eng="dve"):
        return P.op(eng, lambda e: e.tensor_tensor(out.ap, a.ap, b.ap, op), reads=[a, b], writes=[out])
    def ts(out, a, s1, s2, op0, op1=None, eng="dve"):
        rd = [a]
        s1a = s1.ap if isinstance(s1, View) else float(s1)
        if isinstance(s1, View): rd.append(s1)
        s2a = None
        if s2 is not None:
            s2a = s2.ap if isinstance(s2, View) else float(s2)
            if isinstance(s2, View): rd.append(s2)
        if op1 is None:
            return P.op(eng, lambda e: e.tensor_scalar(out.ap, a.ap, s1a, None, op0), reads=rd, writes=[out])
        return P.op(eng, lambda e: e.tensor_scalar(out.ap, a.ap, s1a, s2a, op0, op1), reads=rd, writes=[out])
    def stt(out, a, s, b, op0, op1, eng="dve"):
        rd = [a, b]
        sa = s.ap if isinstance(s, View) else float(s)
        if isinstance(s, View): rd.append(s)
        return P.op(eng, lambda e: e.scalar_tensor_tensor(out.ap, a.ap, sa, b.ap, op0, op1), reads=rd, writes=[out])
    def cp(out, in_, eng=None):
        if eng is None:
            P.toggle ^= 1; eng = "act" if P.toggle else "dve"
        if eng == "act":
            return act(out, in_, AF.Copy)
        return P.op("dve", lambda e: e.tensor_copy(out.ap, in_.ap), reads=[in_], writes=[out])
    def recip(out, in_):
        return P.op("dve", lambda e: e.reciprocal(out.ap, in_.ap), reads=[in_], writes=[out])
    def red(out, in_, op=ALU.add):
        return P.op("dve", lambda e: e.tensor_reduce(out.ap, in_.ap, AX.X, op), reads=[in_], writes=[out])
    def memset(v, val, eng="dve"):
        return P.op(eng, lambda e: e.memset(v.ap, val), writes=[v])
    def dma(q, out, in_, sem, rd=(), wr=()):
        return P.op(q, lambda e: e.dma_start(out=out, in_=in_), reads=list(rd), writes=list(wr), dma_sem=sem)

    wcnt = {"i": 0}
    def wload(dram_ap, shape):
        i = wcnt["i"] % NSLOT; wcnt["i"] += 1
        v = WR.view(wslots[i], BF16, shape)
        dma("pool", v.ap, dram_ap, f"w{i}", wr=[v])
        return v

    def wtile_fm(w_d, k0, nk, c0, ncols):
        return wload(w_d[k0 * 128:(k0 + nk) * 128, c0:c0 + ncols].rearrange("(c p) n -> p c n", p=128), [nk, ncols])

    dma("sp", cst.ap, cst_d, "c0", wr=[cst])
    dma("sp", lnin.ap, lnin_d, "c1", wr=[lnin])
    dma("sp", pp.ap, pp_d, "c2", wr=[pp])
    dma("sp", fbv.ap, fb_d, "c3", wr=[fbv]); dma("sp", vkb.ap, vkb_d, "c4", wr=[vkb])
    dma("sp", vfm[0].ap, vfm_d[0], "c5", wr=[vfm[0]]); dma("sp", vfm[1].ap, vfm_d[1], "c6", wr=[vfm[1]])
    gathv = View(None, "gathd", 0, 1); sendv = View(None, "sendd", 0, 1)
    P.op("dve", lambda e: e.tensor_copy(cstb.ap[:, 0:128], cst.ap[:, 0:128]), reads=[cst], writes=[cstb])
    P.op("dve", lambda e: e.tensor_copy(cstb.ap[:, 128:256], cst.ap[:, 512:640]), reads=[cst], writes=[cstb])
    P.op("dve", lambda e: e.tensor_copy(cstb.ap[:, 256:1280], cst.ap[:, 768:1792]), reads=[cst], writes=[cstb])
    for l in range(NL):
        memset(tails[l], 0.0); memset(kprev[l], 0.0); memset(vprev[l], 0.0)
        zt = T.get(F32, [2048]) if l == 0 else zt
        if l == 0:
            memset(zt, 0.0)
        dma("sp", st_d[l], zt.ap, f"st{l}", rd=[zt], wr=[View(None, f"std{l}", 0, 1)])
    stv = [View(None, f"std{l}", 0, 1) for l in range(NL)]
    for b in range(NB):
        dma("sp", gath_d[b * 128:(b + 1) * 128, :], zt.ap[:, 0:D], f"gz{b}", rd=[zt], wr=[gathv])

    def ppv(l, a, b):
        return View(pp.ap[:, l * NPP + a:l * NPP + b], pp.key, pp.lo, pp.hi)

    def ln_fm(src, g, b, t0):
        TR.top = t0
        s1 = T.get(F32, [TT]); s2 = T.get(F32, [TT]); sq = T.get(F32, [KC, TT])
        red(s1, View(src.ap.rearrange("p c t -> p t c"), src.key, src.lo, src.hi))
        act(sq, src, AF.Square)
        red(s2, View(sq.ap.rearrange("p c t -> p t c"), sq.key, sq.lo, sq.hi))
        pm = bank(); pq = bank()
        mm(pm, onesD, s1); mm(pq, onesD, s2)
        mean = T.get(F32, [TT]); var = T.get(F32, [TT]); rstd = T.get(F32, [TT])
        cp(mean, pm, eng="act")
        tt(var, mean, mean, ALU.mult)
        tt(var, pq, var, ALU.subtract)
        ts(var, var, LN_EPS, None, ALU.add)
        act(var, var, AF.Sqrt)
        recip(rstd, var)
        tmps = [T.get(F32, [TT]), T.get(F32, [TT])]
        for c in range(KC):
            tmp = tmps[c % 2]
            tt(tmp, sub(src, slice(None), c), mean, ALU.subtract)
            tt(tmp, tmp, rstd, ALU.mult)
            act(sub(hres, slice(None), c), tmp, AF.Identity, bias=sub(b, slice(None), slice(c, c + 1)),
                scale=sub(g, slice(None), slice(c, c + 1)))
            cp(sub(hbf, slice(None), c), sub(hres, slice(None), c), eng="dve")

    def proj_fm(w_d, c0, nchunks, src, nk, consume, group=4):
        for j0 in range(0, nchunks, group):
            ng = min(group, nchunks - j0)
            wt = wtile_fm(w_d, 0, nk, c0 + j0 * 128, ng * 128) if nk * ng * 128 <= 4096 else None
            for jj in range(ng):
                ps = bank()
                for k in range(nk):
                    mm(ps, sub(wt, slice(None), k, slice(jj * 128, (jj + 1) * 128)), sub(src, slice(None), k),
                       start=(k == 0), stop=(k == nk - 1))
                consume(j0 + jj, ps)

    out_sems = []
    for s in range(NSTEP):
        P.epoch = s // cfg.get("epoch_steps", 3)
        TR.top = 0
        dma("sp", cosv.ap, cs_d[s, 0], "cos", wr=[cosv])
        dma("sp", sinv.ap, cs_d[s, 1], "sin", wr=[sinv])
        for b in range(NB):
            xt = T.get(F32, [D])
            dma("sp", xt.ap, x_d[s, b], f"x{b}", wr=[xt])
            ssum = T.get(F32, [1]); ssq = T.get(F32, [1]); junk = T.get(F32, [D])
            act(junk, xt, AF.Identity, accum=ssum)
            act(junk, xt, AF.Square, accum=ssq)
            mean = T.get(F32, [1]); var = T.get(F32, [1]); nmr = T.get(F32, [1])
            ts(mean, ssum, 1.0 / D, None, ALU.mult)
            ts(var, ssq, 1.0 / D, None, ALU.mult)
            tt(nmr, mean, mean, ALU.mult)
            tt(var, var, nmr, ALU.subtract)
            ts(var, var, LN_EPS, None, ALU.add)
            act(var, var, AF.Sqrt)
            recip(var, var)
            ts(xt, xt, mean, var, ALU.subtract, ALU.mult)
            rt = T.get(F32, [D])
            dma("sp", rt.ap, gath_d[b * 128:(b + 1) * 128, :], f"r{b}", rd=[gathv], wr=[rt])
            stt(xt, rt, sub(fbv, slice(None), slice(0, 1)), xt, ALU.mult, ALU.add)
            for c0 in range(0, KC, 4):
                ps = bank()
                for c in range(4):
                    tp(sub(ps, slice(None), slice(c * 128, (c + 1) * 128)), sub(xt, slice(None), slice((c0 + c) * 128, (c0 + c + 1) * 128)), identf)
                for c in range(4):
                    act(sub(hres, slice(None), c0 + c, slice(b * 128, (b + 1) * 128)), sub(ps, slice(None), slice(c * 128, (c + 1) * 128)),
                        AF.Identity, bias=sub(lnin, slice(None), slice(8 + c0 + c, 9 + c0 + c)), scale=sub(lnin, slice(None), slice(c0 + c, c0 + c + 1)))
        cp(hbf, hres, eng="dve")

        for l, kind in enumerate(layers):
            cw = View(pp.ap[:, l * NPP:l * NPP + 96].rearrange("p (c k) -> p c k", k=4), pp.key, pp.lo, pp.hi)
            cb = ppv(l, 96, 120)
            ln1g = ppv(l, 120, 128); ln1b = ppv(l, 128, 136); ln2g = ppv(l, 136, 144); ln2b = ppv(l, 144, 152)
            dtb = ppv(l, 152, 184); alog = ppv(l, 184, 216); dsk = ppv(l, 216, 248); snk = ppv(l, 248, 264)
            gn = ppv(l, 264, 280)
            TR.top = 0
            aneg = T.get(F32, [32]); esink = T.get(F32, [16])
            act(aneg, alog, AF.Exp)
            ts(aneg, aneg, -1.0, None, ALU.mult)
            act(esink, snk, AF.Exp)
            base_top = TR.top

            qbf = T.get(BF16, [KC, TT]); kz = T.get(BF16, [2, 4, TT]); vcur = T.get(BF16, [NB, 512])
            memset(kz, 0.0)
            for (cbase, n, sbase) in ((0, 8, 8), (16, 4, 20)):
                for j0 in range(0, n, 4):
                    wa = wload(wup_d[l][(cbase + j0) // 4], [KC, 512])
                    wb = wload(wup_d[l][(sbase + j0) // 4], [KC, 512])
                    for jj in range(4):
                        pa = bank(); pb = bank()
                        for k in range(KC):
                            mm(pa, sub(wa, slice(None), k, slice(jj * 128, (jj + 1) * 128)), sub(hbf, slice(None), k), start=(k == 0), stop=(k == KC - 1))
                        for k in range(KC):
                            mm(pb, sub(wb, slice(None), k, slice(jj * 128, (jj + 1) * 128)), sub(hbf, slice(None), k), start=(k == 0), stop=(k == KC - 1))
                        t1 = T.get(F32, [TT]) if (cbase == 0 and j0 == 0 and jj == 0) else t1
                        t2 = T.get(F32, [TT]) if (cbase == 0 and j0 == 0 and jj == 0) else t2
                        tt(t1, pa, cosv, ALU.mult)
                        tt(t2, pb, sinv, ALU.mult)
                        j = j0 + jj
                        if cbase == 0:
                            tt(sub(qbf, slice(None), j), t1, t2, ALU.add)
                        else:
                            for half in range(2):
                                pr = slice(half * 64, (half + 1) * 64)
                                tt(View(kz.ap[pr, half, j, :], kz.key, kz.lo, kz.hi), View(t1.ap[pr, :], t1.key, t1.lo, t1.hi),
                                   View(t2.ap[pr, :], t2.key, t2.lo, t2.hi), ALU.add)
            wv = wload(wup_d[l][14], [KC, 512])
            for b in range(NB):
                ps = bank()
                for k in range(KC):
                    mm(ps, sub(hbf, slice(None), k, slice(b * 128, (b + 1) * 128)), sub(wv, slice(None), k), start=(k == 0), stop=(k == KC - 1))
                cp(sub(vcur, slice(None), b), ps)
            att_top = TR.top
            def att_iter(b, g, it):
                qs = slice(b * 128, (b + 1) * 128)
                TR.top = att_top + (it % 3) * 4096
                pts = [T.get(BF16, [512]), T.get(BF16, [512])]
                rden = T.get(F32, [512])
                pss = []
                for kb in range(2):
                    ps = bank()
                    mm(ps, identb, mcur if kb == 1 else mprev, start=True, stop=False)
                    for slot in range(4):
                        cc, half = slot // 2, slot % 2
                        if kb == 1:
                            kv = sub(kz, slice(None), half, g, qs)
                        elif b > 0:
                            kv = sub(kz, slice(None), half, g, slice((b - 1) * 128, b * 128))
                        else:
                            kv = sub(kprev[l], slice(None), half, g)
                        mm(sub(ps, slice(None), slice(slot * 128, (slot + 1) * 128)), kv, sub(qbf, slice(None), 2 * g + cc, qs), start=False, stop=(slot == 3))
                    pss.append(ps)
                yield
                for kb in range(2):
                    kbias = None
                    if s < 2:
                        if kb == 1:
                            col = s * 4 + b
                        elif b > 0:
                            col = s * 4 + b - 1
                        else:
                            col = 3 if s == 1 else None
                        kbias = kball if col is None else sub(vkb, slice(None), slice(col, col + 1))
                    act(pts[kb], pss[kb], AF.Exp, bias=kbias, scale=0.125)
                yield
                pn = bank(); pd = bank()
                for kb in range(2):
                    if kb == 1:
                        vv = sub(vcur, slice(None), b, slice(g * 128, (g + 1) * 128))
                    elif b > 0:
                        vv = sub(vcur, slice(None), b - 1, slice(g * 128, (g + 1) * 128))
                    else:
                        vv = sub(vprev[l], slice(None), slice(g * 128, (g + 1) * 128))
                    mm(pn, vv, pts[kb], start=(kb == 0), stop=(kb == 1))
                for kb in range(2):
                    mm(pd, onesb, pts[kb], start=(kb == 0), stop=(kb == 1))
                yield
                for slot in range(4):
                    cc, half = slot // 2, slot % 2
                    h = 4 * g + 2 * cc + half
                    sl = slice(slot * 128, (slot + 1) * 128)
                    ts(sub(rden, slice(None), sl), sub(pd, slice(None), sl), sub(esink, slice(None), slice(h, h + 1)), None, ALU.add)
                recip(rden, rden)
                yield
                for slot in range(4):
                    cc, half = slot // 2, slot % 2
                    pr = slice(half * 64, (half + 1) * 64)
                    sl = slice(slot * 128, (slot + 1) * 128)
                    alo = attnT.lo + ((2 * g + cc) * TT + b * 128) * 2
                    tt(View(attnT.ap[pr, 2 * g + cc, qs], attnT.key, alo, alo + 256),
                       View(pn.ap[pr, sl], pn.key, pn.lo, pn.hi), View(rden.ap[pr, sl], rden.key, rden.lo, rden.hi), ALU.mult)
            interleave([att_iter(b, g, b * 4 + g) for b in range(NB) for g in range(4)], 3)
            TR.top = att_top + 3 * 4096
            cp(kprev[l], sub(kz, slice(None), slice(None), slice(None), slice(TT - 128, TT)), eng="dve")
            cp(vprev[l], sub(vcur, slice(None), NB - 1), eng="dve")

            TR.top = base_top
            state = T.get(F32, [4, 512])
            dma("sp", state.ap, st_d[l].rearrange("p (g n) -> p g n", g=4), f"sl{l}", rd=[stv[l]], wr=[state])
            dtt = T.get(F32, [NB, 32]); dat = T.get(F32, [NB, 32]); expA = T.get(F32, [NB, 32]); cdec = T.get(F32, [NB, 32])
            wdt = wload(wdtw_d[l], [KC, 32])
            for b in range(NB):
                ps = bank()
                for k in range(KC):
                    mm(sub(ps, slice(None), slice(0, 32)), sub(hbf, slice(None), k, slice(b * 128, (b + 1) * 128)), sub(wdt, slice(None), k), start=(k == 0), stop=(k == KC - 1))
                tt(sub(dtt, slice(None), b), sub(ps, slice(None), slice(0, 32)), dtb, ALU.add)
            act(dtt, dtt, AF.Exp)
            act(dtt, dtt, AF.Ln, bias=1.0)
            for b in range(NB):
                tt(sub(dat, slice(None), b), sub(dtt, slice(None), b), aneg, ALU.mult)
                ps = bank()
                mm(sub(ps, slice(None), slice(0, 32)), tri, sub(dat, slice(None), b))
                mm(sub(ps, slice(None), slice(32, 64)), onesf, sub(dat, slice(None), b))
                act(sub(expA, slice(None), b), sub(ps, slice(None), slice(0, 32)), AF.Exp)
                act(sub(cdec, slice(None), b), sub(ps, slice(None), slice(32, 64)), AF.Exp)
            ssd_top = TR.top
            for g in range(4):
                TR.top = ssd_top
                xg = T.get(BF16, [6, TT])
                xpre = [T.get(F32, [TT + 3]) for _ in range(2)]
                cacc = [T.get(F32, [TT]) for _ in range(2)]
                chunks = [4 * g + i for i in range(4)] + [16 + g, 20 + g]
                wx1 = wload(wup_d[l][6 + g], [KC, 512])
                wx2 = wload(wbc_d[l][g], [KC, 256])
                for i, c in enumerate(chunks):
                    wt = wx1 if i < 4 else wx2
                    co = (i * 128) if i < 4 else (i - 4) * 128
                    ps = bank()
                    for k in range(KC):
                        mm(ps, sub(wt, slice(None), k, slice(co, co + 128)), sub(hbf, slice(None), k), start=(k == 0), stop=(k == KC - 1))
                    xp = xpre[i % 2]; ca = cacc[i % 2]
                    cp(sub(xp, slice(None), slice(0, 3)), sub(tails[l], slice(None), c), eng="dve")
                    cp(sub(xp, slice(None), slice(3, TT + 3)), ps, eng="act")
                    if s < 2:
                        tt(sub(xp, slice(None), slice(3, TT + 3)), sub(xp, slice(None), slice(3, TT + 3)), vfm[s], ALU.mult)
                    cp(sub(tails[l], slice(None), c), sub(xp, slice(None), slice(TT, TT + 3)), eng="dve")
                    ts(ca, sub(xp, slice(None), slice(0, TT)), sub(cw, slice(None), c, slice(0, 1)), sub(cb, slice(None), slice(c, c + 1)), ALU.mult, ALU.add)
                    for k in range(1, 4):
                        stt(ca, sub(xp, slice(None), slice(k, TT + k)), sub(cw, slice(None), c, slice(k, k + 1)), ca, ALU.mult, ALU.add)
                    act(sub(xg, slice(None), i), ca, AF.Silu)
                    if s < 2 and i < 4:
                        tt(sub(xg, slice(None), i), sub(xg, slice(None), i), vfm[s], ALU.mult)
                wz = wload(wup_d[l][10 + g], [KC, 512])
                blk_top = TR.top
                def ssd_blk(b, g=g, xg=xg, wz=wz):
                    TR.top = blk_top + (b % 2) * 24 * 1024
                    qs = slice(b * 128, (b + 1) * 128)
                    xs_t = T.get(BF16, [8, 64]); xc = T.get(BF16, [8, 64]); xd = T.get(BF16, [8, 64]); btok = T.get(BF16, [128])
                    rhsD = T.get(F32, [8, 128]); E = T.get(F32, [8, 128]); w1 = T.get(F32, [8]); mcb = T.get(F32, [128])
                    MT = T.get(BF16, [8, 128]); sbf = T.get(BF16, [512]); yv = T.get(F32, [8, 64]); xsd = T.get(F32, [8, 64])
                    sz = T.get(F32, [512]); ss = T.get(F32, [1]); ub = T.get(BF16, [512])
                    pt = bank(BF16)
                    for i in range(4):
                        tp(sub(pt, slice(None), slice(i * 128, (i + 1) * 128)), sub(xg, slice(None), i, qs), identb)
                    tp(sub(pt, slice(None), slice(512, 640)), sub(xg, slice(None), 4, qs), identb)
                    dag = View(dat.ap[:, b, g * 8:(g + 1) * 8], dat.key, dat.lo, dat.hi)
                    tt(rhsD, View(tri.ap.unsqueeze(1).to_broadcast([128, 8, 128]), tri.key, tri.lo, tri.hi),
                       View(dag.ap.unsqueeze(2).to_broadcast([128, 8, 128]), dag.key, dag.lo, dag.hi), ALU.mult)
                    pcb = bank()
                    mm(sub(pcb, slice(None), slice(0, 128)), sub(xg, slice(None), 4, qs), sub(xg, slice(None), 5, qs))
                    pz = bank()
                    for k in range(KC):
                        mm(pz, sub(hbf, slice(None), k, qs), sub(wz, slice(None), k), start=(k == 0), stop=(k == KC - 1))
                    yield
                    ptx = View(pt.ap[:, 0:512].rearrange("p (h d) -> p h d", d=64), pt.key, pt.lo, pt.hi)
                    cp(xs_t, ptx, eng="act")
                    cp(btok, sub(pt, slice(None), slice(512, 640)), eng="act")
                    act(sz, pz, AF.Silu)
                    tt(mcb, sub(pcb, slice(None), slice(0, 128)), tri, ALU.mult)
                    pseg = [bank(), bank()]
                    for hf in range(2):
                        mm(pseg[hf], ustr, View(rhsD.ap[:, hf * 4:(hf + 1) * 4, :].rearrange("p h l -> p (h l)"), rhsD.key, rhsD.lo, rhsD.hi))
                    yield
                    for hf in range(2):
                        act(View(E.ap[:, hf * 4:(hf + 1) * 4, :].rearrange("p h l -> p (h l)"), E.key, E.lo, E.hi), pseg[hf], AF.Exp)
                    yield
                    dtg = View(dtt.ap[:, b, g * 8:(g + 1) * 8], dtt.key, dtt.lo, dtt.hi)
                    tt(w1, dtg, View(E.ap[:, :, 127], E.key, E.lo, E.hi), ALU.mult)
                    tt(xc, xs_t, View(dtg.ap.unsqueeze(2).to_broadcast([128, 8, 64]), dtg.key, dtg.lo, dtg.hi), ALU.mult)
                    tt(xd, xs_t, View(w1.ap.unsqueeze(2).to_broadcast([128, 8, 64]), w1.key, w1.lo, w1.hi), ALU.mult)
                    tt(MT, E, View(mcb.ap.unsqueeze(1).to_broadcast([128, 8, 128]), mcb.key, mcb.lo, mcb.hi), ALU.mult)
                    dsg = View(dsk.ap[:, g * 8:(g + 1) * 8], dsk.key, dsk.lo, dsk.hi)
                    tt(xsd, xs_t, View(dsg.ap.unsqueeze(2).to_broadcast([128, 8, 64]), dsg.key, dsg.lo, dsg.hi), ALU.mult)
                    yield
                    py = bank()
                    for hh in range(8):
                        mm(sub(py, slice(None), slice(hh * 64, (hh + 1) * 64)), sub(MT, slice(None), hh), sub(xc, slice(None), hh))
                    pst = bank()
                    mm(pst, btok, View(xd.ap.rearrange("p h d -> p (h d)"), xd.key, xd.lo, xd.hi))
                    yield
                    po = bank()
                    cp(sbf, sub(state, slice(None), g), eng="dve")
                    mm(po, sub(xg, slice(None), 5, qs), sbf)
                    cdg = View(cdec.ap[:, b, g * 8:(g + 1) * 8], cdec.key, cdec.lo, cdec.hi)
                    sgv = sub(state, slice(None), g)
                    sg3 = View(state.ap[:, g, :].rearrange("p (h d) -> p h d", d=64), state.key, sgv.lo, sgv.hi)
                    tt(sg3, sg3, View(cdg.ap.unsqueeze(2).to_broadcast([128, 8, 64]), cdg.key, cdg.lo, cdg.hi), ALU.mult)
                    tt(sgv, sgv, pst, ALU.add)
                    yield
                    eag = View(expA.ap[:, b, g * 8:(g + 1) * 8], expA.key, expA.lo, expA.hi)
                    po3 = View(po.ap.rearrange("p (h d) -> p h d", d=64), po.key, po.lo, po.hi)
                    py3 = View(py.ap.rearrange("p (h d) -> p h d", d=64), py.key, py.lo, py.hi)
                    tt(yv, po3, View(eag.ap.unsqueeze(2).to_broadcast([128, 8, 64]), eag.key, eag.lo, eag.hi), ALU.mult)
                    tt(yv, yv, py3, ALU.add)
                    tt(yv, yv, xsd, ALU.add)
                    y2 = View(yv.ap.rearrange("p h d -> p (h d)"), yv.key, yv.lo, yv.hi)
                    tt(y2, y2, sz, ALU.mult)
                    yield
                    act(sz, y2, AF.Square, accum=ss)
                    ts(ss, ss, 1.0 / 512, RMS_EPS, ALU.mult, ALU.add)
                    act(ss, ss, AF.Sqrt)
                    recip(ss, ss)
                    ts(ub, y2, ss, None, ALU.mult)
                    yield
                    pu = bank(BF16)
                    for i in range(4):
                        tp(sub(pu, slice(None), slice(i * 128, (i + 1) * 128)), sub(ub, slice(None), slice(i * 128, (i + 1) * 128)), identb)
                    yield
                    for i in range(4):
                        act(sub(unT, slice(None), 4 * g + i, qs), sub(pu, slice(None), slice(i * 128, (i + 1) * 128)), AF.Identity,
                            scale=sub(gn, slice(None), slice(4 * g + i, 4 * g + i + 1)))
                interleave([ssd_blk(b) for b in range(NB)], 2)
            dma("sp", st_d[l].rearrange("p (g n) -> p g n", g=4), state.ap, f"ss{l}", rd=[state], wr=[stv[l]])

            TR.top = base_top
            mT = T.get(BF16, [KC, TT])
            for c in range(KC):
                w1t = wload(wmg_d[l][c, :, 0:24, :], [24, 128])
                w2t = wload(wmg_d[l][c, :, 24:40, :], [16, 128])
                pa = bank(); psd = bank(); pga = bank(); pgs = bank()
                for k in range(KC):
                    mm(pa, sub(w1t, slice(None), k), sub(attnT, slice(None), k), start=(k == 0), stop=(k == KC - 1))
                for k in range(16):
                    mm(psd, sub(w1t, slice(None), 8 + k), sub(unT, slice(None), k), start=(k == 0), stop=(k == 15))
                for k in range(KC):
                    mm(pga, sub(w2t, slice(None), k), sub(hbf, slice(None), k), start=(k == 0), stop=(k == KC - 1))
                for k in range(KC):
                    mm(pgs, sub(w2t, slice(None), 8 + k), sub(hbf, slice(None), k), start=(k == 0), stop=(k == KC - 1))
                if c == 0:
                    sa = T.get(F32, [TT]); sb_ = T.get(F32, [TT])
                act(sa, pga, AF.Sigmoid); act(sb_, pgs, AF.Sigmoid)
                tt(sa, sa, pa, ALU.mult); tt(sb_, sb_, psd, ALU.mult)
                tt(sub(mT, slice(None), c), sa, sb_, ALU.add)
            for c in range(KC):
                wo = wload(wo_d[l][c], [KC, 128])
                po = bank()
                for k in range(KC):
                    mm(po, sub(wo, slice(None), k), sub(mT, slice(None), k), start=(k == 0), stop=(k == KC - 1))
                stt(sub(hres, slice(None), c), sub(hres, slice(None), c), ALPHA, po, ALU.mult, ALU.add)
            ln_fm(hres, ln1g, ln1b, TR.top)

            TR.top = base_top
            if kind == "dense":
                NF = DFF // 128
                actb = T.get(BF16, [NF, TT])
                tus = [T.get(F32, [TT]), T.get(F32, [TT])]
                for j0 in range(0, NF, 2):
                    ng = 2
                    wgt = wload(wg_d[l][0, j0 // 2], [KC, 256])
                    wut = wload(wu_d[l][0, j0 // 2], [KC, 256])
                    for jj in range(ng):
                        pg = bank(); pu = bank()
                        for k in range(KC):
                            mm(pg, sub(wgt, slice(None), k, slice(jj * 128, (jj + 1) * 128)), sub(hbf, slice(None), k), start=(k == 0), stop=(k == KC - 1))
                        for k in range(KC):
                            mm(pu, sub(wut, slice(None), k, slice(jj * 128, (jj + 1) * 128)), sub(hbf, slice(None), k), start=(k == 0), stop=(k == KC - 1))
                        tq = tus[(j0 + jj) % 2]
                        act(tq, pg, AF.Silu)
                        tt(sub(actb, slice(None), j0 + jj), tq, pu, ALU.mult)
                for c0 in range(0, KC, 4):
                    pos = [bank() for _ in range(4)]
                    for k0 in range(0, NF, 8):
                        nk = min(8, NF - k0)
                        wdt_ = wload(wd_d[l][c0 // 4, :, k0:k0 + nk, :], [nk, 512])
                        for c in range(4):
                            for k in range(nk):
                                mm(pos[c], sub(wdt_, slice(None), k, slice(c * 128, (c + 1) * 128)), sub(actb, slice(None), k0 + k),
                                   start=(k0 + k == 0), stop=(k0 + k == NF - 1))
                    for c in range(4):
                        stt(sub(hres, slice(None), c0 + c), sub(hres, slice(None), c0 + c), ALPHA, pos[c], ALU.mult, ALU.add)
            else:
                NF = DFFE // 128
                actb = T.get(BF16, [NF, TT]); tu = T.get(F32, [TT]); tus = [tu, T.get(F32, [TT])]
                gateb = T.get(F32, [NE, TT])
                wrt = T.get(F32, [KC, NE])
                dma("sp", wrt.ap, wr_d[l].rearrange("(c p) e -> p c e", p=128), "wr", wr=[wrt])
                for b in range(NB):
                    qs = slice(b * 128, (b + 1) * 128)
                    ps = bank()
                    for k in range(KC):
                        mm(sub(ps, slice(None), slice(0, NE)), sub(hres, slice(None), k, qs), sub(wrt, slice(None), k), start=(k == 0), stop=(k == KC - 1))
                    lg = T.get(F32, [NE]); m1 = T.get(F32, [1]); m2 = T.get(F32, [1]); msk = T.get(F32, [NE]); gt = T.get(F32, [NE]); dg = T.get(F32, [128])
                    cp(lg, sub(ps, slice(None), slice(0, NE)), eng="dve")
                    red(m1, lg, ALU.max)
                    ts(msk, lg, m1, NEG, ALU.is_ge, ALU.mult)
                    tt(msk, msk, lg, ALU.add)
                    red(m2, msk, ALU.max)
                    ts(gt, lg, m1, None, ALU.subtract)
                    act(gt, gt, AF.Exp)
                    ts(msk, lg, m2, None, ALU.is_ge)
                    tt(gt, gt, msk, ALU.mult)
                    red(m1, gt, ALU.add)
                    recip(m1, m1)
                    ts(gt, gt, m1, None, ALU.mult)
                    for e_ in range(NE):
                        ts(dg, identf, sub(gt, slice(None), slice(e_, e_ + 1)), None, ALU.mult)
                        pb_ = bank()
                        mm(sub(pb_, slice(None), slice(0, 128)), onesf, dg)
                        cp(sub(gateb, slice(None), e_, qs), sub(pb_, slice(None), slice(0, 128)))
                ts(hres, hres, ALPHA, None, ALU.mult)
                for e_ in range(NE):
                    for j0 in range(0, NF, 4):
                        ng = min(4, NF - j0)
                        wgt = wload(wg_d[l][e_, j0 // 4], [KC, 512])
                        wut = wload(wu_d[l][e_, j0 // 4], [KC, 512])
                        for jj in range(ng):
                            pg = bank(); pu = bank()
                            for k in range(KC):
                                mm(pg, sub(wgt, slice(None), k, slice(jj * 128, (jj + 1) * 128)), sub(hbf, slice(None), k), start=(k == 0), stop=(k == KC - 1))
                            for k in range(KC):
                                mm(pu, sub(wut, slice(None), k, slice(jj * 128, (jj + 1) * 128)), sub(hbf, slice(None), k), start=(k == 0), stop=(k == KC - 1))
                            tq = tus[(j0 + jj) % 2]
                            act(tq, pg, AF.Silu)
                            tt(sub(actb, slice(None), j0 + jj), tq, pu, ALU.mult)
                    for c in range(KC):
                        wds = [wload(wd_d[l][e_, c, :, k0:k0 + 14, :], [14, 128]) for k0 in range(0, NF, 14)]
                        po = bank()
                        for k in range(NF):
                            mm(po, sub(wds[k // 14], slice(None), k % 14), sub(actb, slice(None), k), start=(k == 0), stop=(k == NF - 1))
                        tt(tu, po, sub(gateb, slice(None), e_), ALU.mult)
                        tt(sub(hres, slice(None), c), sub(hres, slice(None), c), tu, ALU.add)
            ln_fm(hres, ln2g, ln2b, TR.top)

        TR.top = 0
        for b in range(NB):
            ot = T.get(F32, [D])
            for c0 in range(0, KC, 4):
                ps = bank()
                for c in range(4):
                    tp(sub(ps, slice(None), slice(c * 128, (c + 1) * 128)), sub(hres, slice(None), c0 + c, slice(b * 128, (b + 1) * 128)), identf)
                cp(sub(ot, slice(None), slice(c0 * 128, (c0 + 4) * 128)), ps)
            o = dma("sp", y_d[s, b], ot.ap, f"o{b}", rd=[ot], wr=[])
            out_sems.append(o)
            if s < NSTEP - 1:
                dma("sp", send_d[b * 128:(b + 1) * 128, :], ot.ap, f"sd{b}", rd=[ot], wr=[sendv])
        if s < NSTEP - 1:
            cco = P.op("pool", lambda e: e.collective_compute("AllGather", ALU.bypass, replica_groups=[[0, 1], [2, 3], [4, 5], [6, 7]],
                                                           ins=[send_d.opt()], outs=[gath_d.opt()]), reads=[sendv], writes=[gathv])
            cco.noval = True

    def final_waits():
        last = {}
        for o in out_sems:
            last[id(o.sem)] = (o.sem, o.semval)
        return list(last.values())
    P.emit(final_waits)
    return nc, es


def _consts():
    c = np.zeros((128, 6 * 128 + 1024 + 2), np.float32)
    i = np.arange(128)
    c[:, 0:128] = np.eye(128)
    c[:, 128:256] = (i[:, None] <= i[None, :])
    c[:, 256:384] = (i[:, None] > i[None, :])
    c[:, 384:512] = 1.0 / D
    c[:, 512:640] = 1.0
    mprev = np.where(i[:, None] > i[None, :], 0.0, NEG)
    mcur = np.where(i[:, None] <= i[None, :], 0.0, NEG)
    c[:, 768:768 + 512] = np.tile(mprev, (1, 4))
    c[:, 768 + 512:768 + 1024] = np.tile(mcur, (1, 4))
    c[:, 768 + 1024] = np.where(i < PAD, NEG, 0.0)
    c[:, 768 + 1025] = NEG
    return c


def _cossin(nstep):
    T_ = nstep * TT
    pos = (np.arange(T_) - PAD).astype(np.float32)
    inv = (np.float32(10000.0) ** (-np.arange(0, 64, 2, dtype=np.float32) / np.float32(64))).astype(np.float32)
    ang = (pos[:, None] * inv[None, :]).astype(np.float32)
    cos = np.cos(ang).astype(np.float32); sin = np.sin(ang).astype(np.float32)
    p = np.arange(128)
    f = p % 32
    sign = np.where((p % 64) < 32, -1.0, 1.0).astype(np.float32)
    cosT = cos[:, f].T
    sinT = sin[:, f].T * sign[:, None]
    out = np.zeros((nstep, 2, 128, TT), np.float32)
    for s in range(nstep):
        out[s, 0] = cosT[:, s * TT:(s + 1) * TT]
        out[s, 1] = sinT[:, s * TT:(s + 1) * TT]
    return out


def _fm(v, n):
    return np.ascontiguousarray(v.reshape(n, 128).T)


def _layer_inputs(inp, l, li, kind, m):
    w_in = inp["w_in"][l]
    q = w_in[:, 2048:3072]; k = w_in[:, 3072:3328]; v = w_in[:, 3328:3584]
    z = w_in[:, 3584:5632]; xbc = w_in[:, 5632:8704]; dtw = w_in[:, 8704:8736]
    ga = w_in[:, 0:1024]; gs = w_in[:, 1024:2048]
    sw = (np.arange(64) + 32) % 64
    qs = q.reshape(D, 16, 64)[:, :, sw].reshape(D, 1024)
    ks = k.reshape(D, 4, 64)[:, :, sw].reshape(D, 256)
    dup = lambda a: np.repeat(a.reshape(D, 4, 1, 64), 2, axis=2).reshape(D, 512)
    def grp(W, gc):
        return W.reshape(KC, 128, W.shape[1] // gc, gc).transpose(2, 1, 0, 3)
    xs_w = xbc[:, 0:2048]; b_w = xbc[:, 2048:2560]; c_w = xbc[:, 2560:3072]
    m[f"wup{li}"] = np.ascontiguousarray(np.concatenate([grp(q, 512), grp(qs, 512), grp(dup(k), 512), grp(dup(ks), 512),
                                                         grp(xs_w, 512), grp(z, 512), grp(dup(v), 512)], axis=0))
    bc = np.concatenate([b_w.reshape(D, 4, 128), c_w.reshape(D, 4, 128)], axis=2).reshape(D, 1024)
    m[f"wbc{li}"] = np.ascontiguousarray(grp(bc, 256))
    m[f"wdtw{li}"] = np.ascontiguousarray(dtw.reshape(KC, 128, 32).transpose(1, 0, 2))
    def cpk(W):
        return W.reshape(W.shape[0] // 128, 128, 8, 128).transpose(2, 1, 0, 3)
    m[f"wmg{li}"] = np.ascontiguousarray(np.concatenate([cpk(inp["w_attn_out"][l]), cpk(inp["w_ssd_out"][l]), cpk(ga), cpk(gs)], axis=2))
    m[f"wo{li}"] = np.ascontiguousarray(cpk(inp["w_o"][l]))
    if kind == "dense":
        m[f"wg{li}"] = np.ascontiguousarray(grp(inp["ffn_wg"][l // 2], 256)[None]); m[f"wu{li}"] = np.ascontiguousarray(grp(inp["ffn_wu"][l // 2], 256)[None])
        m[f"wd{li}"] = np.ascontiguousarray(inp["ffn_wd"][l // 2].reshape(DFF // 128, 128, 2, 512).transpose(2, 1, 0, 3))
    else:
        m[f"wg{li}"] = np.ascontiguousarray(inp["moe_wg"][l // 2].reshape(NE, KC, 128, DFFE // 512, 512).transpose(0, 3, 2, 1, 4))
        m[f"wu{li}"] = np.ascontiguousarray(inp["moe_wu"][l // 2].reshape(NE, KC, 128, DFFE // 512, 512).transpose(0, 3, 2, 1, 4)); m[f"wd{li}"] = np.ascontiguousarray(inp["moe_wd"][l // 2].reshape(NE, DFFE // 128, 128, 8, 128).transpose(0, 3, 2, 1, 4))
        m[f"wr{li}"] = inp["moe_router"][l // 2]
    pp = np.zeros((128, NPP), np.float32)
    pp[:, 0:96] = np.transpose(inp["conv_w"][l].reshape(4, 24, 128), (2, 1, 0)).reshape(128, 96)
    pp[:, 96:120] = _fm(inp["conv_b"][l], 24)
    pp[:, 120:128] = _fm(inp["ln1_g"][l], 8); pp[:, 128:136] = _fm(inp["ln1_b"][l], 8)
    pp[:, 136:144] = _fm(inp["ln2_g"][l], 8); pp[:, 144:152] = _fm(inp["ln2_b"][l], 8)
    pp[:, 152:184] = inp["dt_bias"][l][None, :]; pp[:, 184:216] = inp["a_log"][l][None, :]
    pp[:, 216:248] = inp["d_skip"][l][None, :]; pp[:, 248:264] = inp["sinks"][l][None, :]
    pp[:, 264:280] = _fm(inp["ssd_norm_g"][l], 16)
    return pp


def _masks(valid2):
    vfm = np.ascontiguousarray(np.broadcast_to(valid2[:, None, :], (2, 128, TT))).astype(np.float32)
    vkb = np.ascontiguousarray(((valid2.reshape(2, NB, 128) - 1.0) * (-NEG)).transpose(2, 0, 1).reshape(128, 8)).astype(np.float32)
    return vfm, vkb


def make_inputs(inp, stages, nstep, seqs):
    nst = len(stages)
    stage_shared = []
    kinds = None
    for layer_ids in stages:
        kinds = ["dense" if l % 2 == 0 else "moe" for l in layer_ids]
        sh = {}
        pps = [_layer_inputs(inp, l, li, kinds[li], sh) for li, l in enumerate(layer_ids)]
        sh["pp"] = np.ascontiguousarray(np.concatenate(pps, axis=1))
        sh["cst"] = _consts()
        sh["cossin"] = None
        stage_shared.append(sh)
    cs = _cossin(nstep)
    lnin_real = np.ascontiguousarray(np.concatenate([_fm(inp["ln_in_g"], 8), _fm(inp["ln_in_b"], 8)], axis=1))
    lnin_id = np.ascontiguousarray(np.concatenate([np.ones((128, 8), np.float32), np.zeros((128, 8), np.float32)], axis=1))
    vpad = np.ones(TT, np.float32); vpad[:PAD] = 0.0
    maps = []
    Tn = nstep * TT
    for b in seqs:
        for si in range(nst):
            m = dict(stage_shared[si])
            xs = np.zeros((Tn, D), np.float32)
            valid2 = np.ones((2, TT), np.float32)
            if si == 0:
                xs[PAD:PAD + NMETA] = inp["meta_tokens"]
                n = min(Tn - 128 - (nst - 1) * TT, inp["x"].shape[1])
                xs[128:128 + n] = inp["x"][b, :n]
                valid2[0] = vpad
                m["lnin"] = lnin_real; m["fb"] = np.zeros((128, 1), np.float32)
                m["cossin"] = cs
            else:
                valid2[0] = 0.0; valid2[1] = vpad
                m["lnin"] = lnin_id; m["fb"] = np.ones((128, 1), np.float32)
                cs1 = np.zeros_like(cs); cs1[1:] = cs[:-1]; cs1[0] = cs[0]
                m["cossin"] = cs1
            m["vfm"], m["vkb"] = _masks(valid2)
            m["x"] = xs.reshape(nstep, NB, 128, D)
            maps.append(m)
    return maps, kinds


NTILE_FULL = 17
STAGES = [[0, 1], [2, 3]]


def kernel(**inputs):
    inp = {k: np.asarray(v) for k, v in inputs.items()}
    nstep = NTILE_FULL + len(STAGES) - 1
    maps, kinds = make_inputs(inp, STAGES, nstep, list(range(4)))
    nc, es = build(dict(layers=kinds, nstep=nstep))
    with es:
        res = run_bass_kernel_spmd(nc, maps, core_ids=list(range(8)))
    S = inp["x"].shape[1]
    outs = []
    for b in range(4):
        y = res.results[2 * b + 1]["y"].reshape(nstep, TT, D)[1:].reshape(NTILE_FULL * TT, D)
        outs.append(y[128:128 + S])
    return np.stack(outs, axis=0).astype(np.float32)
```
